# Optimizing a Trainium2 kernel written in Bass

```python
import jax
import jax.numpy as jnp
from jax import lax
import numpy as np


D_MODEL = 4096
BATCH = 2
SEQ = 4096
DEPTH = 4

GRID_W = 64
CTX_LEN = 256
N_MIXERS = 3
EXPAND = 2
D_INNER = EXPAND * D_MODEL
MLSTM_HEADS = 8
MLSTM_HEAD_DIM = D_INNER // MLSTM_HEADS
QKV_BLOCK = 4
CONV_W = 4
CHUNK = 64
POOL_WINDOWS = (2, 4, 8, 16)
N_GROUPS = 4
GROUP_W = D_INNER // N_GROUPS
F_BIAS_LO = 3.0
F_BIAS_HI = 6.0
POS_BASE = 10000.0
EPS = 1e-6

kernel_name = 'hybrid_mlstm_pool_fourier_prefix'


def rmsnorm(x, g):
    xf = x.astype(jnp.float32)
    y = xf * lax.rsqrt(jnp.mean(xf * xf, axis=-1, keepdims=True) + EPS)
    return (y * g.astype(jnp.float32)).astype(x.dtype)


def pos_embed_2d(n_tok, dim):
    rows = n_tok // GRID_W
    r = jnp.repeat(jnp.arange(rows, dtype=jnp.float32), GRID_W)
    col = jnp.tile(jnp.arange(GRID_W, dtype=jnp.float32), rows)
    quarter = dim // 4
    omega = 1.0 / (POS_BASE ** (jnp.arange(quarter, dtype=jnp.float32) / quarter))

    def axis_emb(p):
        a = p[:, None] * omega[None, :]
        return jnp.concatenate([jnp.sin(a), jnp.cos(a)], axis=-1)

    return jnp.concatenate([axis_emb(r), axis_emb(col)], axis=-1)


def centred_dwconv(u, w, b):
    t = u.shape[1]
    left = (CONV_W - 1) // 2
    up = jnp.pad(u, ((0, 0), (left, CONV_W - 1 - left), (0, 0)))
    out = up[:, 0:t] * w[0]
    for j in range(1, CONV_W):
        out = out + up[:, j:j + t] * w[j]
    return out + b


def blockdiag(u, w):
    bsz, t, e = u.shape
    ub = u.reshape(bsz, t, e // QKV_BLOCK, QKV_BLOCK)
    return jnp.einsum('btgi,gio->btgo', ub, w).reshape(bsz, t, e)


def to_heads(a):
    bsz, t, _ = a.shape
    return a.reshape(bsz, t, MLSTM_HEADS, MLSTM_HEAD_DIM).transpose(0, 2, 1, 3)


def flip_time(a, rev):
    return jnp.flip(a, axis=2) if rev else a


def zero_state(bsz):
    c0 = jnp.zeros((bsz, MLSTM_HEADS, MLSTM_HEAD_DIM, MLSTM_HEAD_DIM), jnp.float32)
    n0 = jnp.zeros((bsz, MLSTM_HEADS, MLSTM_HEAD_DIM), jnp.float32)
    m0 = jnp.zeros((bsz, MLSTM_HEADS), jnp.float32)
    return (c0, n0, m0)


def chunk_scan(q, k, v, log_i, log_f, state, with_h):
    bsz, nh, t, _ = q.shape
    dv = v.shape[-1]
    nc = t // CHUNK

    def to_chunks(a):
        a = a.astype(jnp.float32).reshape((bsz, nh, nc, CHUNK) + a.shape[3:])
        return jnp.moveaxis(a, 2, 0)

    xs = (to_chunks(q), to_chunks(k), to_chunks(v), to_chunks(log_i), to_chunks(log_f))
    causal = jnp.tril(jnp.ones((CHUNK, CHUNK), dtype=bool))

    def step(carry, inp):
        cmat, nvec, m = carry
        qc, kc, vc, li, lf = inp
        b = jnp.cumsum(lf, axis=-1)
        b_end = b[..., -1]
        g = b_end[..., None] - b + li
        m_new = jnp.maximum(b_end + m, jnp.max(g, axis=-1))
        wg = jnp.exp(g - m_new[..., None])
        decay = jnp.exp(b_end + m - m_new)
        c_new = decay[..., None, None] * cmat + jnp.einsum('bhsv,bhsd->bhvd', vc * wg[..., None], kc)
        n_new = decay[..., None] * nvec + jnp.einsum('bhs,bhsd->bhd', wg, kc)
        if not with_h:
            return (c_new, n_new, m_new), None
        logw = jnp.where(causal, b[..., :, None] - b[..., None, :] + li[..., None, :], -jnp.inf)
        inter = b + m[..., None]
        m_t = jnp.maximum(jnp.max(logw, axis=-1), inter)
        s = jnp.einsum('bhtd,bhsd->bhts', qc, kc) * jnp.exp(logw - m_t[..., None])
        w_inter = jnp.exp(inter - m_t)
        num = jnp.einsum('bhts,bhsv->bhtv', s, vc) + w_inter[..., None] * jnp.einsum('bhvd,bhtd->bhtv', cmat, qc)
        den = jnp.sum(s, axis=-1) + w_inter * jnp.einsum('bhd,bhtd->bht', nvec, qc)
        h = num / jnp.maximum(jnp.abs(den), jnp.exp(-m_t))[..., None]
        return (c_new, n_new, m_new), h

    state, hs = lax.scan(step, state, xs)
    if not with_h:
        return None, state
    h = jnp.moveaxis(hs, 0, 2).reshape(bsz, nh, t, dv)
    return h, state


def mlstm_branch(hl, hc, w_in, conv_w, conv_b, wq, wk, wv, w_ig, b_ig, w_fg, b_fg,
                 hnorm_w, skip, w_out, ctx_out):
    def project(h):
        u, z = jnp.split(h @ w_in, 2, axis=-1)
        xc = jax.nn.silu(centred_dwconv(u, conv_w, conv_b))
        q = blockdiag(xc, wq)
        k = blockdiag(xc, wk)
        v = blockdiag(u, wv)
        gin = jnp.concatenate([q, k, v], axis=-1)
        li = (jnp.einsum('bte,zen->zbnt', gin, w_ig) + b_ig[:, None, :, None]).astype(jnp.float32)
        lf = jax.nn.log_sigmoid(
            (jnp.einsum('bte,zen->zbnt', gin, w_fg) + b_fg[:, None, :, None]).astype(jnp.float32))
        heads = (to_heads(q), to_heads(k) * (MLSTM_HEAD_DIM ** -0.5), to_heads(v), li, lf)
        return heads, xc, z

    pc, xc_c, z_c = project(hc)
    pl, xc_l, z_l = project(hl)
    bsz_c = hc.shape[0]
    h_lat = None
    h_ctx = None
    for d in range(2):
        rev = d == 1
        hcd, st = chunk_scan(flip_time(pc[0], rev), flip_time(pc[1], rev), flip_time(pc[2], rev),
                             flip_time(pc[3][d], rev), flip_time(pc[4][d], rev),
                             zero_state(bsz_c), ctx_out)
        hld, _ = chunk_scan(flip_time(pl[0], rev), flip_time(pl[1], rev), flip_time(pl[2], rev),
                            flip_time(pl[3][d], rev), flip_time(pl[4][d], rev), st, True)
        hld = flip_time(hld, rev)
        h_lat = hld if h_lat is None else h_lat + hld
        if ctx_out:
            hcd = flip_time(hcd, rev)
            h_ctx = hcd if h_ctx is None else h_ctx + hcd

    def finish(h, xc, z):
        bsz, nh, t, dh = h.shape
        hf = jnp.transpose(h, (0, 2, 1, 3))
        mu = jnp.mean(hf, axis=-1, keepdims=True)
        var = jnp.mean(jnp.square(hf - mu), axis=-1, keepdims=True)
        hn = ((hf - mu) * lax.rsqrt(var + EPS)).reshape(bsz, t, nh * dh).astype(xc.dtype) * hnorm_w
        return ((hn + skip * xc) * jax.nn.silu(z)) @ w_out

    yl = finish(h_lat, xc_l, z_l)
    yc = finish(h_ctx, xc_c, z_c) if ctx_out else None
    return yl, yc


def pool_mix(u, w_grp):
    bsz, t, e = u.shape
    uf = u.astype(jnp.float32)
    csum = jnp.concatenate([jnp.zeros((bsz, 1, e), jnp.float32), lax.cumsum(uf, axis=1)], axis=1)
    pos = jnp.arange(t)
    outs = []
    for g, w in enumerate(POOL_WINDOWS):
        lo = w // 2
        hi = w - 1 - lo
        a = jnp.clip(pos - lo, 0, t)
        bnd = jnp.clip(pos + hi + 1, 0, t)
        cg = csum[..., g * GROUP_W:(g + 1) * GROUP_W]
        cnt = (bnd - a).astype(jnp.float32)[None, :, None]
        mean = (cg[:, bnd] - cg[:, a]) / cnt
        dlt = (mean - uf[..., g * GROUP_W:(g + 1) * GROUP_W]).astype(u.dtype)
        outs.append(dlt @ w_grp[g])
    return jnp.concatenate(outs, axis=-1)


def pool_branch(hl, hc, w_in, w_grp, scale, w_out, ctx_out):
    def run(h):
        u, z = jnp.split(h @ w_in, 2, axis=-1)
        return (pool_mix(u, w_grp) * scale * jax.nn.silu(z)) @ w_out
    return run(hl), (run(hc) if ctx_out else None)


def fourier_mix(u, w_grp):
    bsz, t, e = u.shape
    ug = u.astype(jnp.float32).reshape(bsz, t, N_GROUPS, GROUP_W)
    f = jnp.real(jnp.fft.fft2(ug, axes=(1, 3), norm='ortho')).astype(u.dtype)
    return jnp.einsum('btgi,gio->btgo', f, w_grp).reshape(bsz, t, e)


def fourier_branch(hl, hc, w_in, w_grp, w_out, ctx_out):
    def run(h):
        u, z = jnp.split(h @ w_in, 2, axis=-1)
        return (fourier_mix(u, w_grp) * jax.nn.silu(z)) @ w_out
    return run(hl), (run(hc) if ctx_out else None)


def setup_inputs(seed: int = 0) -> dict:
    key = jax.random.key(seed)
    ks = jax.random.split(key, 32)
    d = D_MODEL
    e = D_INNER
    nh = MLSTM_HEADS
    n_a = len(range(0, DEPTH, N_MIXERS))
    n_b = len(range(1, DEPTH, N_MIXERS))
    n_c = len(range(2, DEPTH, N_MIXERS))

    def nrm(k, shape, s):
        return jax.random.normal(k, shape, jnp.float32) * s

    f_bias = jnp.linspace(F_BIAS_LO, F_BIAS_HI, nh, dtype=jnp.float32)[None, None, :]
    return {
        'x': nrm(ks[0], (BATCH, SEQ, d), 1.0),
        'c': nrm(ks[1], (BATCH, d), 1.0),
        'ctx': nrm(ks[2], (BATCH, CTX_LEN, d), 1.0),
        'c_ctx': nrm(ks[3], (d,), 1.0),
        'ada_w': nrm(ks[4], (DEPTH, d, 3 * d), 0.5 * d ** -0.5),
        'ada_b': nrm(ks[5], (DEPTH, 3 * d), 0.02),
        'norm_g': 1.0 + nrm(ks[6], (DEPTH, d), 0.02),
        'final_g': 1.0 + nrm(ks[7], (d,), 0.02),
        'a_w_in': nrm(ks[8], (n_a, d, 2 * e), d ** -0.5),
        'a_conv_w': nrm(ks[9], (n_a, CONV_W, e), CONV_W ** -0.5),
        'a_conv_b': nrm(ks[10], (n_a, e), 0.02),
        'a_wq': nrm(ks[11], (n_a, e // QKV_BLOCK, QKV_BLOCK, QKV_BLOCK), QKV_BLOCK ** -0.5),
        'a_wk': nrm(ks[12], (n_a, e // QKV_BLOCK, QKV_BLOCK, QKV_BLOCK), QKV_BLOCK ** -0.5),
        'a_wv': nrm(ks[13], (n_a, e // QKV_BLOCK, QKV_BLOCK, QKV_BLOCK), QKV_BLOCK ** -0.5),
        'a_w_ig': nrm(ks[14], (n_a, 2, 3 * e, nh), (3 * e) ** -0.5),
        'a_b_ig': nrm(ks[15], (n_a, 2, nh), 0.1),
        'a_w_fg': nrm(ks[16], (n_a, 2, 3 * e, nh), (3 * e) ** -0.5),
        'a_b_fg': f_bias + nrm(ks[17], (n_a, 2, nh), 0.1),
        'a_hnorm_w': 1.0 + nrm(ks[18], (n_a, e), 0.02),
        'a_skip': 1.0 + nrm(ks[19], (n_a, e), 0.02),
        'a_w_out': nrm(ks[20], (n_a, e, d), e ** -0.5),
        'b_w_in': nrm(ks[21], (n_b, d, 2 * e), d ** -0.5),
        'b_w_grp': nrm(ks[22], (n_b, N_GROUPS, GROUP_W, GROUP_W), GROUP_W ** -0.5),
        'b_scale': 1.0 + nrm(ks[23], (n_b, e), 0.02),
        'b_w_out': nrm(ks[24], (n_b, e, d), e ** -0.5),
        'c_w_in': nrm(ks[25], (n_c, d, 2 * e), d ** -0.5),
        'c_w_grp': nrm(ks[26], (n_c, N_GROUPS, GROUP_W, GROUP_W), GROUP_W ** -0.5),
        'c_w_out': nrm(ks[27], (n_c, e, d), e ** -0.5),
    }


def reference(x, c, ctx, c_ctx, ada_w, ada_b, norm_g, final_g,
              a_w_in, a_conv_w, a_conv_b, a_wq, a_wk, a_wv, a_w_ig, a_b_ig, a_w_fg, a_b_fg,
              a_hnorm_w, a_skip, a_w_out,
              b_w_in, b_w_grp, b_scale, b_w_out,
              c_w_in, c_w_grp, c_w_out):
    x = x + pos_embed_2d(x.shape[1], x.shape[2]).astype(x.dtype)[None]
    cx = ctx
    s_lat = jax.nn.silu(c)
    s_ctx = jax.nn.silu(c_ctx)[None]
    for i in range(DEPTH):
        kind = i % N_MIXERS
        j = i // N_MIXERS
        ctx_out = i < DEPTH - 1
        shift, scale, gate = jnp.split((s_lat @ ada_w[i] + ada_b[i])[:, None, :], 3, axis=-1)
        cshift, cscale, cgate = jnp.split((s_ctx @ ada_w[i] + ada_b[i])[:, None, :], 3, axis=-1)
        hl = rmsnorm(x, norm_g[i]) * (1.0 + scale) + shift
        hc = rmsnorm(cx, norm_g[i]) * (1.0 + cscale) + cshift
        if kind == 0:
            yl, yc = mlstm_branch(hl, hc, a_w_in[j], a_conv_w[j], a_conv_b[j], a_wq[j], a_wk[j], a_wv[j],
                                  a_w_ig[j], a_b_ig[j], a_w_fg[j], a_b_fg[j], a_hnorm_w[j], a_skip[j],
                                  a_w_out[j], ctx_out)
        elif kind == 1:
            yl, yc = pool_branch(hl, hc, b_w_in[j], b_w_grp[j], b_scale[j], b_w_out[j], ctx_out)
        else:
            yl, yc = fourier_branch(hl, hc, c_w_in[j], c_w_grp[j], c_w_out[j], ctx_out)
        x = x + gate * yl
        if ctx_out:
            cx = cx + cgate * yc
    return rmsnorm(x, final_g)
```

```python
import math
from contextlib import ExitStack

import numpy as np
import ml_dtypes
import concourse.bass as bass
import concourse.mybir as mybir
from concourse.bass_utils import run_bass_kernel_spmd

F32 = mybir.dt.float32
BF16 = mybir.dt.bfloat16
AF = mybir.ActivationFunctionType
ALU = mybir.AluOpType
AX = mybir.AxisListType
NEG = -30000.0
EPS = 1e-6
POOL_WINDOWS = (2, 4, 8, 16)

_uid = [0]


def uid(p):
    _uid[0] += 1
    return f"{p}_{_uid[0]}"


class Seq:
    def __init__(self, nc, limit=20000, tag="seq"):
        self.nc = nc
        self.n = 0
        self.limit = limit
        self.tag = tag
        self._new_sem()
        self.prev = None

    def _new_sem(self):
        self.sem = self.nc.alloc_semaphore(f"{self.tag}{self.n}")
        self.n += 1
        self.val = 0

    def _pre(self, eng):
        if self.prev is not None:
            eng.wait_ge(self.prev[0], self.prev[1])
        if self.val > self.limit:
            self._new_sem()

    def dmas(self, eng, items):
        if eng is self.nc.gpsimd and len(items) > 1:
            for kw in items:
                self.dmas(eng, [kw])
            return
        self._pre(eng)
        for kw in items:
            eng.dma_start(**kw).then_inc(self.sem, 16)
            self.val += 16
        self.prev = (self.sem, self.val)

    def group(self, eng, fn):
        self._pre(eng)
        last = fn()
        last.then_inc(self.sem, 1)
        self.val += 1
        self.prev = (self.sem, self.val)


def load_eng(nc, src_dtype, dst_dtype):
    return nc.sync if src_dtype == dst_dtype else nc.gpsimd


def mm(nc, seq, out_hbm, pairs, *, scale=None, func=None, accumulate=False, nt=512, mb=None, fp32=False):
    M, N = out_hbm.shape
    func = func or AF.Identity
    cdt = F32 if fp32 else BF16
    esz = 4 if fp32 else 2
    Ks = [l.shape[0] for l, r in pairs]
    kcs = [max(1, K // 128) for K in Ks]
    kps = [min(K, 128) for K in Ks]
    tot_kc = sum(kcs)
    if mb is None:
        mb = 1024
        while tot_kc * mb * esz > 64 * 1024 and mb > 128:
            mb //= 2
    mb = min(mb, M)
    nt = min(nt, N)
    while tot_kc * nt * esz > 48 * 1024 and nt > 128:
        nt //= 2
    mcb = max(1, (mb + 127) // 128)
    assert mcb <= 8
    with ExitStack() as es:
        wsb = es.enter_context(nc.sbuf_tensor(uid("mm_w"), [128, tot_kc, mb], cdt))
        xsb = es.enter_context(nc.sbuf_tensor(uid("mm_x"), [128, tot_kc, nt], cdt))
        osb = es.enter_context(nc.sbuf_tensor(uid("mm_o"), [128, mcb, nt], out_hbm.dtype))
        ps = [es.enter_context(nc.psum_tensor(uid("mm_ps"), [128, 512], F32)) for _ in range(mcb)]
        for m0 in range(0, M, mb):
            mbs = min(mb, M - m0)
            items = []
            off = 0
            for (l, r), K, kc, kp in zip(pairs, Ks, kcs, kps):
                src = l[:, m0:m0 + mbs]
                if kc > 1:
                    src = src.rearrange("(kc p) m -> p kc m", p=128)
                    dst = wsb[:, off:off + kc, :mbs]
                else:
                    dst = wsb[:kp, off, :mbs]
                items.append(dict(out=dst, in_=src))
                off += kc
            seq.dmas(load_eng(nc, pairs[0][0].dtype, cdt), items)
            for n0 in range(0, N, nt):
                ns = min(nt, N - n0)
                items = []
                off = 0
                for (l, r), K, kc, kp in zip(pairs, Ks, kcs, kps):
                    src = r[:, n0:n0 + ns]
                    if kc > 1:
                        src = src.rearrange("(kc p) n -> p kc n", p=128)
                        dst = xsb[:, off:off + kc, :ns]
                    else:
                        dst = xsb[:kp, off, :ns]
                    items.append(dict(out=dst, in_=src))
                    off += kc
                seq.dmas(load_eng(nc, pairs[0][1].dtype, cdt), items)
                nmc = (mbs + 127) // 128
                steps = []
                off = 0
                for kc, kp in zip(kcs, kps):
                    for k in range(kc):
                        steps.append((off + k, kp))
                    off += kc

                def pe():
                    last = None
                    for mi in range(nmc):
                        ms = min(128, mbs - mi * 128)
                        for si, (kk, kp) in enumerate(steps):
                            last = nc.tensor.matmul(ps[mi][:ms, :ns], wsb[:kp, kk, mi * 128:mi * 128 + ms],
                                                    xsb[:kp, kk, :ns], start=(si == 0), stop=(si == len(steps) - 1))
                    return last

                seq.group(nc.tensor, pe)

                def ev():
                    last = None
                    for mi in range(nmc):
                        ms = min(128, mbs - mi * 128)
                        gm = (m0 // 128) + mi
                        sc = 1.0 if scale is None else scale(gm, n0)
                        if isinstance(sc, float):
                            last = nc.scalar.activation(out=osb[:ms, mi, :ns], in_=ps[mi][:ms, :ns], func=func, scale=sc)
                        else:
                            last = nc.scalar.activation(out=osb[:ms, mi, :ns], in_=ps[mi][:ms, :ns], func=func,
                                                        scale=sc[:ms])
                    return last

                seq.group(nc.scalar, ev)
                dst = out_hbm[m0:m0 + mbs, n0:n0 + ns]
                if nmc > 1:
                    dst = dst.rearrange("(mc p) n -> p mc n", p=128)
                    srcs = osb[:, :nmc, :ns]
                else:
                    srcs = osb[:mbs, 0, :ns]
                if accumulate:
                    seq.dmas(nc.gpsimd, [dict(out=dst, in_=srcs, accum_op=ALU.add)])
                else:
                    seq.dmas(nc.sync, [dict(out=dst, in_=srcs)])


class Pipe:
    def __init__(self, nc):
        self.sW = nc.alloc_semaphore("pW")
        self.vW = 0
        self.sW2 = nc.alloc_semaphore("pW2")
        self.vW2 = 0
        self.sL = [nc.alloc_semaphore(f"pL{i}") for i in range(2)]
        self.vL = [0, 0]
        self.sPE = nc.alloc_semaphore("pPE")
        self.vPE = 0
        self.sEV = nc.alloc_semaphore("pEV")
        self.vEV = 0
        self.sST = [nc.alloc_semaphore(f"pST{i}") for i in range(2)]
        self.vST = [0, 0]


def mm2(nc, seq, pipe, out_hbm, pairs, *, scale=None, func=None, accumulate=False, nt=512):
    M, N = out_hbm.shape
    func = func or AF.Identity
    cdt = BF16
    Ks = [l.shape[0] for l, r in pairs]
    kcs = [max(1, K // 128) for K in Ks]
    kps = [min(K, 128) for K in Ks]
    tot_kc = sum(kcs)
    mb = 1024
    while tot_kc * mb * 2 > 64 * 1024 and mb > 128:
        mb //= 2
    mb = min(mb, M)
    nt = min(nt, N)
    while tot_kc * nt * 2 > 32 * 1024 and nt > 128:
        nt //= 2
    mcb = max(1, (mb + 127) // 128)
    hb = 4 if mcb > 4 else mcb
    eW = load_eng(nc, pairs[0][0].dtype, cdt)
    eL = load_eng(nc, pairs[0][1].dtype, cdt)
    eS = nc.gpsimd if (accumulate or (eL is nc.sync and eW is nc.sync)) else nc.sync
    if accumulate:
        assert eS is nc.gpsimd
    start_tok = seq.prev
    steps_kk = []
    off = 0
    for kc, kp in zip(kcs, kps):
        for k in range(kc):
            steps_kk.append((off + k, kp))
        off += kc
    with ExitStack() as es:
        wsb = es.enter_context(nc.sbuf_tensor(uid("m2w"), [128, tot_kc, mb], cdt))
        xsb = [es.enter_context(nc.sbuf_tensor(uid("m2x"), [128, tot_kc, nt], cdt)) for _ in range(2)]
        osb = [es.enter_context(nc.sbuf_tensor(uid("m2o"), [128, mcb, nt], out_hbm.dtype)) for _ in range(2)]
        psets = [[es.enter_context(nc.psum_tensor(uid("m2p"), [128, 512], F32)) for _ in range(hb)] for _ in range(2)]
        blocks = [(m0, min(mb, M - m0)) for m0 in range(0, M, mb)]
        ntiles = [(n0, min(nt, N - n0)) for n0 in range(0, N, nt)]
        steps = [(bi, ni) for bi in range(len(blocks)) for ni in range(len(ntiles))]
        pe_after_step = {}
        st_after_step = {}
        set_free = [pipe.vEV, pipe.vEV]
        w_ready = None

        def chained_dmas(eng, items, sem, val):
            for kw in items:
                eng.dma_start(**kw).then_inc(sem, 16)
                val += 16
                if eng is nc.gpsimd and len(items) > 1:
                    eng.wait_ge(sem, val)
            return val

        def emit_L(si):
            bi, ni = steps[si]
            n0, ns = ntiles[ni]
            p = si % 2
            if si >= 2:
                eL.wait_ge(pipe.sPE, pe_after_step[si - 2])
            elif start_tok is not None:
                eL.wait_ge(start_tok[0], start_tok[1])
            items = []
            off = 0
            for (l, r), kc, kp in zip(pairs, kcs, kps):
                src = r[:, n0:n0 + ns]
                if kc > 1:
                    src = src.rearrange("(kc p) n -> p kc n", p=128)
                    dst = xsb[p][:, off:off + kc, :ns]
                else:
                    dst = xsb[p][:kp, off, :ns]
                items.append(dict(out=dst, in_=src))
                off += kc
            pipe.vL[p] = chained_dmas(eL, items, pipe.sL[p], pipe.vL[p])
            return pipe.vL[p]

        l_val = {}
        for si in range(min(2, len(steps))):
            if si == 0 or steps[si][0] == 0 or True:
                l_val[si] = None
        def emit_W(bi, half, wait_tok):
            m0, mbs = blocks[bi]
            if half is None:
                c_lo, c_hi = 0, mbs
            elif half == 0:
                c_lo, c_hi = 0, min(512, mbs)
            else:
                c_lo, c_hi = 512, mbs
            if wait_tok is not None:
                eW.wait_ge(wait_tok[0], wait_tok[1])
            items = []
            off = 0
            for (l, r), kc, kp in zip(pairs, kcs, kps):
                src = l[:, m0 + c_lo:m0 + c_hi]
                if kc > 1:
                    src = src.rearrange("(kc p) m -> p kc m", p=128)
                    dst = wsb[:, off:off + kc, c_lo:c_hi]
                else:
                    dst = wsb[:kp, off, c_lo:c_hi]
                items.append(dict(out=dst, in_=src))
                off += kc
            if half == 1:
                pipe.vW2 = chained_dmas(eW, items, pipe.sW2, pipe.vW2)
            else:
                pipe.vW = chained_dmas(eW, items, pipe.sW, pipe.vW)

        def split_block(bi):
            return blocks[bi][1] > 512

        w_ready = [None, None]
        nxt_ready = [None, None]
        last_step_of_block = {}
        for si_, (bi_, ni_) in enumerate(steps):
            last_step_of_block[bi_] = si_
        cur_block = -1
        for si, (bi, ni) in enumerate(steps):
            m0, mbs = blocks[bi]
            n0, ns = ntiles[ni]
            p = si % 2
            nmc = (mbs + 127) // 128
            if bi != cur_block:
                cur_block = bi
                if si == 0:
                    if split_block(bi):
                        emit_W(bi, 0, start_tok)
                        w_ready[0] = (pipe.sW, pipe.vW)
                        emit_W(bi, 1, None)
                        w_ready[1] = (pipe.sW2, pipe.vW2)
                    else:
                        emit_W(bi, None, start_tok)
                        w_ready[0] = w_ready[1] = (pipe.sW, pipe.vW)
                elif not _CTX.get("w_prefetched", False):
                    emit_W(bi, None, (pipe.sPE, pe_after_step[si - 1]))
                    w_ready[0] = w_ready[1] = (pipe.sW, pipe.vW)
                else:
                    w_ready[0], w_ready[1] = nxt_ready[0], nxt_ready[1]
                _CTX["w_prefetched"] = False
            if si == 0:
                l_val[0] = emit_L(0)
                if len(steps) > 1:
                    l_val[1] = emit_L(1)
            halves = [(0, 0, min(4, nmc)), (1, 4, nmc)] if nmc > 4 else [(si % 2, 0, nmc)]
            ev_last = None
            for (st_i, c0, c1) in halves:
                nc.tensor.wait_ge(pipe.sL[p], l_val[si])
                wr = w_ready[0 if c0 == 0 else 1]
                nc.tensor.wait_ge(wr[0], wr[1])
                if len(halves) == 1 and w_ready[1] is not w_ready[0]:
                    nc.tensor.wait_ge(w_ready[1][0], w_ready[1][1])
                nc.tensor.wait_ge(pipe.sEV, set_free[st_i])
                last = None
                for mi in range(c0, c1):
                    ms = min(128, mbs - mi * 128)
                    for k_i, (kk, kp) in enumerate(steps_kk):
                        last = nc.tensor.matmul(psets[st_i][mi - c0][:ms, :ns], wsb[:kp, kk, mi * 128:mi * 128 + ms],
                                                xsb[p][:kp, kk, :ns], start=(k_i == 0), stop=(k_i == len(steps_kk) - 1))
                last.then_inc(pipe.sPE, 1)
                pipe.vPE += 1
                pe_val = pipe.vPE
                if (si == last_step_of_block[bi] and bi + 1 < len(blocks) and len(halves) == 2 and split_block(bi + 1)):
                    hsel = 0 if c0 == 0 else 1
                    emit_W(bi + 1, hsel, (pipe.sPE, pe_val))
                    nxt_ready[hsel] = (pipe.sW, pipe.vW) if hsel == 0 else (pipe.sW2, pipe.vW2)
                    if hsel == 1:
                        _CTX["w_prefetched"] = True
                nc.scalar.wait_ge(pipe.sPE, pe_val)
                if si >= 2 and c0 == 0:
                    pp, vv = st_after_step[si - 2]
                    nc.scalar.wait_ge(pipe.sST[pp], vv)
                last = None
                for mi in range(c0, c1):
                    ms = min(128, mbs - mi * 128)
                    gm = (m0 // 128) + mi
                    sc = 1.0 if scale is None else scale(gm, n0)
                    if isinstance(sc, float):
                        last = nc.scalar.activation(out=osb[p][:ms, mi, :ns], in_=psets[st_i][mi - c0][:ms, :ns], func=func, scale=sc)
                    else:
                        last = nc.scalar.activation(out=osb[p][:ms, mi, :ns], in_=psets[st_i][mi - c0][:ms, :ns], func=func,
                                                    scale=sc[:ms])
                last.then_inc(pipe.sEV, 1)
                pipe.vEV += 1
                set_free[st_i] = pipe.vEV
                ev_last = pipe.vEV
            pe_after_step[si] = pipe.vPE
            if si + 2 < len(steps):
                l_val[si + 2] = emit_L(si + 2)
            eS.wait_ge(pipe.sEV, ev_last)
            dst = out_hbm[m0:m0 + mbs, n0:n0 + ns]
            if nmc > 1:
                dst = dst.rearrange("(mc p) n -> p mc n", p=128)
                srcs = osb[p][:, :nmc, :ns]
            else:
                srcs = osb[p][:mbs, 0, :ns]
            if accumulate:
                eS.dma_start(out=dst, in_=srcs, accum_op=ALU.add).then_inc(pipe.sST[p], 16)
            else:
                eS.dma_start(out=dst, in_=srcs).then_inc(pipe.sST[p], 16)
            pipe.vST[p] += 16
            st_after_step[si] = (p, pipe.vST[p])
        nc.vector.wait_ge(pipe.sST[0], pipe.vST[0])
        nc.vector.wait_ge(pipe.sST[1], pipe.vST[1])
        jt = es.enter_context(nc.sbuf_tensor(uid("m2j"), [128, 1], F32))
        seq.prev = None
        seq.group(nc.vector, lambda: nc.vector.memset(jt[:], 0.0))


_CTX = {}


def mmp(nc, seq, out_hbm, pairs, **kw):
    return mm2(nc, seq, _CTX["pipe"], out_hbm, pairs, **kw)


def lanes(nc, seq, make_bufs, items, body, nl=2):
    T0 = seq.prev
    lseqs = _CTX["lane_seqs"][:nl]
    with ExitStack() as es:
        bufs = [make_bufs(es, l) for l in range(nl)]
        T1 = seq.prev
        for ls in lseqs:
            ls.prev = T1
        pending = list(items)
        gens = [None] * nl
        while True:
            progressed = False
            for l in range(nl):
                if gens[l] is None and pending:
                    gens[l] = body(lseqs[l], bufs[l], pending.pop(0))
                if gens[l] is not None:
                    progressed = True
                    try:
                        next(gens[l])
                    except StopIteration:
                        gens[l] = None
            if not progressed:
                break
        for ls in lseqs:
            if ls.prev is not None:
                nc.vector.wait_ge(ls.prev[0], ls.prev[1])
        jt = es.enter_context(nc.sbuf_tensor(uid("lj"), [128, 1], F32))
        seq.prev = None
        seq.group(nc.vector, lambda: nc.vector.memset(jt[:], 0.0))


class Cfg:
    def __init__(self, D=4096, T=4096, TC=256, depth=4):
        self.D, self.T, self.TC, self.depth = D, T, TC, depth
        self.N = T + TC
        self.E = 2 * D
        self.NH = 8
        self.DH = self.E // 8
        self.GW = self.E // 4
        self.DC = D // 128
        self.EC = self.E // 128
        self.n_a = len(range(0, depth, 3))
        self.n_b = len(range(1, depth, 3))
        self.n_c = len(range(2, depth, 3))


def host_consts(cfg):
    T, TC, D, GW, N = cfg.T, cfg.TC, cfg.D, cfg.GW, cfg.N
    bf = ml_dtypes.bfloat16
    c = {}

    def dft(n):
        k = np.arange(n, dtype=np.float64)
        ang = 2.0 * np.pi * ((k[:, None] * k[None, :]) % n) / n
        return np.cos(ang) / math.sqrt(n), np.sin(ang) / math.sqrt(n)

    cc, sc = dft(GW)
    c["k_cc"], c["k_sc"] = cc.astype(bf), sc.astype(bf)
    ct, st = dft(T)
    c["k_ct"], c["k_nst"] = ct.astype(bf), (-st).astype(bf)
    ctc, stc = dft(TC)
    c["k_ctc"], c["k_nstc"] = ctc.astype(bf), (-stc).astype(bf)
    inv = np.zeros((4, N), np.float32)
    for g, w in enumerate(POOL_WINDOWS):
        lo = w // 2
        hi = w - 1 - lo
        for (o, n) in ((0, T), (T, TC)):
            pos = np.arange(n)
            a = np.clip(pos - lo, 0, n)
            b = np.clip(pos + hi + 1, 0, n)
            inv[g, o:o + n] = 1.0 / (b - a)
    c["k_invc"] = np.ascontiguousarray(np.broadcast_to(inv[:, None, :], (4, 128, N))).astype(np.float32)
    s = np.arange(128)[:, None]
    t = np.arange(256)[None, :]
    m = np.zeros((2, 2, 128, 256), np.float32)
    for half in range(2):
        m[0, half] = np.where(s + 128 * half <= t, 0.0, NEG)
        m[1, half] = np.where(s + 128 * half >= t, 0.0, NEG)
    c["k_mask"] = np.ascontiguousarray(m.reshape(4, 128, 256).transpose(1, 0, 2))
    c["k_ident"] = np.eye(128, dtype=np.float32)
    sel = np.zeros((8, 8, 128), np.float32)
    for n in range(8):
        sel[n, n, :] = 1.0
    c["k_sel"] = sel
    rows = T // 64
    r = np.repeat(np.arange(rows, dtype=np.float32), 64)
    col = np.tile(np.arange(64, dtype=np.float32), rows)
    quarter = D // 4
    omega = (1.0 / (np.float32(10000.0) ** (np.arange(quarter, dtype=np.float32) / np.float32(quarter)))).astype(np.float32)

    def axis_emb(p):
        a = (p[:, None] * omega[None, :]).astype(np.float32)
        return np.concatenate([np.sin(a), np.cos(a)], axis=-1)

    pe = np.concatenate([axis_emb(r), axis_emb(col)], axis=-1).astype(np.float32)
    c["k_posT"] = np.ascontiguousarray(pe.T)
    return c


WEIGHT_NAMES = ["ada_w", "ada_b", "norm_g", "final_g",
                "a_w_in", "a_conv_w", "a_conv_b", "a_wq", "a_wk", "a_wv", "a_w_ig", "a_b_ig", "a_w_fg", "a_b_fg",
                "a_hnorm_w", "a_skip", "a_w_out", "b_w_in", "b_w_grp", "b_scale", "b_w_out",
                "c_w_in", "c_w_grp", "c_w_out"]


def build(cfg, shapes, const_shapes, debug=()):
    D, T, TC, N, E, NH, DH, GW, DC, EC = cfg.D, cfg.T, cfg.TC, cfg.N, cfg.E, cfg.NH, cfg.DH, cfg.GW, cfg.DC, cfg.EC
    nc = bass.Bass("TRN2", target_bir_lowering=False)
    seq = Seq(nc)
    _CTX["pipe"] = Pipe(nc)
    _CTX["att"] = None
    _CTX["lane_seqs"] = [Seq(nc, limit=120000, tag=f"lane{l}_") for l in range(2)]
    I = {}
    for name, (shp, dt) in shapes.items():
        I[name] = nc.dram_tensor(name, list(shp), dt, kind="ExternalInput").ap()
    for name, (shp, dt) in const_shapes.items():
        I[name] = nc.dram_tensor(name, list(shp), dt, kind="ExternalInput").ap()
    yT = nc.dram_tensor("yT", [D, T], F32, kind="ExternalOutput").ap()

    def scratch(name, shape, dt):
        kind = "ExternalOutput" if name in debug else "Internal"
        return nc.dram_tensor(name, list(shape), dt, kind=kind)

    X_h = scratch("X", [D, N], F32)
    X = X_h.ap()
    H = scratch("H", [D, N], BF16).ap()
    U = scratch("U", [E, N], F32).ap()
    Z = scratch("Z", [E, N], F32).ap()
    G = scratch("G", [E, N], BF16).ap()
    P = scratch("P", [E, N], F32).ap()
    XC = scratch("XC", [E, N], F32).ap()
    DL = scratch("DL", [E, N], BF16).ap()
    QT = scratch("QT", [E, N], BF16).ap()
    KT = scratch("KT", [E, N], BF16).ap()
    VT = scratch("VT", [E, N], BF16).ap()
    VTOK = scratch("VTOK", [N, E], BF16).ap()
    FA = scratch("FA", [N, E], BF16).ap()
    FB = scratch("FB", [N, E], BF16).ap()
    HD = [scratch(f"HD{z}", [N, E], F32).ap() for z in range(2)]
    BD_h = scratch("BD", [3 * EC * 128, 128], F32)
    BD = BD_h.ap()
    WG = scratch("WG", [3 * E, 32], F32).ap()
    GP = scratch("GP", [32, N], F32).ap()
    ST = scratch("ST", [D, 2], F32).ap()
    BZ = scratch("BZ", [2, 8, N], F32).ap()
    LMB = scratch("LMB", [2, 8, N], F32).ap()
    MODT = scratch("MODT", [3 * D, 2], F32).ap()

    with ExitStack() as top:
        top.enter_context(nc.allow_non_contiguous_dma(reason="small per-channel parameter vectors"))

        def sb(name, shape, dt=F32):
            return top.enter_context(nc.sbuf_tensor(uid(name), shape, dt))

        ident = sb("ident", [128, 128])
        ones_f = sb("ones_f", [128, 128])
        ones_b = sb("ones_b", [128, 8], BF16)
        eps_t = sb("eps_t", [128, 1])
        one_t = sb("one_t", [128, 1])
        selT = sb("selT", [8, 8, 128])
        normA = [sb(f"normA{i}", [128, DC, 2]) for i in range(cfg.depth)]
        normB = [sb(f"normB{i}", [128, DC, 2]) for i in range(cfg.depth)]
        gateS = [sb(f"gateS{i}", [128, DC, 2]) for i in range(cfg.depth)]
        finA = sb("finA", [128, DC])
        zeroB = sb("zeroB", [128, DC])

        def g0():
            nc.vector.memset(ones_f[:], 1.0)
            nc.vector.memset(ones_b[:], 1.0)
            nc.vector.memset(eps_t[:], EPS)
            nc.vector.memset(zeroB[:], 0.0)
            return nc.vector.memset(one_t[:], 1.0)

        seq.group(nc.vector, g0)
        seq.dmas(nc.sync, [dict(out=ident[:], in_=I["k_ident"]), dict(out=selT[:], in_=I["k_sel"]),
                           dict(out=finA[:], in_=I["final_g"].rearrange("(c p) -> p c", p=128))])

        with ExitStack() as es:
            xt = es.enter_context(nc.sbuf_tensor(uid("xi"), [128, N], F32))
            pt = es.enter_context(nc.sbuf_tensor(uid("pi"), [128, T], F32))
            for dc in range(DC):
                rs = slice(dc * 128, (dc + 1) * 128)
                seq.dmas(nc.sync, [dict(out=xt[:], in_=I["xT"][rs, :]), dict(out=pt[:], in_=I["k_posT"][rs, :])])
                seq.group(nc.vector, lambda: nc.vector.tensor_tensor(out=xt[:, :T], in0=xt[:, :T], in1=pt[:], op=ALU.add))
                seq.dmas(nc.sync, [dict(out=X[rs, :], in_=xt[:])])

        with ExitStack() as es:
            cs = es.enter_context(nc.sbuf_tensor(uid("cs"), [128, DC, 2], F32))
            md = es.enter_context(nc.sbuf_tensor(uid("md"), [128, 3 * DC, 2], F32))
            ab = es.enter_context(nc.sbuf_tensor(uid("ab"), [128, 3 * DC], F32))
            gg = es.enter_context(nc.sbuf_tensor(uid("gg"), [128, DC], F32))
            t1 = es.enter_context(nc.sbuf_tensor(uid("t1"), [128, DC, 2], F32))
            seq.dmas(nc.sync, [dict(out=cs[:], in_=I["ccT"].rearrange("(c p) r -> p c r", p=128))])
            seq.group(nc.scalar, lambda: nc.scalar.activation(out=cs[:], in_=cs[:], func=AF.Silu))
            seq.dmas(nc.sync, [dict(out=ST.rearrange("(c p) r -> p c r", p=128), in_=cs[:])])
            for i in range(cfg.depth):
                mmp(nc, seq, MODT, [(I["ada_w"][i], ST)])
                seq.dmas(nc.sync, [dict(out=md[:], in_=MODT.rearrange("(c p) r -> p c r", p=128)),
                                   dict(out=ab[:], in_=I["ada_b"][i].rearrange("(c p) -> p c", p=128)),
                                   dict(out=gg[:], in_=I["norm_g"][i].rearrange("(c p) -> p c", p=128))])

                def f1():
                    last = None
                    for r in range(2):
                        last = nc.vector.tensor_tensor(out=md[:, :, r], in0=md[:, :, r], in1=ab[:], op=ALU.add)
                    return last

                seq.group(nc.vector, f1)

                def f2():
                    nc.vector.tensor_copy(out=normB[i][:], in_=md[:, 0:DC, :])
                    nc.vector.tensor_copy(out=gateS[i][:], in_=md[:, 2 * DC:3 * DC, :])
                    return nc.vector.tensor_scalar(out=t1[:], in0=md[:, DC:2 * DC, :], scalar1=1.0, scalar2=None, op0=ALU.add)

                seq.group(nc.vector, f2)

                def f3():
                    last = None
                    for r in range(2):
                        last = nc.vector.tensor_tensor(out=normA[i][:, :, r], in0=t1[:, :, r], in1=gg[:], op=ALU.mult)
                    return last

                seq.group(nc.vector, f3)

        def norm_stage(A_of, B_of, out_hbm, out_dt, ncols):
            NT = 128

            def mk(es, l):
                return dict(xt=es.enter_context(nc.sbuf_tensor(uid("nx"), [128, DC, NT], F32)),
                            sq=es.enter_context(nc.sbuf_tensor(uid("nsq"), [128, DC, NT], F32)),
                            ho=es.enter_context(nc.sbuf_tensor(uid("nh"), [128, DC, NT], out_dt)),
                            rs=es.enter_context(nc.sbuf_tensor(uid("nrs"), [128, NT], F32)),
                            pss=es.enter_context(nc.psum_tensor(uid("nps"), [128, 512], F32)))

            def body(ls, Bf, n0):
                xt, sq, ho, rs, pss = Bf["xt"], Bf["sq"], Bf["ho"], Bf["rs"], Bf["pss"]
                ns = min(NT, ncols - n0)
                r = 0 if n0 < T else 1
                ls.dmas(nc.sync, [dict(out=xt[:, :, :ns], in_=X[:, n0:n0 + ns].rearrange("(c p) n -> p c n", p=128))])
                yield
                ls.group(nc.scalar, lambda: nc.scalar.activation(out=sq[:, :, :ns], in_=xt[:, :, :ns], func=AF.Square))
                yield

                def pe():
                    last = None
                    for c in range(DC):
                        last = nc.tensor.matmul(pss[:, :ns], ones_f[:], sq[:, c, :ns], start=(c == 0), stop=(c == DC - 1))
                    return last

                ls.group(nc.tensor, pe)
                yield
                ls.group(nc.scalar, lambda: nc.scalar.activation(out=rs[:, :ns], in_=pss[:, :ns], func=AF.Sqrt,
                                                                 bias=eps_t[:], scale=1.0 / D))
                yield
                ls.group(nc.vector, lambda: nc.vector.reciprocal(out=rs[:, :ns], in_=rs[:, :ns]))
                yield

                def f1():
                    last = None
                    for c in range(DC):
                        last = nc.vector.tensor_tensor(out=sq[:, c, :ns], in0=xt[:, c, :ns], in1=rs[:, :ns], op=ALU.mult)
                    return last

                ls.group(nc.vector, f1)
                yield

                def f2():
                    last = None
                    for c in range(DC):
                        last = nc.scalar.activation(out=ho[:, c, :ns], in_=sq[:, c, :ns], func=AF.Identity,
                                                    scale=A_of(r)[:, c:c + 1], bias=B_of(r)[:, c:c + 1])
                    return last

                ls.group(nc.scalar, f2)
                yield
                ls.dmas(nc.sync, [dict(out=out_hbm[:, n0:n0 + ns].rearrange("(c p) n -> p c n", p=128), in_=ho[:, :, :ns])])
                yield

            lanes(nc, seq, mk, list(range(0, ncols, NT)), body)

        def gate_stage(scale_hbm):
            with ExitStack() as es0:
                scs = es0.enter_context(nc.sbuf_tensor(uid("gs"), [128, EC], F32))
                if scale_hbm is not None:
                    seq.dmas(nc.sync, [dict(out=scs[:], in_=scale_hbm.rearrange("(c p) -> p c", p=128))])
                else:
                    seq.group(nc.vector, lambda: nc.vector.memset(scs[:], 1.0))

                def mk(es, l):
                    return dict(pt=es.enter_context(nc.sbuf_tensor(uid("gp"), [128, N], F32)),
                                zt=es.enter_context(nc.sbuf_tensor(uid("gz"), [128, N], F32)),
                                go=es.enter_context(nc.sbuf_tensor(uid("go"), [128, N], BF16)))

                def body(ls, Bf, ec):
                    pt_, zt, go = Bf["pt"], Bf["zt"], Bf["go"]
                    rs_ = slice(ec * 128, (ec + 1) * 128)
                    ls.dmas(nc.sync, [dict(out=pt_[:], in_=P[rs_, :]), dict(out=zt[:], in_=Z[rs_, :])])
                    yield
                    ls.group(nc.scalar, lambda: nc.scalar.activation(out=zt[:], in_=zt[:], func=AF.Silu))
                    yield
                    ls.group(nc.vector, lambda: nc.vector.scalar_tensor_tensor(out=go[:], in0=pt_[:], scalar=scs[:, ec:ec + 1],
                                                                               in1=zt[:], op0=ALU.mult, op1=ALU.mult))
                    yield
                    ls.dmas(nc.sync, [dict(out=G[rs_, :], in_=go[:])])
                    yield

                lanes(nc, seq, mk, list(range(EC)), body)

        def wout_stage(i, w_out):
            def sc(gm, n0):
                return gateS[i][:, gm, (0 if n0 < T else 1):(1 if n0 < T else 2)]
            mmp(nc, seq, X, [(w_out, G)], scale=sc, accumulate=True)

        for i in range(cfg.depth):
            kind, j = i % 3, i // 3
            norm_stage(lambda r: normA[i][:, :, r], lambda r: normB[i][:, :, r], H, BF16, N)
            w_in = (I["a_w_in"], I["b_w_in"], I["c_w_in"])[kind][j]
            mmp(nc, seq, U, [(w_in[:, 0:E], H)])
            mmp(nc, seq, Z, [(w_in[:, E:2 * E], H)])
            if kind == 1:
                pool_layer(nc, seq, cfg, I, j, U, DL, P)
                gate_stage(I["b_scale"][j])
                wout_stage(i, I["b_w_out"][j])
            elif kind == 2:
                fourier_layer(nc, seq, cfg, I, j, U, FA, FB, DL, P)
                gate_stage(None)
                wout_stage(i, I["c_w_out"][j])
            else:
                mlstm_layer(nc, seq, cfg, I, j, dict(UZ=U, Z=Z, XC=XC, QT=QT, KT=KT, VT=VT, VTOK=VTOK, HD=HD, BD=BD, BD_h=BD_h,
                                                     WG=WG, GP=GP, HN=P, G=G, ident=ident, ones_b=ones_b, eps_t=eps_t,
                                                     one_t=one_t, selT=selT, BZ=BZ, LMB=LMB))
                wout_stage(i, I["a_w_out"][j])

        norm_stage(lambda r: finA[:], lambda r: zeroB[:], yT, F32, T)
        nc.sync.wait_ge(seq.prev[0], seq.prev[1])
    nc._n_seq_sems = seq.n
    return nc


def pool_layer(nc, seq, cfg, I, j, UZ, DL, P):
    T, TC, N, E, GW, EC = cfg.T, cfg.TC, cfg.N, cfg.E, cfg.GW, cfg.EC
    PAD = 16
    segs = ((0, T), (T, TC))
    with ExitStack() as es0:
        invc = es0.enter_context(nc.sbuf_tensor(uid("pinv"), [128, N], F32))
        for g in range(4):
            w = POOL_WINDOWS[g]
            lo = w // 2
            hi = w - 1 - lo
            seq.dmas(nc.sync, [dict(out=invc[:], in_=I["k_invc"][g])])

            def mk(es, l):
                bufs = []
                for si, (o, n) in enumerate(segs):
                    bufs.append(tuple(es.enter_context(nc.sbuf_tensor(uid("pb"), [128, n + 2 * PAD], F32)) for _ in range(3)))
                do = es.enter_context(nc.sbuf_tensor(uid("pdo"), [128, N], BF16))

                def z0():
                    last = None
                    for tri in bufs:
                        for t_ in tri:
                            last = nc.vector.memset(t_[:], 0.0)
                    return last

                seq.group(nc.vector, z0)
                return dict(bufs=bufs, do=do)

            def body(ls, Bf, ec, w=w, hi=hi):
                bufs, do = Bf["bufs"], Bf["do"]
                rs_ = slice(ec * 128, (ec + 1) * 128)
                ls.dmas(nc.sync, [dict(out=bufs[si][0][:, PAD:PAD + n], in_=UZ[rs_, o:o + n]) for si, (o, n) in enumerate(segs)])
                yield
                cov = 1
                cur = 0
                while cov < w:
                    nxt = 1 if cur != 1 else 2

                    def stp(cur=cur, nxt=nxt, cov=cov):
                        last = None
                        for si, (o, n) in enumerate(segs):
                            L = n + 2 * PAD
                            last = nc.vector.tensor_tensor(out=bufs[si][nxt][:, cov:L], in0=bufs[si][cur][:, cov:L],
                                                           in1=bufs[si][cur][:, 0:L - cov], op=ALU.add)
                        return last

                    ls.group(nc.vector, stp)
                    yield
                    cur = nxt
                    cov *= 2
                tmpi = 1 if cur != 1 else 2

                def m1(cur=cur, tmpi=tmpi):
                    last = None
                    for si, (o, n) in enumerate(segs):
                        last = nc.vector.tensor_tensor(out=bufs[si][tmpi][:, PAD:PAD + n], in0=bufs[si][cur][:, PAD + hi:PAD + hi + n],
                                                       in1=invc[:, o:o + n], op=ALU.mult)
                    return last

                ls.group(nc.vector, m1)
                yield

                def m2(tmpi=tmpi):
                    last = None
                    for si, (o, n) in enumerate(segs):
                        last = nc.vector.tensor_tensor(out=do[:, o:o + n], in0=bufs[si][tmpi][:, PAD:PAD + n],
                                                       in1=bufs[si][0][:, PAD:PAD + n], op=ALU.subtract)
                    return last

                ls.group(nc.vector, m2)
                yield
                ls.dmas(nc.sync, [dict(out=DL[rs_, :], in_=do[:])])
                yield

            cpg = GW // 128
            lanes(nc, seq, mk, list(range(g * cpg, (g + 1) * cpg)), body)
    for g in range(4):
        mmp(nc, seq, P[g * GW:(g + 1) * GW, :], [(I["b_w_grp"][j, g], DL[g * GW:(g + 1) * GW, :])])


def fourier_layer(nc, seq, cfg, I, j, UZ, FA, FB, DL, P):
    T, TC, N, E, GW = cfg.T, cfg.TC, cfg.N, cfg.E, cfg.GW
    for g in range(4):
        gs = slice(g * GW, (g + 1) * GW)
        mmp(nc, seq, FA[:, gs], [(UZ[gs, :], I["k_cc"])])
        mmp(nc, seq, FB[:, gs], [(UZ[gs, :], I["k_sc"])])
    mmp(nc, seq, DL[:, 0:T], [(FA[0:T, :], I["k_ct"]), (FB[0:T, :], I["k_nst"])])
    mmp(nc, seq, DL[:, T:N], [(FA[T:N, :], I["k_ctc"]), (FB[T:N, :], I["k_nstc"])])
    for g in range(4):
        gs = slice(g * GW, (g + 1) * GW)
        mmp(nc, seq, P[gs, :], [(I["c_w_grp"][j, g], DL[gs, :])])


def mlstm_layer(nc, seq, cfg, I, j, S):
    T, TC, N, E, NH, DH, EC = cfg.T, cfg.TC, cfg.N, cfg.E, cfg.NH, cfg.DH, cfg.EC
    UZ, XC, QT, KT, VT, VTOK, HD, BD, WG, GP, HN, G = (S[k] for k in ("UZ", "XC", "QT", "KT", "VT", "VTOK", "HD", "BD", "WG", "GP", "HN", "G"))
    Z = S["Z"]
    ident, ones_b, eps_t, one_t, selT = S["ident"], S["ones_b"], S["eps_t"], S["one_t"], S["selT"]
    segs = ((0, T), (T, TC))
    NTC = N // 128
    DCH = DH // 128

    with ExitStack() as es0:
        cw = es0.enter_context(nc.sbuf_tensor(uid("cw"), [128, 4, EC], F32))
        cb = es0.enter_context(nc.sbuf_tensor(uid("cb"), [128, EC], F32))
        seq.dmas(nc.sync, [dict(out=cw[:, t_, :], in_=I["a_conv_w"][j, t_].rearrange("(c p) -> p c", p=128)) for t_ in range(4)]
                 + [dict(out=cb[:], in_=I["a_conv_b"][j].rearrange("(c p) -> p c", p=128))])

        def mk(es, l):
            ub = [es.enter_context(nc.sbuf_tensor(uid("cu"), [128, n + 3], F32)) for (o, n) in segs]
            acc = [es.enter_context(nc.sbuf_tensor(uid("ca"), [128, N], F32)) for _ in range(2)]

            def z0():
                nc.vector.memset(ub[0][:], 0.0)
                return nc.vector.memset(ub[1][:], 0.0)

            seq.group(nc.vector, z0)
            return dict(ub=ub, acc=acc)

        def body(ls, Bf, ec):
            ub, acc = Bf["ub"], Bf["acc"]
            rs_ = slice(ec * 128, (ec + 1) * 128)
            ls.dmas(nc.sync, [dict(out=ub[si][:, 1:1 + n], in_=UZ[rs_, o:o + n]) for si, (o, n) in enumerate(segs)])
            yield
            for t_ in range(4):
                src, dst = acc[(t_ + 1) % 2], acc[t_ % 2]

                def stp(t_=t_, src=src, dst=dst):
                    last = None
                    for si, (o, n) in enumerate(segs):
                        if t_ == 0:
                            last = nc.vector.tensor_scalar(out=dst[:, o:o + n], in0=ub[si][:, 0:n], scalar1=cw[:, 0, ec:ec + 1],
                                                           scalar2=None, op0=ALU.mult)
                        else:
                            last = nc.vector.scalar_tensor_tensor(out=dst[:, o:o + n], in0=ub[si][:, t_:t_ + n],
                                                                  scalar=cw[:, t_, ec:ec + 1], in1=src[:, o:o + n],
                                                                  op0=ALU.mult, op1=ALU.add)
                    return last

                ls.group(nc.vector, stp)
                yield
            ls.group(nc.scalar, lambda: nc.scalar.activation(out=acc[0][:], in_=acc[1][:], func=AF.Silu, bias=cb[:, ec:ec + 1], scale=1.0))
            yield
            ls.dmas(nc.sync, [dict(out=XC[rs_, :], in_=acc[0][:])])
            yield

        lanes(nc, seq, mk, list(range(EC)), body)

    with ExitStack() as es:
        zt = es.enter_context(nc.sbuf_tensor(uid("bz"), [128, EC, 128], F32))
        seq.group(nc.vector, lambda: nc.vector.memset(zt[:], 0.0))
        seq.dmas(nc.sync, [dict(out=BD[m * EC * 128:(m + 1) * EC * 128, :].rearrange("(c p) q -> p c q", p=128), in_=zt[:]) for m in range(3)])
        items = []
        for m, wname in enumerate(("a_wq", "a_wk", "a_wv")):
            w = I[wname]
            for c in range(EC):
                dst = bass.AP(S["BD_h"], (m * EC + c) * 128 * 128, [[516, 32], [128, 4], [1, 4]])
                items.append(dict(out=dst, in_=w[j, 32 * c:32 * (c + 1), :, :]))
        for k0 in range(0, len(items), 32):
            seq.dmas(nc.sync, items[k0:k0 + 32])
    if True:
        NT = 512
        tiles = [(n0, min(NT, N - n0)) for n0 in range(0, N, NT)]
        jobs = [(m, n0, ns) for m in range(3) for (n0, ns) in tiles]

        def mk(es, l):
            return dict(xcb=es.enter_context(nc.sbuf_tensor(uid("qx"), [128, N], BF16)),
                        ubf=es.enter_context(nc.sbuf_tensor(uid("qu"), [128, N], BF16)),
                        bdb=es.enter_context(nc.sbuf_tensor(uid("qb"), [128, 3, 128], BF16)),
                        oq=es.enter_context(nc.sbuf_tensor(uid("qo"), [128, 3, N], BF16)),
                        ov=es.enter_context(nc.sbuf_tensor(uid("qv"), [128, NTC, 128], BF16)),
                        ps=[es.enter_context(nc.psum_tensor(uid("qps"), [128, 512], F32)) for _ in range(4)])

        def body(ls, Bf, ec):
            xcb, ubf, bdb, oq, ov, ps = (Bf[k] for k in ("xcb", "ubf", "bdb", "oq", "ov", "ps"))
            rs_ = slice(ec * 128, (ec + 1) * 128)
            ls.dmas(nc.gpsimd, [dict(out=xcb[:], in_=XC[rs_, :]), dict(out=ubf[:], in_=UZ[rs_, :])]
                    + [dict(out=bdb[:, m, :], in_=BD[(m * EC + ec) * 128:(m * EC + ec + 1) * 128, :]) for m in range(3)])
            yield
            for b0 in range(0, len(jobs), 4):
                jb = jobs[b0:b0 + 4]

                def pe(jb=jb):
                    last = None
                    for bi, (m, n0, ns) in enumerate(jb):
                        src = ubf if m == 2 else xcb
                        last = nc.tensor.matmul(ps[bi][:, :ns], bdb[:, m, :], src[:, n0:n0 + ns], start=True, stop=True)
                    return last

                ls.group(nc.tensor, pe)
                yield

                def ev(jb=jb):
                    last = None
                    for bi, (m, n0, ns) in enumerate(jb):
                        last = nc.scalar.copy(out=oq[:, m, n0:n0 + ns], in_=ps[bi][:, :ns])
                    return last

                ls.group(nc.scalar, ev)
                yield
            ls.dmas(nc.sync, [dict(out=QT[rs_, :], in_=oq[:, 0, :]), dict(out=KT[rs_, :], in_=oq[:, 1, :]), dict(out=VT[rs_, :], in_=oq[:, 2, :])])
            yield
            for b0 in range(0, NTC, 16):
                tcs = list(range(b0, min(NTC, b0 + 16)))

                def pe2(tcs=tcs):
                    last = None
                    for bi, tc in enumerate(tcs):
                        last = nc.tensor.matmul(ps[bi // 4][:, (bi % 4) * 128:(bi % 4 + 1) * 128], ubf[:, tc * 128:(tc + 1) * 128],
                                                bdb[:, 2, :], start=True, stop=True)
                    return last

                ls.group(nc.tensor, pe2)
                yield

                def ev2(tcs=tcs):
                    last = None
                    for bi, tc in enumerate(tcs):
                        last = nc.scalar.copy(out=ov[:, tc, :], in_=ps[bi // 4][:, (bi % 4) * 128:(bi % 4 + 1) * 128])
                    return last

                ls.group(nc.scalar, ev2)
                yield
            ls.dmas(nc.sync, [dict(out=VTOK[:, rs_].rearrange("(tc p) e -> p tc e", p=128), in_=ov[:])])
            yield

        lanes(nc, seq, mk, list(range(EC)), body)

    items = []
    for gi, (wn, z) in enumerate((("a_w_ig", 0), ("a_w_ig", 1), ("a_w_fg", 0), ("a_w_fg", 1))):
        for r0 in range(0, 3 * E, 2048):
            r1 = min(3 * E, r0 + 2048)
            items.append(dict(out=WG[r0:r1, gi * 8:(gi + 1) * 8], in_=I[wn][j, z, r0:r1, :]))
    for k0 in range(0, len(items), 16):
        seq.dmas(nc.sync, items[k0:k0 + 16])
    mmp(nc, seq, GP, [(WG[0:E, :], QT), (WG[E:2 * E, :], KT), (WG[2 * E:3 * E, :], VT)])

    with ExitStack() as es:
        def t8(name):
            return es.enter_context(nc.sbuf_tensor(uid(name), [8, N], F32))
        Bz = [t8("Bz0"), t8("Bz1")]
        LmB = [t8("Lm0"), t8("Lm1")]
        li = [t8("li0"), t8("li1")]
        xf = [t8("xf0"), t8("xf1")]
        tmp = t8("tmp")
        onesr = t8("onesr")
        bi_ = es.enter_context(nc.sbuf_tensor(uid("bi"), [8, 2], F32))
        bf_ = es.enter_context(nc.sbuf_tensor(uid("bf"), [8, 2], F32))
        tot = es.enter_context(nc.sbuf_tensor(uid("tot"), [8, 4], F32))
        seq.dmas(nc.sync, [dict(out=li[z][:], in_=GP[8 * z:8 * z + 8, :]) for z in range(2)]
                 + [dict(out=xf[z][:], in_=GP[16 + 8 * z:24 + 8 * z, :]) for z in range(2)]
                 + [dict(out=bi_[:, z:z + 1], in_=I["a_b_ig"][j, z].rearrange("(n o) -> n o", o=1)) for z in range(2)]
                 + [dict(out=bf_[:, z:z + 1], in_=I["a_b_fg"][j, z].rearrange("(n o) -> n o", o=1)) for z in range(2)])

        def g1():
            nc.vector.memset(onesr[:], 1.0)
            return nc.vector.tensor_scalar(out=bf_[:], in0=bf_[:], scalar1=-1.0, scalar2=None, op0=ALU.mult)

        seq.group(nc.vector, g1)

        def g2():
            last = None
            for z in range(2):
                nc.scalar.activation(out=li[z][:], in_=li[z][:], func=AF.Identity, bias=bi_[:, z:z + 1], scale=1.0)
                last = nc.scalar.activation(out=xf[z][:], in_=xf[z][:], func=AF.Exp, bias=bf_[:, z:z + 1], scale=-1.0)
            return last

        seq.group(nc.scalar, g2)

        def g3():
            last = None
            for z in range(2):
                last = nc.scalar.activation(out=xf[z][:], in_=xf[z][:], func=AF.Ln, bias=S["one_t"][:8, :], scale=1.0)
            return last

        seq.group(nc.scalar, g3)
        def g4():
            last = None
            for z in range(2):
                for (o, n) in segs:
                    last = nc.vector.tensor_tensor_scan(out=Bz[z][:, o:o + n], data0=onesr[:, o:o + n], data1=xf[z][:, o:o + n],
                                                        initial=0.0, op0=ALU.mult, op1=ALU.add)
            return last

        seq.group(nc.vector, g4)
        def g5():
            nc.vector.tensor_copy(out=tot[:, 0:1], in_=Bz[0][:, N - 1:N])
            nc.vector.tensor_copy(out=tot[:, 1:2], in_=Bz[1][:, N - 1:N])
            nc.vector.tensor_copy(out=tot[:, 2:3], in_=Bz[1][:, T - 1:T])
            return nc.vector.tensor_tensor(out=tmp[:], in0=xf[1][:], in1=Bz[1][:], op=ALU.subtract)

        seq.group(nc.vector, g5)

        def g6():
            nc.vector.tensor_tensor(out=tot[:, 3:4], in0=tot[:, 2:3], in1=tot[:, 1:2], op=ALU.add)
            return nc.vector.tensor_scalar(out=Bz[0][:, 0:T], in0=Bz[0][:, 0:T], scalar1=tot[:, 0:1], scalar2=None, op0=ALU.add)

        seq.group(nc.vector, g6)

        def g7():
            nc.vector.tensor_scalar(out=Bz[1][:, 0:T], in0=tmp[:, 0:T], scalar1=tot[:, 3:4], scalar2=None, op0=ALU.add)
            nc.vector.tensor_scalar(out=Bz[1][:, T:N], in0=tmp[:, T:N], scalar1=tot[:, 1:2], scalar2=None, op0=ALU.add)
            return nc.vector.tensor_scalar(out=Bz[0][:], in0=Bz[0][:], scalar1=-1.0, scalar2=None, op0=ALU.mult)

        seq.group(nc.vector, g7)
        seq.group(nc.vector, lambda: nc.vector.tensor_scalar(out=Bz[1][:], in0=Bz[1][:], scalar1=-1.0, scalar2=None, op0=ALU.mult))

        def g8():
            last = None
            for z in range(2):
                last = nc.vector.scalar_tensor_tensor(out=LmB[z][:], in0=li[z][:], scalar=-0.5 * math.log(DH), in1=Bz[z][:],
                                                      op0=ALU.add, op1=ALU.subtract)
            return last

        seq.group(nc.vector, g8)
        seq.dmas(nc.sync, [dict(out=S["BZ"][z], in_=Bz[z][:]) for z in range(2)] + [dict(out=S["LMB"][z], in_=LmB[z][:]) for z in range(2)])

    if True:
        QW = 256
        TS = QW // 128
        VW = min(DH, 512)
        VH = DH // VW
        KTL = T // 128
        with ExitStack() as e5:
            kT = e5.enter_context(nc.sbuf_tensor(uid("akT"), [128, DCH, N], BF16))
            vk = e5.enter_context(nc.sbuf_tensor(uid("avk"), [128, NTC, DH], BF16))
            qT = e5.enter_context(nc.sbuf_tensor(uid("aqT"), [128, DCH, QW], BF16))
            csb = e5.enter_context(nc.sbuf_tensor(uid("acs"), [128, NTC], F32))
            lmb = e5.enter_context(nc.sbuf_tensor(uid("almb"), [8, N], F32))
            bzt = e5.enter_context(nc.sbuf_tensor(uid("abzt"), [8, QW], F32))
            brow = e5.enter_context(nc.sbuf_tensor(uid("abr"), [128, 3, QW], F32))
            mask = e5.enter_context(nc.sbuf_tensor(uid("amk"), [128, 4, 256], F32))
            esb = e5.enter_context(nc.sbuf_tensor(uid("aes"), [128, 4, QW], F32))
            wsb = e5.enter_context(nc.sbuf_tensor(uid("aws"), [128, 4, QW], BF16))
            rsb = e5.enter_context(nc.sbuf_tensor(uid("ars"), [128, TS], F32))
            hsb = e5.enter_context(nc.sbuf_tensor(uid("ahs"), [128, TS, DH], F32))
            ps_num = [[e5.enter_context(nc.psum_tensor(uid("apn"), [128, 512], F32)) for _ in range(VH)] for _ in range(TS)]
            ps_den = e5.enter_context(nc.psum_tensor(uid("apd"), [128, 512], F32))
            ps_s = [e5.enter_context(nc.psum_tensor(uid("aps"), [128, 512], F32)) for _ in range(2)]
            ps_t = e5.enter_context(nc.psum_tensor(uid("apt"), [128, 512], F32))
            seq.dmas(nc.sync, [dict(out=mask[:], in_=I["k_mask"])])
            A = _CTX.setdefault("att", None) or dict(sS=nc.alloc_semaphore("aS"), sE=nc.alloc_semaphore("aE"), sV=nc.alloc_semaphore("aV"),
                                                     sN=nc.alloc_semaphore("aN"), vS=0, vE=0, vV=0, vN=0, gb=0)
            _CTX["att"] = A
            for n in range(NH):
                hs_ = slice(n * DH, (n + 1) * DH)
                seq.dmas(nc.sync, [dict(out=kT[:], in_=KT[hs_, :].rearrange("(c p) t -> p c t", p=128)),
                                   dict(out=vk[:], in_=VTOK[:, hs_].rearrange("(tc p) v -> p tc v", p=128))])
                for z in range(2):
                    seq.dmas(nc.sync, [dict(out=lmb[:], in_=S["LMB"][z])])

                    def pc():
                        last = None
                        for kt in range(NTC):
                            last = nc.tensor.matmul(ps_t[:, kt:kt + 1], lmb[:, kt * 128:(kt + 1) * 128], ident[:8, n:n + 1],
                                                    start=True, stop=True)
                        return last

                    seq.group(nc.tensor, pc)
                    seq.group(nc.vector, lambda: nc.vector.tensor_copy(out=csb[:], in_=ps_t[:, :NTC]))
                    qtiles = [(q0, True) for q0 in range(0, T, QW)] + [(T, False)]
                    for (q0, is_lat) in qtiles:
                        qi = q0 // QW if is_lat else 0
                        if is_lat:
                            full = [KTL + c for c in range(TC // 128)]
                            full += list(range(0, 2 * qi)) if z == 0 else list(range(2 * qi + 2, KTL))
                            keys = [(kt, 0) for kt in full] + [(2 * qi, 1), (2 * qi + 1, 2)]
                        else:
                            keys = [(KTL, 1), (KTL + 1, 2)]
                        seq.dmas(nc.sync, [dict(out=qT[:], in_=QT[hs_, q0:q0 + QW].rearrange("(c p) t -> p c t", p=128)),
                                           dict(out=bzt[:], in_=S["BZ"][z][:, q0:q0 + QW])])
                        seq.group(nc.tensor, lambda: nc.tensor.matmul(ps_t[:, :QW], selT[:, n, :], bzt[:], start=True, stop=True))

                        def gb():
                            nc.vector.tensor_copy(out=brow[:, 0, :], in_=ps_t[:, :QW])
                            nc.vector.tensor_tensor(out=brow[:, 1, :], in0=ps_t[:, :QW], in1=mask[:, 2 * z, :], op=ALU.add)
                            return nc.vector.tensor_tensor(out=brow[:, 2, :], in0=ps_t[:, :QW], in1=mask[:, 2 * z + 1, :], op=ALU.add)

                        seq.group(nc.vector, gb)
                        nk = len(keys)
                        T0 = seq.prev
                        batches = [keys[b0:b0 + 2] for b0 in range(0, nk, 2)]
                        nb = len(batches)
                        sval, eval_, vval, nval = {}, {}, {}, {}

                        def emit_E(b):
                            par = (A["gb"] + b) % 2
                            if b == 0:
                                nc.scalar.wait_ge(T0[0], T0[1])
                            if b >= 2:
                                nc.scalar.wait_ge(A["sV"], vval[b - 2])
                            last = None
                            for bi, (kt, mt) in enumerate(batches[b]):
                                last = nc.scalar.activation(out=esb[:, par * 2 + bi, :], in_=brow[:, mt, :], func=AF.Exp,
                                                            bias=csb[:, kt:kt + 1], scale=1.0)
                            last.then_inc(A["sE"], 1)
                            A["vE"] += 1
                            eval_[b] = A["vE"]

                        def emit_S(b):
                            par = (A["gb"] + b) % 2
                            if b == 0:
                                nc.tensor.wait_ge(T0[0], T0[1])
                            if b >= 2:
                                nc.tensor.wait_ge(A["sV"], vval[b - 2])
                            last = None
                            for bi, (kt, mt) in enumerate(batches[b]):
                                o_ = ps_s[par][:, bi * QW:(bi + 1) * QW]
                                for dc in range(DCH):
                                    last = nc.tensor.matmul(o_, kT[:, dc, kt * 128:(kt + 1) * 128], qT[:, dc, :],
                                                            start=(dc == 0), stop=(dc == DCH - 1))
                            last.then_inc(A["sS"], 1)
                            A["vS"] += 1
                            sval[b] = A["vS"]

                        def emit_V(b):
                            par = (A["gb"] + b) % 2
                            nc.vector.wait_ge(A["sS"], sval[b])
                            nc.vector.wait_ge(A["sE"], eval_[b])
                            if b >= 2:
                                nc.vector.wait_ge(A["sN"], nval[b - 2])
                            last = None
                            for bi, (kt, mt) in enumerate(batches[b]):
                                o_ = ps_s[par][:, bi * QW:(bi + 1) * QW]
                                last = nc.vector.tensor_tensor(out=wsb[:, par * 2 + bi, :], in0=o_, in1=esb[:, par * 2 + bi, :], op=ALU.mult)
                            last.then_inc(A["sV"], 1)
                            A["vV"] += 1
                            vval[b] = A["vV"]

                        def emit_N(b):
                            par = (A["gb"] + b) % 2
                            nc.tensor.wait_ge(A["sV"], vval[b])
                            last = None
                            for bi, (kt, mt) in enumerate(batches[b]):
                                first = (b == 0 and bi == 0)
                                lastk = (b == nb - 1 and bi == len(batches[b]) - 1)
                                for ts in range(TS):
                                    lw = wsb[:, par * 2 + bi, ts * 128:(ts + 1) * 128]
                                    for vh in range(VH):
                                        nc.tensor.matmul(ps_num[ts][vh][:, :VW], lw, vk[:, kt, vh * VW:(vh + 1) * VW],
                                                         start=first, stop=lastk)
                                    last = nc.tensor.matmul(ps_den[:, ts:ts + 1], lw, ones_b[:, 0:1], start=(first and ts == 0),
                                                            stop=lastk, skip_group_check=True)
                            last.then_inc(A["sN"], 1)
                            A["vN"] += 1
                            nval[b] = A["vN"]

                        for b in range(nb):
                            emit_E(b)
                            emit_S(b)
                            emit_V(b)
                            if b >= 1:
                                emit_N(b - 1)
                        emit_N(nb - 1)
                        A["gb"] += nb
                        seq.prev = (A["sN"], A["vN"])
                        seq.group(nc.scalar, lambda: nc.scalar.activation(out=rsb[:], in_=ps_den[:, :TS], func=AF.Abs))
                        seq.group(nc.vector, lambda: nc.vector.tensor_scalar(out=rsb[:], in0=rsb[:], scalar1=1.0, scalar2=None,
                                                                             op0=ALU.max))
                        seq.group(nc.vector, lambda: nc.vector.reciprocal(out=rsb[:], in_=rsb[:]))

                        def a2():
                            last = None
                            for ts in range(TS):
                                for vh in range(VH):
                                    last = nc.scalar.activation(out=hsb[:, ts, vh * VW:(vh + 1) * VW], in_=ps_num[ts][vh][:, :VW],
                                                                func=AF.Identity, scale=rsb[:, ts:ts + 1])
                            return last

                        seq.group(nc.scalar, a2)
                        seq.dmas(nc.sync, [dict(out=HD[z][q0:q0 + QW, hs_].rearrange("(ts p) v -> p ts v", p=128), in_=hsb[:])])

    NHH = NH // 2
    ECH = EC // 2

    def mk6(es, l):
        return dict(a=es.enter_context(nc.sbuf_tensor(uid("fa"), [128, NHH, DH], F32)),
                    b=es.enter_context(nc.sbuf_tensor(uid("fb"), [128, NHH, DH], F32)),
                    c=es.enter_context(nc.sbuf_tensor(uid("fc"), [128, NHH, DH], F32)),
                    st=es.enter_context(nc.sbuf_tensor(uid("fs"), [128, 4, NHH], F32)),
                    hT=es.enter_context(nc.sbuf_tensor(uid("fT"), [128, ECH, 128], F32)),
                    ps=[es.enter_context(nc.psum_tensor(uid("fps"), [128, 512], F32)) for _ in range(4)])

    def body6(ls, Bf, item):
        tc, hh = item
        a, b, c, st, hT, ps = (Bf[k] for k in ("a", "b", "c", "st", "hT", "ps"))
        ts_ = slice(tc * 128, (tc + 1) * 128)
        es_ = slice(hh * NHH * DH, (hh + 1) * NHH * DH)
        ls.dmas(nc.sync, [dict(out=a[:], in_=HD[0][ts_, es_].rearrange("p (n v) -> p n v", n=NHH)),
                          dict(out=b[:], in_=HD[1][ts_, es_].rearrange("p (n v) -> p n v", n=NHH))])
        yield
        ls.group(nc.vector, lambda: nc.vector.tensor_tensor(out=a[:], in0=a[:], in1=b[:], op=ALU.add))
        yield
        ls.group(nc.vector, lambda: nc.vector.reduce_sum(out=st[:, 0, :], in_=a[:], axis=AX.X))
        yield
        ls.group(nc.vector, lambda: nc.vector.tensor_scalar(out=st[:, 1, :], in0=st[:, 0, :], scalar1=-1.0 / DH, scalar2=None, op0=ALU.mult))
        yield

        def f1():
            last = None
            for n in range(NHH):
                last = nc.scalar.activation(out=b[:, n, :], in_=a[:, n, :], func=AF.Identity, bias=st[:, 1, n:n + 1], scale=1.0)
            return last

        ls.group(nc.scalar, f1)
        yield
        ls.group(nc.vector, lambda: nc.vector.tensor_tensor(out=c[:], in0=b[:], in1=b[:], op=ALU.mult))
        yield
        ls.group(nc.vector, lambda: nc.vector.reduce_sum(out=st[:, 2, :], in_=c[:], axis=AX.X))
        yield
        ls.group(nc.scalar, lambda: nc.scalar.activation(out=st[:, 3, :], in_=st[:, 2, :], func=AF.Sqrt, bias=eps_t[:], scale=1.0 / DH))
        yield
        ls.group(nc.vector, lambda: nc.vector.reciprocal(out=st[:, 3, :], in_=st[:, 3, :]))
        yield

        def f3():
            last = None
            for n in range(NHH):
                last = nc.vector.tensor_scalar(out=a[:, n, :], in0=b[:, n, :], scalar1=st[:, 3, n:n + 1], scalar2=None, op0=ALU.mult)
            return last

        ls.group(nc.vector, f3)
        yield
        for b0 in range(0, ECH, 16):
            ecs = list(range(b0, min(ECH, b0 + 16)))

            def pe(ecs=ecs):
                last = None
                for bi, ec in enumerate(ecs):
                    n, vc = divmod(ec, DCH)
                    last = nc.tensor.matmul(ps[bi // 4][:, (bi % 4) * 128:(bi % 4 + 1) * 128], a[:, n, vc * 128:(vc + 1) * 128],
                                            ident[:], start=True, stop=True)
                return last

            ls.group(nc.tensor, pe)
            yield

            def ev(ecs=ecs):
                last = None
                for bi, ec in enumerate(ecs):
                    last = nc.scalar.copy(out=hT[:, ec, :], in_=ps[bi // 4][:, (bi % 4) * 128:(bi % 4 + 1) * 128])
                return last

            ls.group(nc.scalar, ev)
            yield
        ls.dmas(nc.sync, [dict(out=HN[hh * ECH * 128:(hh + 1) * ECH * 128, ts_].rearrange("(c p) t -> p c t", p=128), in_=hT[:])])
        yield

    lanes(nc, seq, mk6, [(tc, hh) for tc in range(NTC) for hh in range(2)], body6)

    with ExitStack() as es0:
        hw = es0.enter_context(nc.sbuf_tensor(uid("g5"), [128, EC], F32))
        sk = es0.enter_context(nc.sbuf_tensor(uid("g6"), [128, EC], F32))
        seq.dmas(nc.sync, [dict(out=hw[:], in_=I["a_hnorm_w"][j].rearrange("(c p) -> p c", p=128)),
                           dict(out=sk[:], in_=I["a_skip"][j].rearrange("(c p) -> p c", p=128))])

        def mk7(es, l):
            return dict(ht=es.enter_context(nc.sbuf_tensor(uid("g1"), [128, N], F32)),
                        xt=es.enter_context(nc.sbuf_tensor(uid("g2"), [128, N], F32)),
                        zt=es.enter_context(nc.sbuf_tensor(uid("g3"), [128, N], F32)),
                        go=es.enter_context(nc.sbuf_tensor(uid("g4"), [128, N], BF16)))

        def body7(ls, Bf, ec):
            ht, xt, zt, go = Bf["ht"], Bf["xt"], Bf["zt"], Bf["go"]
            rs_ = slice(ec * 128, (ec + 1) * 128)
            ls.dmas(nc.sync, [dict(out=ht[:], in_=HN[rs_, :]), dict(out=xt[:], in_=XC[rs_, :]), dict(out=zt[:], in_=Z[rs_, :])])
            yield

            def f1():
                nc.scalar.activation(out=ht[:], in_=ht[:], func=AF.Identity, scale=hw[:, ec:ec + 1])
                return nc.scalar.activation(out=zt[:], in_=zt[:], func=AF.Silu)

            ls.group(nc.scalar, f1)
            yield
            ls.group(nc.vector, lambda: nc.vector.scalar_tensor_tensor(out=xt[:], in0=xt[:], scalar=sk[:, ec:ec + 1], in1=ht[:],
                                                                       op0=ALU.mult, op1=ALU.add))
            yield
            ls.group(nc.vector, lambda: nc.vector.tensor_tensor(out=go[:], in0=xt[:], in1=zt[:], op=ALU.mult))
            yield
            ls.dmas(nc.sync, [dict(out=G[rs_, :], in_=go[:])])
            yield

        lanes(nc, seq, mk7, list(range(EC)), body7)


_NP2BIR = {np.dtype(np.float32): F32, np.dtype(ml_dtypes.bfloat16): BF16}


def run(cfg, inputs, debug=(), trace=False):
    consts = host_consts(cfg)
    B = inputs["x"].shape[0]
    weights = {k: np.ascontiguousarray(inputs[k], dtype=np.float32) for k in WEIGHT_NAMES}
    shapes = {k: (v.shape, F32) for k, v in weights.items()}
    shapes["xT"] = ((cfg.D, cfg.N), F32)
    shapes["ccT"] = ((cfg.D, 2), F32)
    const_shapes = {k: (v.shape, _NP2BIR[v.dtype]) for k, v in consts.items()}
    nc = build(cfg, shapes, const_shapes, debug=debug)
    in_maps = []
    for b in range(B):
        m = dict(weights)
        m.update(consts)
        m["xT"] = np.ascontiguousarray(np.concatenate([inputs["x"][b].T, inputs["ctx"][b].T], axis=1), dtype=np.float32)
        m["ccT"] = np.ascontiguousarray(np.stack([inputs["c"][b], inputs["c_ctx"]], axis=1), dtype=np.float32)
        in_maps.append(m)
    res = run_bass_kernel_spmd(nc, in_maps, core_ids=list(range(B)), trace=trace)
    out = np.stack([np.ascontiguousarray(res.results[b]["yT"].T) for b in range(B)], axis=0).astype(np.float32)
    return out, res


def kernel(**inputs):
    cfg = Cfg()
    out, _ = run(cfg, inputs)
    return out
```

```python
import math
from contextlib import ExitStack

import numpy as np
import ml_dtypes
import concourse.bass as bass
import concourse.mybir as mybir
from concourse.bass_utils import run_bass_kernel_spmd

F32 = mybir.dt.float32
BF16 = mybir.dt.bfloat16
AF = mybir.ActivationFunctionType
ALU = mybir.AluOpType
AX = mybir.AxisListType
NEG = -30000.0
EPS = 1e-6
POOL_WINDOWS = (2, 4, 8, 16)

_uid = [0]


def uid(p):
    _uid[0] += 1
    return f"{p}_{_uid[0]}"


class Seq:
    def __init__(self, nc, limit=20000, tag="seq"):
        self.nc = nc
        self.n = 0
        self.limit = limit
        self.tag = tag
        self._new_sem()
        self.prev = None

    def _new_sem(self):
        self.sem = self.nc.alloc_semaphore(f"{self.tag}{self.n}")
        self.n += 1
        self.val = 0

    def _pre(self, eng):
        if self.prev is not None:
            eng.wait_ge(self.prev[0], self.prev[1])
        if self.val > self.limit:
            self._new_sem()

    def dmas(self, eng, items):
        if eng is self.nc.gpsimd and len(items) > 1:
            for kw in items:
                self.dmas(eng, [kw])
            return
        self._pre(eng)
        for kw in items:
            eng.dma_start(**kw).then_inc(self.sem, 16)
            self.val += 16
        self.prev = (self.sem, self.val)

    def group(self, eng, fn):
        self._pre(eng)
        last = fn()
        last.then_inc(self.sem, 1)
        self.val += 1
        self.prev = (self.sem, self.val)


def load_eng(nc, src_dtype, dst_dtype):
    return nc.sync if src_dtype == dst_dtype else nc.gpsimd


def mm(nc, seq, out_hbm, pairs, *, scale=None, func=None, accumulate=False, nt=512, mb=None, fp32=False):
    M, N = out_hbm.shape
    func = func or AF.Identity
    cdt = F32 if fp32 else BF16
    esz = 4 if fp32 else 2
    Ks = [l.shape[0] for l, r in pairs]
    kcs = [max(1, K // 128) for K in Ks]
    kps = [min(K, 128) for K in Ks]
    tot_kc = sum(kcs)
    if mb is None:
        mb = 1024
        while tot_kc * mb * esz > 64 * 1024 and mb > 128:
            mb //= 2
    mb = min(mb, M)
    nt = min(nt, N)
    while tot_kc * nt * esz > 48 * 1024 and nt > 128:
        nt //= 2
    mcb = max(1, (mb + 127) // 128)
    assert mcb <= 8
    with ExitStack() as es:
        wsb = es.enter_context(nc.sbuf_tensor(uid("mm_w"), [128, tot_kc, mb], cdt))
        xsb = es.enter_context(nc.sbuf_tensor(uid("mm_x"), [128, tot_kc, nt], cdt))
        osb = es.enter_context(nc.sbuf_tensor(uid("mm_o"), [128, mcb, nt], out_hbm.dtype))
        ps = [es.enter_context(nc.psum_tensor(uid("mm_ps"), [128, 512], F32)) for _ in range(mcb)]
        for m0 in range(0, M, mb):
            mbs = min(mb, M - m0)
            items = []
            off = 0
            for (l, r), K, kc, kp in zip(pairs, Ks, kcs, kps):
                src = l[:, m0:m0 + mbs]
                if kc > 1:
                    src = src.rearrange("(kc p) m -> p kc m", p=128)
                    dst = wsb[:, off:off + kc, :mbs]
                else:
                    dst = wsb[:kp, off, :mbs]
                items.append(dict(out=dst, in_=src))
                off += kc
            seq.dmas(load_eng(nc, pairs[0][0].dtype, cdt), items)
            for n0 in range(0, N, nt):
                ns = min(nt, N - n0)
                items = []
                off = 0
                for (l, r), K, kc, kp in zip(pairs, Ks, kcs, kps):
                    src = r[:, n0:n0 + ns]
                    if kc > 1:
                        src = src.rearrange("(kc p) n -> p kc n", p=128)
                        dst = xsb[:, off:off + kc, :ns]
                    else:
                        dst = xsb[:kp, off, :ns]
                    items.append(dict(out=dst, in_=src))
                    off += kc
                seq.dmas(load_eng(nc, pairs[0][1].dtype, cdt), items)
                nmc = (mbs + 127) // 128
                steps = []
                off = 0
                for kc, kp in zip(kcs, kps):
                    for k in range(kc):
                        steps.append((off + k, kp))
                    off += kc

                def pe():
                    last = None
                    for mi in range(nmc):
                        ms = min(128, mbs - mi * 128)
                        for si, (kk, kp) in enumerate(steps):
                            last = nc.tensor.matmul(ps[mi][:ms, :ns], wsb[:kp, kk, mi * 128:mi * 128 + ms],
                                                    xsb[:kp, kk, :ns], start=(si == 0), stop=(si == len(steps) - 1))
                    return last

                seq.group(nc.tensor, pe)

                def ev():
                    last = None
                    for mi in range(nmc):
                        ms = min(128, mbs - mi * 128)
                        gm = (m0 // 128) + mi
                        sc = 1.0 if scale is None else scale(gm, n0)
                        if isinstance(sc, float):
                            last = nc.scalar.activation(out=osb[:ms, mi, :ns], in_=ps[mi][:ms, :ns], func=func, scale=sc)
                        else:
                            last = nc.scalar.activation(out=osb[:ms, mi, :ns], in_=ps[mi][:ms, :ns], func=func,
                                                        scale=sc[:ms])
                    return last

                seq.group(nc.scalar, ev)
                dst = out_hbm[m0:m0 + mbs, n0:n0 + ns]
                if nmc > 1:
                    dst = dst.rearrange("(mc p) n -> p mc n", p=128)
                    srcs = osb[:, :nmc, :ns]
                else:
                    srcs = osb[:mbs, 0, :ns]
                if accumulate:
                    seq.dmas(nc.gpsimd, [dict(out=dst, in_=srcs, accum_op=ALU.add)])
                else:
                    seq.dmas(nc.sync, [dict(out=dst, in_=srcs)])


class Pipe:
    def __init__(self, nc):
        self.sW = nc.alloc_semaphore("pW")
        self.vW = 0
        self.sW2 = nc.alloc_semaphore("pW2")
        self.vW2 = 0
        self.sL = [nc.alloc_semaphore(f"pL{i}") for i in range(2)]
        self.vL = [0, 0]
        self.sPE = nc.alloc_semaphore("pPE")
        self.vPE = 0
        self.sEV = nc.alloc_semaphore("pEV")
        self.vEV = 0
        self.sST = [nc.alloc_semaphore(f"pST{i}") for i in range(2)]
        self.vST = [0, 0]


def mm2(nc, seq, pipe, out_hbm, pairs, *, scale=None, func=None, accumulate=False, nt=512):
    M, N = out_hbm.shape
    func = func or AF.Identity
    cdt = BF16
    Ks = [l.shape[0] for l, r in pairs]
    kcs = [max(1, K // 128) for K in Ks]
    kps = [min(K, 128) for K in Ks]
    tot_kc = sum(kcs)
    mb = 1024
    while tot_kc * mb * 2 > 64 * 1024 and mb > 128:
        mb //= 2
    mb = min(mb, M)
    nt = min(nt, N)
    while tot_kc * nt * 2 > 32 * 1024 and nt > 128:
        nt //= 2
    mcb = max(1, (mb + 127) // 128)
    hb = 4 if mcb > 4 else mcb
    eW = load_eng(nc, pairs[0][0].dtype, cdt)
    eL = load_eng(nc, pairs[0][1].dtype, cdt)
    eS = nc.gpsimd if (accumulate or (eL is nc.sync and eW is nc.sync)) else nc.sync
    if accumulate:
        assert eS is nc.gpsimd
    start_tok = seq.prev
    steps_kk = []
    off = 0
    for kc, kp in zip(kcs, kps):
        for k in range(kc):
            steps_kk.append((off + k, kp))
        off += kc
    with ExitStack() as es:
        wsb = es.enter_context(nc.sbuf_tensor(uid("m2w"), [128, tot_kc, mb], cdt))
        xsb = [es.enter_context(nc.sbuf_tensor(uid("m2x"), [128, tot_kc, nt], cdt)) for _ in range(2)]
        osb = [es.enter_context(nc.sbuf_tensor(uid("m2o"), [128, mcb, nt], out_hbm.dtype)) for _ in range(2)]
        psets = [[es.enter_context(nc.psum_tensor(uid("m2p"), [128, 512], F32)) for _ in range(hb)] for _ in range(2)]
        blocks = [(m0, min(mb, M - m0)) for m0 in range(0, M, mb)]
        ntiles = [(n0, min(nt, N - n0)) for n0 in range(0, N, nt)]
        steps = [(bi, ni) for bi in range(len(blocks)) for ni in range(len(ntiles))]
        pe_after_step = {}
        st_after_step = {}
        set_free = [pipe.vEV, pipe.vEV]
        w_ready = None

        def chained_dmas(eng, items, sem, val):
            for kw in items:
                eng.dma_start(**kw).then_inc(sem, 16)
                val += 16
                if eng is nc.gpsimd and len(items) > 1:
                    eng.wait_ge(sem, val)
            return val

        def emit_L(si):
            bi, ni = steps[si]
            n0, ns = ntiles[ni]
            p = si % 2
            if si >= 2:
                eL.wait_ge(pipe.sPE, pe_after_step[si - 2])
            elif start_tok is not None:
                eL.wait_ge(start_tok[0], start_tok[1])
            items = []
            off = 0
            for (l, r), kc, kp in zip(pairs, kcs, kps):
                src = r[:, n0:n0 + ns]
                if kc > 1:
                    src = src.rearrange("(kc p) n -> p kc n", p=128)
                    dst = xsb[p][:, off:off + kc, :ns]
                else:
                    dst = xsb[p][:kp, off, :ns]
                items.append(dict(out=dst, in_=src))
                off += kc
            pipe.vL[p] = chained_dmas(eL, items, pipe.sL[p], pipe.vL[p])
            return pipe.vL[p]

        l_val = {}
        for si in range(min(2, len(steps))):
            if si == 0 or steps[si][0] == 0 or True:
                l_val[si] = None
        def emit_W(bi, half, wait_tok):
            m0, mbs = blocks[bi]
            if half is None:
                c_lo, c_hi = 0, mbs
            elif half == 0:
                c_lo, c_hi = 0, min(512, mbs)
            else:
                c_lo, c_hi = 512, mbs
            if wait_tok is not None:
                eW.wait_ge(wait_tok[0], wait_tok[1])
            items = []
            off = 0
            for (l, r), kc, kp in zip(pairs, kcs, kps):
                src = l[:, m0 + c_lo:m0 + c_hi]
                if kc > 1:
                    src = src.rearrange("(kc p) m -> p kc m", p=128)
                    dst = wsb[:, off:off + kc, c_lo:c_hi]
                else:
                    dst = wsb[:kp, off, c_lo:c_hi]
                items.append(dict(out=dst, in_=src))
                off += kc
            if half == 1:
                pipe.vW2 = chained_dmas(eW, items, pipe.sW2, pipe.vW2)
            else:
                pipe.vW = chained_dmas(eW, items, pipe.sW, pipe.vW)

        def split_block(bi):
            return blocks[bi][1] > 512

        w_ready = [None, None]
        nxt_ready = [None, None]
        last_step_of_block = {}
        for si_, (bi_, ni_) in enumerate(steps):
            last_step_of_block[bi_] = si_
        cur_block = -1
        for si, (bi, ni) in enumerate(steps):
            m0, mbs = blocks[bi]
            n0, ns = ntiles[ni]
            p = si % 2
            nmc = (mbs + 127) // 128
            if bi != cur_block:
                cur_block = bi
                if si == 0:
                    if split_block(bi):
                        emit_W(bi, 0, start_tok)
                        w_ready[0] = (pipe.sW, pipe.vW)
                        emit_W(bi, 1, None)
                        w_ready[1] = (pipe.sW2, pipe.vW2)
                    else:
                        emit_W(bi, None, start_tok)
                        w_ready[0] = w_ready[1] = (pipe.sW, pipe.vW)
                elif not _CTX.get("w_prefetched", False):
                    emit_W(bi, None, (pipe.sPE, pe_after_step[si - 1]))
                    w_ready[0] = w_ready[1] = (pipe.sW, pipe.vW)
                else:
                    w_ready[0], w_ready[1] = nxt_ready[0], nxt_ready[1]
                _CTX["w_prefetched"] = False
            if si == 0:
                l_val[0] = emit_L(0)
                if len(steps) > 1:
                    l_val[1] = emit_L(1)
            halves = [(0, 0, min(4, nmc)), (1, 4, nmc)] if nmc > 4 else [(si % 2, 0, nmc)]
            ev_last = None
            for (st_i, c0, c1) in halves:
                nc.tensor.wait_ge(pipe.sL[p], l_val[si])
                wr = w_ready[0 if c0 == 0 else 1]
                nc.tensor.wait_ge(wr[0], wr[1])
                if len(halves) == 1 and w_ready[1] is not w_ready[0]:
                    nc.tensor.wait_ge(w_ready[1][0], w_ready[1][1])
                nc.tensor.wait_ge(pipe.sEV, set_free[st_i])
                last = None
                for mi in range(c0, c1):
                    ms = min(128, mbs - mi * 128)
                    for k_i, (kk, kp) in enumerate(steps_kk):
                        last = nc.tensor.matmul(psets[st_i][mi - c0][:ms, :ns], wsb[:kp, kk, mi * 128:mi * 128 + ms],
                                                xsb[p][:kp, kk, :ns], start=(k_i == 0), stop=(k_i == len(steps_kk) - 1))
                last.then_inc(pipe.sPE, 1)
                pipe.vPE += 1
                pe_val = pipe.vPE
                if (si == last_step_of_block[bi] and bi + 1 < len(blocks) and len(halves) == 2 and split_block(bi + 1)):
                    hsel = 0 if c0 == 0 else 1
                    emit_W(bi + 1, hsel, (pipe.sPE, pe_val))
                    nxt_ready[hsel] = (pipe.sW, pipe.vW) if hsel == 0 else (pipe.sW2, pipe.vW2)
                    if hsel == 1:
                        _CTX["w_prefetched"] = True
                nc.scalar.wait_ge(pipe.sPE, pe_val)
                if si >= 2 and c0 == 0:
                    pp, vv = st_after_step[si - 2]
                    nc.scalar.wait_ge(pipe.sST[pp], vv)
                last = None
                for mi in range(c0, c1):
                    ms = min(128, mbs - mi * 128)
                    gm = (m0 // 128) + mi
                    sc = 1.0 if scale is None else scale(gm, n0)
                    if isinstance(sc, float):
                        last = nc.scalar.activation(out=osb[p][:ms, mi, :ns], in_=psets[st_i][mi - c0][:ms, :ns], func=func, scale=sc)
                    else:
                        last = nc.scalar.activation(out=osb[p][:ms, mi, :ns], in_=psets[st_i][mi - c0][:ms, :ns], func=func,
                                                    scale=sc[:ms])
                last.then_inc(pipe.sEV, 1)
                pipe.vEV += 1
                set_free[st_i] = pipe.vEV
                ev_last = pipe.vEV
            pe_after_step[si] = pipe.vPE
            if si + 2 < len(steps):
                l_val[si + 2] = emit_L(si + 2)
            eS.wait_ge(pipe.sEV, ev_last)
            dst = out_hbm[m0:m0 + mbs, n0:n0 + ns]
            if nmc > 1:
                dst = dst.rearrange("(mc p) n -> p mc n", p=128)
                srcs = osb[p][:, :nmc, :ns]
            else:
                srcs = osb[p][:mbs, 0, :ns]
            if accumulate:
                eS.dma_start(out=dst, in_=srcs, accum_op=ALU.add).then_inc(pipe.sST[p], 16)
            else:
                eS.dma_start(out=dst, in_=srcs).then_inc(pipe.sST[p], 16)
            pipe.vST[p] += 16
            st_after_step[si] = (p, pipe.vST[p])
        nc.vector.wait_ge(pipe.sST[0], pipe.vST[0])
        nc.vector.wait_ge(pipe.sST[1], pipe.vST[1])
        jt = es.enter_context(nc.sbuf_tensor(uid("m2j"), [128, 1], F32))
        seq.prev = None
        seq.group(nc.vector, lambda: nc.vector.memset(jt[:], 0.0))


_CTX = {}


def mmp(nc, seq, out_hbm, pairs, **kw):
    return mm2(nc, seq, _CTX["pipe"], out_hbm, pairs, **kw)


def lanes(nc, seq, make_bufs, items, body, nl=2):
    T0 = seq.prev
    lseqs = _CTX["lane_seqs"][:nl]
    with ExitStack() as es:
        bufs = [make_bufs(es, l) for l in range(nl)]
        T1 = seq.prev
        for ls in lseqs:
            ls.prev = T1
        pending = list(items)
        gens = [None] * nl
        while True:
            progressed = False
            for l in range(nl):
                if gens[l] is None and pending:
                    gens[l] = body(lseqs[l], bufs[l], pending.pop(0))
                if gens[l] is not None:
                    progressed = True
                    try:
                        next(gens[l])
                    except StopIteration:
                        gens[l] = None
            if not progressed:
                break
        for ls in lseqs:
            if ls.prev is not None:
                nc.vector.wait_ge(ls.prev[0], ls.prev[1])
        jt = es.enter_context(nc.sbuf_tensor(uid("lj"), [128, 1], F32))
        seq.prev = None
        seq.group(nc.vector, lambda: nc.vector.memset(jt[:], 0.0))


class Cfg:
    def __init__(self, D=4096, T=4096, TC=256, depth=4):
        self.D, self.T, self.TC, self.depth = D, T, TC, depth
        self.N = T + TC
        self.E = 2 * D
        self.NH = 8
        self.DH = self.E // 8
        self.GW = self.E // 4
        self.DC = D // 128
        self.EC = self.E // 128
        self.n_a = len(range(0, depth, 3))
        self.n_b = len(range(1, depth, 3))
        self.n_c = len(range(2, depth, 3))


def host_consts(cfg):
    T, TC, D, GW, N = cfg.T, cfg.TC, cfg.D, cfg.GW, cfg.N
    bf = ml_dtypes.bfloat16
    c = {}

    def dft(n):
        k = np.arange(n, dtype=np.float64)
        ang = 2.0 * np.pi * ((k[:, None] * k[None, :]) % n) / n
        return np.cos(ang) / math.sqrt(n), np.sin(ang) / math.sqrt(n)

    cc, sc = dft(GW)
    c["k_cc"], c["k_sc"] = cc.astype(bf), sc.astype(bf)
    ct, st = dft(T)
    c["k_ct"], c["k_nst"] = ct.astype(bf), (-st).astype(bf)
    ctc, stc = dft(TC)
    c["k_ctc"], c["k_nstc"] = ctc.astype(bf), (-stc).astype(bf)
    inv = np.zeros((4, N), np.float32)
    for g, w in enumerate(POOL_WINDOWS):
        lo = w // 2
        hi = w - 1 - lo
        for (o, n) in ((0, T), (T, TC)):
            pos = np.arange(n)
            a = np.clip(pos - lo, 0, n)
            b = np.clip(pos + hi + 1, 0, n)
            inv[g, o:o + n] = 1.0 / (b - a)
    c["k_invc"] = np.ascontiguousarray(np.broadcast_to(inv[:, None, :], (4, 128, N))).astype(np.float32)
    s = np.arange(128)[:, None]
    t = np.arange(256)[None, :]
    m = np.zeros((2, 2, 128, 256), np.float32)
    for half in range(2):
        m[0, half] = np.where(s + 128 * half <= t, 0.0, NEG)
        m[1, half] = np.where(s + 128 * half >= t, 0.0, NEG)
    c["k_mask"] = np.ascontiguousarray(m.reshape(4, 128, 256).transpose(1, 0, 2))
    c["k_ident"] = np.eye(128, dtype=np.float32)
    sel = np.zeros((8, 8, 128), np.float32)
    for n in range(8):
        sel[n, n, :] = 1.0
    c["k_sel"] = sel
    rows = T // 64
    r = np.repeat(np.arange(rows, dtype=np.float32), 64)
    col = np.tile(np.arange(64, dtype=np.float32), rows)
    quarter = D // 4
    omega = (1.0 / (np.float32(10000.0) ** (np.arange(quarter, dtype=np.float32) / np.float32(quarter)))).astype(np.float32)

    def axis_emb(p):
        a = (p[:, None] * omega[None, :]).astype(np.float32)
        return np.concatenate([np.sin(a), np.cos(a)], axis=-1)

    pe = np.concatenate([axis_emb(r), axis_emb(col)], axis=-1).astype(np.float32)
    c["k_posT"] = np.ascontiguousarray(pe.T)
    return c


WEIGHT_NAMES = ["ada_w", "ada_b", "norm_g", "final_g",
                "a_w_in", "a_conv_w", "a_conv_b", "a_wq", "a_wk", "a_wv", "a_w_ig", "a_b_ig", "a_w_fg", "a_b_fg",
                "a_hnorm_w", "a_skip", "a_w_out", "b_w_in", "b_w_grp", "b_scale", "b_w_out",
                "c_w_in", "c_w_grp", "c_w_out"]


def build(cfg, shapes, const_shapes, debug=()):
    D, T, TC, N, E, NH, DH, GW, DC, EC = cfg.D, cfg.T, cfg.TC, cfg.N, cfg.E, cfg.NH, cfg.DH, cfg.GW, cfg.DC, cfg.EC
    nc = bass.Bass("TRN2", target_bir_lowering=False)
    seq = Seq(nc)
    _CTX["pipe"] = Pipe(nc)
    _CTX["att"] = None
    _CTX["lane_seqs"] = [Seq(nc, limit=120000, tag=f"lane{l}_") for l in range(2)]
    I = {}
    for name, (shp, dt) in shapes.items():
        I[name] = nc.dram_tensor(name, list(shp), dt, kind="ExternalInput").ap()
    for name, (shp, dt) in const_shapes.items():
        I[name] = nc.dram_tensor(name, list(shp), dt, kind="ExternalInput").ap()
    yT = nc.dram_tensor("yT", [D, T], F32, kind="ExternalOutput").ap()

    def scratch(name, shape, dt):
        kind = "ExternalOutput" if name in debug else "Internal"
        return nc.dram_tensor(name, list(shape), dt, kind=kind)

    X_h = scratch("X", [D, N], F32)
    X = X_h.ap()
    H = scratch("H", [D, N], BF16).ap()
    U = scratch("U", [E, N], F32).ap()
    Z = scratch("Z", [E, N], F32).ap()
    G = scratch("G", [E, N], BF16).ap()
    P = scratch("P", [E, N], F32).ap()
    XC = scratch("XC", [E, N], F32).ap()
    DL = scratch("DL", [E, N], BF16).ap()
    QT = scratch("QT", [E, N], BF16).ap()
    KT = scratch("KT", [E, N], BF16).ap()
    VT = scratch("VT", [E, N], BF16).ap()
    VTOK = scratch("VTOK", [N, E], BF16).ap()
    FA = scratch("FA", [N, E], BF16).ap()
    FB = scratch("FB", [N, E], BF16).ap()
    HD = [scratch(f"HD{z}", [N, E], F32).ap() for z in range(2)]
    BD_h = scratch("BD", [3 * EC * 128, 128], F32)
    BD = BD_h.ap()
    WG = scratch("WG", [3 * E, 32], F32).ap()
    GP = scratch("GP", [32, N], F32).ap()
    ST = scratch("ST", [D, 2], F32).ap()
    BZ = scratch("BZ", [2, 8, N], F32).ap()
    LMB = scratch("LMB", [2, 8, N], F32).ap()
    MODT = scratch("MODT", [3 * D, 2], F32).ap()

    with ExitStack() as top:
        top.enter_context(nc.allow_non_contiguous_dma(reason="small per-channel parameter vectors"))

        def sb(name, shape, dt=F32):
            return top.enter_context(nc.sbuf_tensor(uid(name), shape, dt))

        ident = sb("ident", [128, 128])
        ones_f = sb("ones_f", [128, 128])
        ones_b = sb("ones_b", [128, 8], BF16)
        eps_t = sb("eps_t", [128, 1])
        one_t = sb("one_t", [128, 1])
        selT = sb("selT", [8, 8, 128])
        normA = [sb(f"normA{i}", [128, DC, 2]) for i in range(cfg.depth)]
        normB = [sb(f"normB{i}", [128, DC, 2]) for i in range(cfg.depth)]
        gateS = [sb(f"gateS{i}", [128, DC, 2]) for i in range(cfg.depth)]
        finA = sb("finA", [128, DC])
        zeroB = sb("zeroB", [128, DC])

        def g0():
            nc.vector.memset(ones_f[:], 1.0)
            nc.vector.memset(ones_b[:], 1.0)
            nc.vector.memset(eps_t[:], EPS)
            nc.vector.memset(zeroB[:], 0.0)
            return nc.vector.memset(one_t[:], 1.0)

        seq.group(nc.vector, g0)
        seq.dmas(nc.sync, [dict(out=ident[:], in_=I["k_ident"]), dict(out=selT[:], in_=I["k_sel"]),
                           dict(out=finA[:], in_=I["final_g"].rearrange("(c p) -> p c", p=128))])

        with ExitStack() as es:
            xt = es.enter_context(nc.sbuf_tensor(uid("xi"), [128, N], F32))
            pt = es.enter_context(nc.sbuf_tensor(uid("pi"), [128, T], F32))
            for dc in range(DC):
                rs = slice(dc * 128, (dc + 1) * 128)
                seq.dmas(nc.sync, [dict(out=xt[:], in_=I["xT"][rs, :]), dict(out=pt[:], in_=I["k_posT"][rs, :])])
                seq.group(nc.vector, lambda: nc.vector.tensor_tensor(out=xt[:, :T], in0=xt[:, :T], in1=pt[:], op=ALU.add))
                seq.dmas(nc.sync, [dict(out=X[rs, :], in_=xt[:])])

        with ExitStack() as es:
            cs = es.enter_context(nc.sbuf_tensor(uid("cs"), [128, DC, 2], F32))
            md = es.enter_context(nc.sbuf_tensor(uid("md"), [128, 3 * DC, 2], F32))
            ab = es.enter_context(nc.sbuf_tensor(uid("ab"), [128, 3 * DC], F32))
            gg = es.enter_context(nc.sbuf_tensor(uid("gg"), [128, DC], F32))
            t1 = es.enter_context(nc.sbuf_tensor(uid("t1"), [128, DC, 2], F32))
            seq.dmas(nc.sync, [dict(out=cs[:], in_=I["ccT"].rearrange("(c p) r -> p c r", p=128))])
            seq.group(nc.scalar, lambda: nc.scalar.activation(out=cs[:], in_=cs[:], func=AF.Silu))
            seq.dmas(nc.sync, [dict(out=ST.rearrange("(c p) r -> p c r", p=128), in_=cs[:])])
            for i in range(cfg.depth):
                mmp(nc, seq, MODT, [(I["ada_w"][i], ST)])
                seq.dmas(nc.sync, [dict(out=md[:], in_=MODT.rearrange("(c p) r -> p c r", p=128)),
                                   dict(out=ab[:], in_=I["ada_b"][i].rearrange("(c p) -> p c", p=128)),
                                   dict(out=gg[:], in_=I["norm_g"][i].rearrange("(c p) -> p c", p=128))])

                def f1():
                    last = None
                    for r in range(2):
                        last = nc.vector.tensor_tensor(out=md[:, :, r], in0=md[:, :, r], in1=ab[:], op=ALU.add)
                    return last

                seq.group(nc.vector, f1)

                def f2():
                    nc.vector.tensor_copy(out=normB[i][:], in_=md[:, 0:DC, :])
                    nc.vector.tensor_copy(out=gateS[i][:], in_=md[:, 2 * DC:3 * DC, :])
                    return nc.vector.tensor_scalar(out=t1[:], in0=md[:, DC:2 * DC, :], scalar1=1.0, scalar2=None, op0=ALU.add)

                seq.group(nc.vector, f2)

                def f3():
                    last = None
                    for r in range(2):
                        last = nc.vector.tensor_tensor(out=normA[i][:, :, r], in0=t1[:, :, r], in1=gg[:], op=ALU.mult)
                    return last

                seq.group(nc.vector, f3)

        def norm_stage(A_of, B_of, out_hbm, out_dt, ncols):
            NT = 128

            def mk(es, l):
                return dict(xt=es.enter_context(nc.sbuf_tensor(uid("nx"), [128, DC, NT], F32)),
                            sq=es.enter_context(nc.sbuf_tensor(uid("nsq"), [128, DC, NT], F32)),
                            ho=es.enter_context(nc.sbuf_tensor(uid("nh"), [128, DC, NT], out_dt)),
                            rs=es.enter_context(nc.sbuf_tensor(uid("nrs"), [128, NT], F32)),
                            pss=es.enter_context(nc.psum_tensor(uid("nps"), [128, 512], F32)))

            def body(ls, Bf, n0):
                xt, sq, ho, rs, pss = Bf["xt"], Bf["sq"], Bf["ho"], Bf["rs"], Bf["pss"]
                ns = min(NT, ncols - n0)
                r = 0 if n0 < T else 1
                ls.dmas(nc.sync, [dict(out=xt[:, :, :ns], in_=X[:, n0:n0 + ns].rearrange("(c p) n -> p c n", p=128))])
                yield
                ls.group(nc.scalar, lambda: nc.scalar.activation(out=sq[:, :, :ns], in_=xt[:, :, :ns], func=AF.Square))
                yield

                def pe():
                    last = None
                    for c in range(DC):
                        last = nc.tensor.matmul(pss[:, :ns], ones_f[:], sq[:, c, :ns], start=(c == 0), stop=(c == DC - 1))
                    return last

                ls.group(nc.tensor, pe)
                yield
                ls.group(nc.scalar, lambda: nc.scalar.activation(out=rs[:, :ns], in_=pss[:, :ns], func=AF.Sqrt,
                                                                 bias=eps_t[:], scale=1.0 / D))
                yield
                ls.group(nc.vector, lambda: nc.vector.reciprocal(out=rs[:, :ns], in_=rs[:, :ns]))
                yield

                def f1():
                    last = None
                    for c in range(DC):
                        last = nc.vector.tensor_tensor(out=sq[:, c, :ns], in0=xt[:, c, :ns], in1=rs[:, :ns], op=ALU.mult)
                    return last

                ls.group(nc.vector, f1)
                yield

                def f2():
                    last = None
                    for c in range(DC):
                        last = nc.scalar.activation(out=ho[:, c, :ns], in_=sq[:, c, :ns], func=AF.Identity,
                                                    scale=A_of(r)[:, c:c + 1], bias=B_of(r)[:, c:c + 1])
                    return last

                ls.group(nc.scalar, f2)
                yield
                ls.dmas(nc.sync, [dict(out=out_hbm[:, n0:n0 + ns].rearrange("(c p) n -> p c n", p=128), in_=ho[:, :, :ns])])
                yield

            lanes(nc, seq, mk, list(range(0, ncols, NT)), body)

        def gate_stage(scale_hbm):
            with ExitStack() as es0:
                scs = es0.enter_context(nc.sbuf_tensor(uid("gs"), [128, EC], F32))
                if scale_hbm is not None:
                    seq.dmas(nc.sync, [dict(out=scs[:], in_=scale_hbm.rearrange("(c p) -> p c", p=128))])
                else:
                    seq.group(nc.vector, lambda: nc.vector.memset(scs[:], 1.0))

                def mk(es, l):
                    return dict(pt=es.enter_context(nc.sbuf_tensor(uid("gp"), [128, N], F32)),
                                zt=es.enter_context(nc.sbuf_tensor(uid("gz"), [128, N], F32)),
                                go=es.enter_context(nc.sbuf_tensor(uid("go"), [128, N], BF16)))

                def body(ls, Bf, ec):
                    pt_, zt, go = Bf["pt"], Bf["zt"], Bf["go"]
                    rs_ = slice(ec * 128, (ec + 1) * 128)
                    ls.dmas(nc.sync, [dict(out=pt_[:], in_=P[rs_, :]), dict(out=zt[:], in_=Z[rs_, :])])
                    yield
                    ls.group(nc.scalar, lambda: nc.scalar.activation(out=zt[:], in_=zt[:], func=AF.Silu))
                    yield
                    ls.group(nc.vector, lambda: nc.vector.scalar_tensor_tensor(out=go[:], in0=pt_[:], scalar=scs[:, ec:ec + 1],
                                                                               in1=zt[:], op0=ALU.mult, op1=ALU.mult))
                    yield
                    ls.dmas(nc.sync, [dict(out=G[rs_, :], in_=go[:])])
                    yield

                lanes(nc, seq, mk, list(range(EC)), body)

        def wout_stage(i, w_out):
            def sc(gm, n0):
                return gateS[i][:, gm, (0 if n0 < T else 1):(1 if n0 < T else 2)]
            mmp(nc, seq, X, [(w_out, G)], scale=sc, accumulate=True)

        for i in range(cfg.depth):
            kind, j = i % 3, i // 3
            norm_stage(lambda r: normA[i][:, :, r], lambda r: normB[i][:, :, r], H, BF16, N)
            w_in = (I["a_w_in"], I["b_w_in"], I["c_w_in"])[kind][j]
            mmp(nc, seq, U, [(w_in[:, 0:E], H)])
            mmp(nc, seq, Z, [(w_in[:, E:2 * E], H)])
            if kind == 1:
                pool_layer(nc, seq, cfg, I, j, U, DL, P)
                gate_stage(I["b_scale"][j])
                wout_stage(i, I["b_w_out"][j])
            elif kind == 2:
                fourier_layer(nc, seq, cfg, I, j, U, FA, FB, DL, P)
                gate_stage(None)
                wout_stage(i, I["c_w_out"][j])
            else:
                mlstm_layer(nc, seq, cfg, I, j, dict(UZ=U, Z=Z, XC=XC, QT=QT, KT=KT, VT=VT, VTOK=VTOK, HD=HD, BD=BD, BD_h=BD_h,
                                                     WG=WG, GP=GP, HN=P, G=G, ident=ident, ones_b=ones_b, eps_t=eps_t,
                                                     one_t=one_t, selT=selT, BZ=BZ, LMB=LMB))
                wout_stage(i, I["a_w_out"][j])

        norm_stage(lambda r: finA[:], lambda r: zeroB[:], yT, F32, T)
        nc.sync.wait_ge(seq.prev[0], seq.prev[1])
    nc._n_seq_sems = seq.n
    return nc


def pool_layer(nc, seq, cfg, I, j, UZ, DL, P):
    T, TC, N, E, GW, EC = cfg.T, cfg.TC, cfg.N, cfg.E, cfg.GW, cfg.EC
    PAD = 16
    segs = ((0, T), (T, TC))
    with ExitStack() as es0:
        invc = es0.enter_context(nc.sbuf_tensor(uid("pinv"), [128, N], F32))
        for g in range(4):
            w = POOL_WINDOWS[g]
            lo = w // 2
            hi = w - 1 - lo
            seq.dmas(nc.sync, [dict(out=invc[:], in_=I["k_invc"][g])])

            def mk(es, l):
                bufs = []
                for si, (o, n) in enumerate(segs):
                    bufs.append(tuple(es.enter_context(nc.sbuf_tensor(uid("pb"), [128, n + 2 * PAD], F32)) for _ in range(3)))
                do = es.enter_context(nc.sbuf_tensor(uid("pdo"), [128, N], BF16))

                def z0():
                    last = None
                    for tri in bufs:
                        for t_ in tri:
                            last = nc.vector.memset(t_[:], 0.0)
                    return last

                seq.group(nc.vector, z0)
                return dict(bufs=bufs, do=do)

            def body(ls, Bf, ec, w=w, hi=hi):
                bufs, do = Bf["bufs"], Bf["do"]
                rs_ = slice(ec * 128, (ec + 1) * 128)
                ls.dmas(nc.sync, [dict(out=bufs[si][0][:, PAD:PAD + n], in_=UZ[rs_, o:o + n]) for si, (o, n) in enumerate(segs)])
                yield
                cov = 1
                cur = 0
                while cov < w:
                    nxt = 1 if cur != 1 else 2

                    def stp(cur=cur, nxt=nxt, cov=cov):
                        last = None
                        for si, (o, n) in enumerate(segs):
                            L = n + 2 * PAD
                            last = nc.vector.tensor_tensor(out=bufs[si][nxt][:, cov:L], in0=bufs[si][cur][:, cov:L],
                                                           in1=bufs[si][cur][:, 0:L - cov], op=ALU.add)
                        return last

                    ls.group(nc.vector, stp)
                    yield
                    cur = nxt
                    cov *= 2
                tmpi = 1 if cur != 1 else 2

                def m1(cur=cur, tmpi=tmpi):
                    last = None
                    for si, (o, n) in enumerate(segs):
                        last = nc.vector.tensor_tensor(out=bufs[si][tmpi][:, PAD:PAD + n], in0=bufs[si][cur][:, PAD + hi:PAD + hi + n],
                                                       in1=invc[:, o:o + n], op=ALU.mult)
                    return last

                ls.group(nc.vector, m1)
                yield

                def m2(tmpi=tmpi):
                    last = None
                    for si, (o, n) in enumerate(segs):
                        last = nc.vector.tensor_tensor(out=do[:, o:o + n], in0=bufs[si][tmpi][:, PAD:PAD + n],
                                                       in1=bufs[si][0][:, PAD:PAD + n], op=ALU.subtract)
                    return last

                ls.group(nc.vector, m2)
                yield
                ls.dmas(nc.sync, [dict(out=DL[rs_, :], in_=do[:])])
                yield

            cpg = GW // 128
            lanes(nc, seq, mk, list(range(g * cpg, (g + 1) * cpg)), body)
    for g in range(4):
        mmp(nc, seq, P[g * GW:(g + 1) * GW, :], [(I["b_w_grp"][j, g], DL[g * GW:(g + 1) * GW, :])])


def fourier_layer(nc, seq, cfg, I, j, UZ, FA, FB, DL, P):
    T, TC, N, E, GW = cfg.T, cfg.TC, cfg.N, cfg.E, cfg.GW
    for g in range(4):
        gs = slice(g * GW, (g + 1) * GW)
        mmp(nc, seq, FA[:, gs], [(UZ[gs, :], I["k_cc"])])
        mmp(nc, seq, FB[:, gs], [(UZ[gs, :], I["k_sc"])])
    mmp(nc, seq, DL[:, 0:T], [(FA[0:T, :], I["k_ct"]), (FB[0:T, :], I["k_nst"])])
    mmp(nc, seq, DL[:, T:N], [(FA[T:N, :], I["k_ctc"]), (FB[T:N, :], I["k_nstc"])])
    for g in range(4):
        gs = slice(g * GW, (g + 1) * GW)
        mmp(nc, seq, P[gs, :], [(I["c_w_grp"][j, g], DL[gs, :])])


def mlstm_layer(nc, seq, cfg, I, j, S):
    T, TC, N, E, NH, DH, EC = cfg.T, cfg.TC, cfg.N, cfg.E, cfg.NH, cfg.DH, cfg.EC
    UZ, XC, QT, KT, VT, VTOK, HD, BD, WG, GP, HN, G = (S[k] for k in ("UZ", "XC", "QT", "KT", "VT", "VTOK", "HD", "BD", "WG", "GP", "HN", "G"))
    Z = S["Z"]
    ident, ones_b, eps_t, one_t, selT = S["ident"], S["ones_b"], S["eps_t"], S["one_t"], S["selT"]
    segs = ((0, T), (T, TC))
    NTC = N // 128
    DCH = DH // 128

    with ExitStack() as es0:
        cw = es0.enter_context(nc.sbuf_tensor(uid("cw"), [128, 4, EC], F32))
        cb = es0.enter_context(nc.sbuf_tensor(uid("cb"), [128, EC], F32))
        seq.dmas(nc.sync, [dict(out=cw[:, t_, :], in_=I["a_conv_w"][j, t_].rearrange("(c p) -> p c", p=128)) for t_ in range(4)]
                 + [dict(out=cb[:], in_=I["a_conv_b"][j].rearrange("(c p) -> p c", p=128))])

        def mk(es, l):
            ub = [es.enter_context(nc.sbuf_tensor(uid("cu"), [128, n + 3], F32)) for (o, n) in segs]
            acc = [es.enter_context(nc.sbuf_tensor(uid("ca"), [128, N], F32)) for _ in range(2)]

            def z0():
                nc.vector.memset(ub[0][:], 0.0)
                return nc.vector.memset(ub[1][:], 0.0)

            seq.group(nc.vector, z0)
            return dict(ub=ub, acc=acc)

        def body(ls, Bf, ec):
            ub, acc = Bf["ub"], Bf["acc"]
            rs_ = slice(ec * 128, (ec + 1) * 128)
            ls.dmas(nc.sync, [dict(out=ub[si][:, 1:1 + n], in_=UZ[rs_, o:o + n]) for si, (o, n) in enumerate(segs)])
            yield
            for t_ in range(4):
                src, dst = acc[(t_ + 1) % 2], acc[t_ % 2]

                def stp(t_=t_, src=src, dst=dst):
                    last = None
                    for si, (o, n) in enumerate(segs):
                        if t_ == 0:
                            last = nc.vector.tensor_scalar(out=dst[:, o:o + n], in0=ub[si][:, 0:n], scalar1=cw[:, 0, ec:ec + 1],
                                                           scalar2=None, op0=ALU.mult)
                        else:
                            last = nc.vector.scalar_tensor_tensor(out=dst[:, o:o + n], in0=ub[si][:, t_:t_ + n],
                                                                  scalar=cw[:, t_, ec:ec + 1], in1=src[:, o:o + n],
                                                                  op0=ALU.mult, op1=ALU.add)
                    return last

                ls.group(nc.vector, stp)
                yield
            ls.group(nc.scalar, lambda: nc.scalar.activation(out=acc[0][:], in_=acc[1][:], func=AF.Silu, bias=cb[:, ec:ec + 1], scale=1.0))
            yield
            ls.dmas(nc.sync, [dict(out=XC[rs_, :], in_=acc[0][:])])
            yield

        lanes(nc, seq, mk, list(range(EC)), body)

    with ExitStack() as es:
        zt = es.enter_context(nc.sbuf_tensor(uid("bz"), [128, EC, 128], F32))
        seq.group(nc.vector, lambda: nc.vector.memset(zt[:], 0.0))
        seq.dmas(nc.sync, [dict(out=BD[m * EC * 128:(m + 1) * EC * 128, :].rearrange("(c p) q -> p c q", p=128), in_=zt[:]) for m in range(3)])
        items = []
        for m, wname in enumerate(("a_wq", "a_wk", "a_wv")):
            w = I[wname]
            for c in range(EC):
                dst = bass.AP(S["BD_h"], (m * EC + c) * 128 * 128, [[516, 32], [128, 4], [1, 4]])
                items.append(dict(out=dst, in_=w[j, 32 * c:32 * (c + 1), :, :]))
        for k0 in range(0, len(items), 32):
            seq.dmas(nc.sync, items[k0:k0 + 32])
    if True:
        NT = 512
        tiles = [(n0, min(NT, N - n0)) for n0 in range(0, N, NT)]
        jobs = [(m, n0, ns) for m in range(3) for (n0, ns) in tiles]

        def mk(es, l):
            return dict(xcb=es.enter_context(nc.sbuf_tensor(uid("qx"), [128, N], BF16)),
                        ubf=es.enter_context(nc.sbuf_tensor(uid("qu"), [128, N], BF16)),
                        bdb=es.enter_context(nc.sbuf_tensor(uid("qb"), [128, 3, 128], BF16)),
                        oq=es.enter_context(nc.sbuf_tensor(uid("qo"), [128, 3, N], BF16)),
                        ov=es.enter_context(nc.sbuf_tensor(uid("qv"), [128, NTC, 128], BF16)),
                        ps=[es.enter_context(nc.psum_tensor(uid("qps"), [128, 512], F32)) for _ in range(4)])

        def body(ls, Bf, ec):
            xcb, ubf, bdb, oq, ov, ps = (Bf[k] for k in ("xcb", "ubf", "bdb", "oq", "ov", "ps"))
            rs_ = slice(ec * 128, (ec + 1) * 128)
            ls.dmas(nc.gpsimd, [dict(out=xcb[:], in_=XC[rs_, :]), dict(out=ubf[:], in_=UZ[rs_, :])]
                    + [dict(out=bdb[:, m, :], in_=BD[(m * EC + ec) * 128:(m * EC + ec + 1) * 128, :]) for m in range(3)])
            yield
            for b0 in range(0, len(jobs), 4):
                jb = jobs[b0:b0 + 4]

                def pe(jb=jb):
                    last = None
                    for bi, (m, n0, ns) in enumerate(jb):
                        src = ubf if m == 2 else xcb
                        last = nc.tensor.matmul(ps[bi][:, :ns], bdb[:, m, :], src[:, n0:n0 + ns], start=True, stop=True)
                    return last

                ls.group(nc.tensor, pe)
                yield

                def ev(jb=jb):
                    last = None
                    for bi, (m, n0, ns) in enumerate(jb):
                        last = nc.scalar.copy(out=oq[:, m, n0:n0 + ns], in_=ps[bi][:, :ns])
                    return last

                ls.group(nc.scalar, ev)
                yield
            ls.dmas(nc.sync, [dict(out=QT[rs_, :], in_=oq[:, 0, :]), dict(out=KT[rs_, :], in_=oq[:, 1, :]), dict(out=VT[rs_, :], in_=oq[:, 2, :])])
            yield
            for b0 in range(0, NTC, 16):
                tcs = list(range(b0, min(NTC, b0 + 16)))

                def pe2(tcs=tcs):
                    last = None
                    for bi, tc in enumerate(tcs):
                        last = nc.tensor.matmul(ps[bi // 4][:, (bi % 4) * 128:(bi % 4 + 1) * 128], ubf[:, tc * 128:(tc + 1) * 128],
                                                bdb[:, 2, :], start=True, stop=True)
                    return last

                ls.group(nc.tensor, pe2)
                yield

                def ev2(tcs=tcs):
                    last = None
                    for bi, tc in enumerate(tcs):
                        last = nc.scalar.copy(out=ov[:, tc, :], in_=ps[bi // 4][:, (bi % 4) * 128:(bi % 4 + 1) * 128])
                    return last

                ls.group(nc.scalar, ev2)
                yield
            ls.dmas(nc.sync, [dict(out=VTOK[:, rs_].rearrange("(tc p) e -> p tc e", p=128), in_=ov[:])])
            yield

        lanes(nc, seq, mk, list(range(EC)), body)

    items = []
    for gi, (wn, z) in enumerate((("a_w_ig", 0), ("a_w_ig", 1), ("a_w_fg", 0), ("a_w_fg", 1))):
        for r0 in range(0, 3 * E, 2048):
            r1 = min(3 * E, r0 + 2048)
            items.append(dict(out=WG[r0:r1, gi * 8:(gi + 1) * 8], in_=I[wn][j, z, r0:r1, :]))
    for k0 in range(0, len(items), 16):
        seq.dmas(nc.sync, items[k0:k0 + 16])
    mmp(nc, seq, GP, [(WG[0:E, :], QT), (WG[E:2 * E, :], KT), (WG[2 * E:3 * E, :], VT)])

    with ExitStack() as es:
        def t8(name):
            return es.enter_context(nc.sbuf_tensor(uid(name), [8, N], F32))
        Bz = [t8("Bz0"), t8("Bz1")]
        LmB = [t8("Lm0"), t8("Lm1")]
        li = [t8("li0"), t8("li1")]
        xf = [t8("xf0"), t8("xf1")]
        tmp = t8("tmp")
        onesr = t8("onesr")
        bi_ = es.enter_context(nc.sbuf_tensor(uid("bi"), [8, 2], F32))
        bf_ = es.enter_context(nc.sbuf_tensor(uid("bf"), [8, 2], F32))
        tot = es.enter_context(nc.sbuf_tensor(uid("tot"), [8, 4], F32))
        seq.dmas(nc.sync, [dict(out=li[z][:], in_=GP[8 * z:8 * z + 8, :]) for z in range(2)]
                 + [dict(out=xf[z][:], in_=GP[16 + 8 * z:24 + 8 * z, :]) for z in range(2)]
                 + [dict(out=bi_[:, z:z + 1], in_=I["a_b_ig"][j, z].rearrange("(n o) -> n o", o=1)) for z in range(2)]
                 + [dict(out=bf_[:, z:z + 1], in_=I["a_b_fg"][j, z].rearrange("(n o) -> n o", o=1)) for z in range(2)])

        def g1():
            nc.vector.memset(onesr[:], 1.0)
            return nc.vector.tensor_scalar(out=bf_[:], in0=bf_[:], scalar1=-1.0, scalar2=None, op0=ALU.mult)

        seq.group(nc.vector, g1)

        def g2():
            last = None
            for z in range(2):
                nc.scalar.activation(out=li[z][:], in_=li[z][:], func=AF.Identity, bias=bi_[:, z:z + 1], scale=1.0)
                last = nc.scalar.activation(out=xf[z][:], in_=xf[z][:], func=AF.Exp, bias=bf_[:, z:z + 1], scale=-1.0)
            return last

        seq.group(nc.scalar, g2)

        def g3():
            last = None
            for z in range(2):
                last = nc.scalar.activation(out=xf[z][:], in_=xf[z][:], func=AF.Ln, bias=S["one_t"][:8, :], scale=1.0)
            return last

        seq.group(nc.scalar, g3)
        def g4():
            last = None
            for z in range(2):
                for (o, n) in segs:
                    last = nc.vector.tensor_tensor_scan(out=Bz[z][:, o:o + n], data0=onesr[:, o:o + n], data1=xf[z][:, o:o + n],
                                                        initial=0.0, op0=ALU.mult, op1=ALU.add)
            return last

        seq.group(nc.vector, g4)
        def g5():
            nc.vector.tensor_copy(out=tot[:, 0:1], in_=Bz[0][:, N - 1:N])
            nc.vector.tensor_copy(out=tot[:, 1:2], in_=Bz[1][:, N - 1:N])
            nc.vector.tensor_copy(out=tot[:, 2:3], in_=Bz[1][:, T - 1:T])
            return nc.vector.tensor_tensor(out=tmp[:], in0=xf[1][:], in1=Bz[1][:], op=ALU.subtract)

        seq.group(nc.vector, g5)

        def g6():
            nc.vector.tensor_tensor(out=tot[:, 3:4], in0=tot[:, 2:3], in1=tot[:, 1:2], op=ALU.add)
            return nc.vector.tensor_scalar(out=Bz[0][:, 0:T], in0=Bz[0][:, 0:T], scalar1=tot[:, 0:1], scalar2=None, op0=ALU.add)

        seq.group(nc.vector, g6)

        def g7():
            nc.vector.tensor_scalar(out=Bz[1][:, 0:T], in0=tmp[:, 0:T], scalar1=tot[:, 3:4], scalar2=None, op0=ALU.add)
            nc.vector.tensor_scalar(out=Bz[1][:, T:N], in0=tmp[:, T:N], scalar1=tot[:, 1:2], scalar2=None, op0=ALU.add)
            return nc.vector.tensor_scalar(out=Bz[0][:], in0=Bz[0][:], scalar1=-1.0, scalar2=None, op0=ALU.mult)

        seq.group(nc.vector, g7)
        seq.group(nc.vector, lambda: nc.vector.tensor_scalar(out=Bz[1][:], in0=Bz[1][:], scalar1=-1.0, scalar2=None, op0=ALU.mult))

        def g8():
            last = None
            for z in range(2):
                last = nc.vector.scalar_tensor_tensor(out=LmB[z][:], in0=li[z][:], scalar=-0.5 * math.log(DH), in1=Bz[z][:],
                                                      op0=ALU.add, op1=ALU.subtract)
            return last

        seq.group(nc.vector, g8)
        seq.dmas(nc.sync, [dict(out=S["BZ"][z], in_=Bz[z][:]) for z in range(2)] + [dict(out=S["LMB"][z], in_=LmB[z][:]) for z in range(2)])

    if True:
        QW = 256
        TS = QW // 128
        VW = min(DH, 512)
        VH = DH // VW
        KTL = T // 128
        with ExitStack() as e5:
            kT = e5.enter_context(nc.sbuf_tensor(uid("akT"), [128, DCH, N], BF16))
            vk = e5.enter_context(nc.sbuf_tensor(uid("avk"), [128, NTC, DH], BF16))
            qT2 = [e5.enter_context(nc.sbuf_tensor(uid("aqT"), [128, DCH, QW], BF16)) for _ in range(2)]
            csb = e5.enter_context(nc.sbuf_tensor(uid("acs"), [128, NTC], F32))
            lmb = e5.enter_context(nc.sbuf_tensor(uid("almb"), [8, N], F32))
            bzt2 = [e5.enter_context(nc.sbuf_tensor(uid("abzt"), [8, QW], F32)) for _ in range(2)]
            brow = e5.enter_context(nc.sbuf_tensor(uid("abr"), [128, 3, QW], F32))
            mask = e5.enter_context(nc.sbuf_tensor(uid("amk"), [128, 4, 256], F32))
            esb = e5.enter_context(nc.sbuf_tensor(uid("aes"), [128, 4, QW], F32))
            wsb = e5.enter_context(nc.sbuf_tensor(uid("aws"), [128, 4, QW], BF16))
            rsb = e5.enter_context(nc.sbuf_tensor(uid("ars"), [128, TS], F32))
            hsb2 = [e5.enter_context(nc.sbuf_tensor(uid("ahs"), [128, TS, DH], F32)) for _ in range(2)]
            ps_num = [[e5.enter_context(nc.psum_tensor(uid("apn"), [128, 512], F32)) for _ in range(VH)] for _ in range(TS)]
            ps_den = e5.enter_context(nc.psum_tensor(uid("apd"), [128, 512], F32))
            ps_s = [e5.enter_context(nc.psum_tensor(uid("aps"), [128, 512], F32)) for _ in range(2)]
            ps_t = e5.enter_context(nc.psum_tensor(uid("apt"), [128, 512], F32))
            seq.dmas(nc.sync, [dict(out=mask[:], in_=I["k_mask"])])
            A = _CTX.setdefault("att", None) or dict(sS=nc.alloc_semaphore("aS"), sE=nc.alloc_semaphore("aE"), sV=nc.alloc_semaphore("aV"),
                                                     sN=nc.alloc_semaphore("aN"), vS=0, vE=0, vV=0, vN=0, gb=0, qn=0,
                                                     sLD=[nc.alloc_semaphore("aLD0"), nc.alloc_semaphore("aLD1")], vLD=[0, 0],
                                                     sST=[nc.alloc_semaphore("aST0"), nc.alloc_semaphore("aST1")], vST=[0, 0])
            _CTX["att"] = A
            for n in range(NH):
                hs_ = slice(n * DH, (n + 1) * DH)
                seq.dmas(nc.sync, [dict(out=kT[:], in_=KT[hs_, :].rearrange("(c p) t -> p c t", p=128)),
                                   dict(out=vk[:], in_=VTOK[:, hs_].rearrange("(tc p) v -> p tc v", p=128))])
                for z in range(2):
                    seq.dmas(nc.sync, [dict(out=lmb[:], in_=S["LMB"][z])])

                    def pc():
                        last = None
                        for kt in range(NTC):
                            last = nc.tensor.matmul(ps_t[:, kt:kt + 1], lmb[:, kt * 128:(kt + 1) * 128], ident[:8, n:n + 1],
                                                    start=True, stop=True)
                        return last

                    seq.group(nc.tensor, pc)
                    seq.group(nc.vector, lambda: nc.vector.tensor_copy(out=csb[:], in_=ps_t[:, :NTC]))
                    qtiles = [(q0, True) for q0 in range(0, T, QW)] + [(T, False)]
                    for qidx, (q0, is_lat) in enumerate(qtiles):
                        qpar = A["qn"] % 2
                        qT, bzt, hsb = qT2[qpar], bzt2[qpar], hsb2[qpar]
                        qi = q0 // QW if is_lat else 0
                        if is_lat:
                            full = [KTL + c for c in range(TC // 128)]
                            full += list(range(0, 2 * qi)) if z == 0 else list(range(2 * qi + 2, KTL))
                            keys = [(kt, 0) for kt in full] + [(2 * qi, 1), (2 * qi + 1, 2)]
                        else:
                            keys = [(KTL, 1), (KTL + 1, 2)]
                        def load_q(par_, q0_):
                            nc.sync.dma_start(out=qT2[par_][:], in_=QT[hs_, q0_:q0_ + QW].rearrange("(c p) t -> p c t", p=128)).then_inc(A["sLD"][par_], 16)
                            nc.sync.dma_start(out=bzt2[par_][:], in_=S["BZ"][z][:, q0_:q0_ + QW]).then_inc(A["sLD"][par_], 16)
                            A["vLD"][par_] += 32

                        if qidx == 0:
                            nc.sync.wait_ge(seq.prev[0], seq.prev[1])
                            load_q(qpar, q0)
                        ld_val = A["vLD"][qpar]

                        def brow_mm():
                            nc.tensor.wait_ge(A["sLD"][qpar], ld_val)
                            return nc.tensor.matmul(ps_t[:, :QW], selT[:, n, :], bzt[:], start=True, stop=True)

                        seq.group(nc.tensor, brow_mm)
                        if qidx + 1 < len(qtiles):
                            nc.sync.wait_ge(seq.prev[0], seq.prev[1])
                            load_q(1 - qpar, qtiles[qidx + 1][0])

                        def gb():
                            nc.vector.tensor_copy(out=brow[:, 0, :], in_=ps_t[:, :QW])
                            nc.vector.tensor_tensor(out=brow[:, 1, :], in0=ps_t[:, :QW], in1=mask[:, 2 * z, :], op=ALU.add)
                            return nc.vector.tensor_tensor(out=brow[:, 2, :], in0=ps_t[:, :QW], in1=mask[:, 2 * z + 1, :], op=ALU.add)

                        seq.group(nc.vector, gb)
                        nk = len(keys)
                        T0 = seq.prev
                        batches = [keys[b0:b0 + 2] for b0 in range(0, nk, 2)]
                        nb = len(batches)
                        sval, eval_, vval, nval = {}, {}, {}, {}

                        def emit_E(b):
                            par = (A["gb"] + b) % 2
                            if b == 0:
                                nc.scalar.wait_ge(T0[0], T0[1])
                            if b >= 2:
                                nc.scalar.wait_ge(A["sV"], vval[b - 2])
                            last = None
                            for bi, (kt, mt) in enumerate(batches[b]):
                                last = nc.scalar.activation(out=esb[:, par * 2 + bi, :], in_=brow[:, mt, :], func=AF.Exp,
                                                            bias=csb[:, kt:kt + 1], scale=1.0)
                            last.then_inc(A["sE"], 1)
                            A["vE"] += 1
                            eval_[b] = A["vE"]

                        def emit_S(b):
                            par = (A["gb"] + b) % 2
                            if b == 0:
                                nc.tensor.wait_ge(T0[0], T0[1])
                            if b >= 2:
                                nc.tensor.wait_ge(A["sV"], vval[b - 2])
                            last = None
                            for bi, (kt, mt) in enumerate(batches[b]):
                                o_ = ps_s[par][:, bi * QW:(bi + 1) * QW]
                                for dc in range(DCH):
                                    last = nc.tensor.matmul(o_, kT[:, dc, kt * 128:(kt + 1) * 128], qT[:, dc, :],
                                                            start=(dc == 0), stop=(dc == DCH - 1))
                            last.then_inc(A["sS"], 1)
                            A["vS"] += 1
                            sval[b] = A["vS"]

                        def emit_V(b):
                            par = (A["gb"] + b) % 2
                            nc.vector.wait_ge(A["sS"], sval[b])
                            nc.vector.wait_ge(A["sE"], eval_[b])
                            if b >= 2:
                                nc.vector.wait_ge(A["sN"], nval[b - 2])
                            last = None
                            for bi, (kt, mt) in enumerate(batches[b]):
                                o_ = ps_s[par][:, bi * QW:(bi + 1) * QW]
                                last = nc.vector.tensor_tensor(out=wsb[:, par * 2 + bi, :], in0=o_, in1=esb[:, par * 2 + bi, :], op=ALU.mult)
                            last.then_inc(A["sV"], 1)
                            A["vV"] += 1
                            vval[b] = A["vV"]

                        def emit_N(b):
                            par = (A["gb"] + b) % 2
                            nc.tensor.wait_ge(A["sV"], vval[b])
                            last = None
                            for bi, (kt, mt) in enumerate(batches[b]):
                                first = (b == 0 and bi == 0)
                                lastk = (b == nb - 1 and bi == len(batches[b]) - 1)
                                for ts in range(TS):
                                    lw = wsb[:, par * 2 + bi, ts * 128:(ts + 1) * 128]
                                    for vh in range(VH):
                                        nc.tensor.matmul(ps_num[ts][vh][:, :VW], lw, vk[:, kt, vh * VW:(vh + 1) * VW],
                                                         start=first, stop=lastk)
                                    last = nc.tensor.matmul(ps_den[:, ts:ts + 1], lw, ones_b[:, 0:1], start=(first and ts == 0),
                                                            stop=lastk, skip_group_check=True)
                            last.then_inc(A["sN"], 1)
                            A["vN"] += 1
                            nval[b] = A["vN"]

                        for b in range(nb):
                            emit_E(b)
                            emit_S(b)
                            emit_V(b)
                            if b >= 1:
                                emit_N(b - 1)
                        emit_N(nb - 1)
                        A["gb"] += nb
                        seq.prev = (A["sN"], A["vN"])
                        seq.group(nc.scalar, lambda: nc.scalar.activation(out=rsb[:], in_=ps_den[:, :TS], func=AF.Abs))
                        seq.group(nc.vector, lambda: nc.vector.tensor_scalar(out=rsb[:], in0=rsb[:], scalar1=1.0, scalar2=None,
                                                                             op0=ALU.max))
                        seq.group(nc.vector, lambda: nc.vector.reciprocal(out=rsb[:], in_=rsb[:]))

                        def a2():
                            last = None
                            for ts in range(TS):
                                for vh in range(VH):
                                    last = nc.scalar.activation(out=hsb[:, ts, vh * VW:(vh + 1) * VW], in_=ps_num[ts][vh][:, :VW],
                                                                func=AF.Identity, scale=rsb[:, ts:ts + 1])
                            return last

                        def a2w():
                            nc.scalar.wait_ge(A["sST"][qpar], A["vST"][qpar])
                            return a2()

                        seq.group(nc.scalar, a2w)
                        nc.sync.wait_ge(seq.prev[0], seq.prev[1])
                        nc.sync.dma_start(out=HD[z][q0:q0 + QW, hs_].rearrange("(ts p) v -> p ts v", p=128), in_=hsb[:]).then_inc(A["sST"][qpar], 16)
                        A["vST"][qpar] += 16
                        A["qn"] += 1
            nc.vector.wait_ge(A["sST"][0], A["vST"][0])
            nc.vector.wait_ge(A["sST"][1], A["vST"][1])
            seq.group(nc.vector, lambda: nc.vector.memset(rsb[:], 0.0))

    NHH = NH // 2
    ECH = EC // 2

    def mk6(es, l):
        return dict(a=es.enter_context(nc.sbuf_tensor(uid("fa"), [128, NHH, DH], F32)),
                    b=es.enter_context(nc.sbuf_tensor(uid("fb"), [128, NHH, DH], F32)),
                    c=es.enter_context(nc.sbuf_tensor(uid("fc"), [128, NHH, DH], F32)),
                    st=es.enter_context(nc.sbuf_tensor(uid("fs"), [128, 4, NHH], F32)),
                    hT=es.enter_context(nc.sbuf_tensor(uid("fT"), [128, ECH, 128], F32)),
                    ps=[es.enter_context(nc.psum_tensor(uid("fps"), [128, 512], F32)) for _ in range(4)])

    def body6(ls, Bf, item):
        tc, hh = item
        a, b, c, st, hT, ps = (Bf[k] for k in ("a", "b", "c", "st", "hT", "ps"))
        ts_ = slice(tc * 128, (tc + 1) * 128)
        es_ = slice(hh * NHH * DH, (hh + 1) * NHH * DH)
        ls.dmas(nc.sync, [dict(out=a[:], in_=HD[0][ts_, es_].rearrange("p (n v) -> p n v", n=NHH)),
                          dict(out=b[:], in_=HD[1][ts_, es_].rearrange("p (n v) -> p n v", n=NHH))])
        yield
        ls.group(nc.vector, lambda: nc.vector.tensor_tensor(out=a[:], in0=a[:], in1=b[:], op=ALU.add))
        yield
        ls.group(nc.vector, lambda: nc.vector.reduce_sum(out=st[:, 0, :], in_=a[:], axis=AX.X))
        yield
        ls.group(nc.vector, lambda: nc.vector.tensor_scalar(out=st[:, 1, :], in0=st[:, 0, :], scalar1=-1.0 / DH, scalar2=None, op0=ALU.mult))
        yield

        def f1():
            last = None
            for n in range(NHH):
                last = nc.scalar.activation(out=b[:, n, :], in_=a[:, n, :], func=AF.Identity, bias=st[:, 1, n:n + 1], scale=1.0)
            return last

        ls.group(nc.scalar, f1)
        yield
        ls.group(nc.vector, lambda: nc.vector.tensor_tensor(out=c[:], in0=b[:], in1=b[:], op=ALU.mult))
        yield
        ls.group(nc.vector, lambda: nc.vector.reduce_sum(out=st[:, 2, :], in_=c[:], axis=AX.X))
        yield
        ls.group(nc.scalar, lambda: nc.scalar.activation(out=st[:, 3, :], in_=st[:, 2, :], func=AF.Sqrt, bias=eps_t[:], scale=1.0 / DH))
        yield
        ls.group(nc.vector, lambda: nc.vector.reciprocal(out=st[:, 3, :], in_=st[:, 3, :]))
        yield

        def f3():
            last = None
            for n in range(NHH):
                last = nc.vector.tensor_scalar(out=a[:, n, :], in0=b[:, n, :], scalar1=st[:, 3, n:n + 1], scalar2=None, op0=ALU.mult)
            return last

        ls.group(nc.vector, f3)
        yield
        for b0 in range(0, ECH, 16):
            ecs = list(range(b0, min(ECH, b0 + 16)))

            def pe(ecs=ecs):
                last = None
                for bi, ec in enumerate(ecs):
                    n, vc = divmod(ec, DCH)
                    last = nc.tensor.matmul(ps[bi // 4][:, (bi % 4) * 128:(bi % 4 + 1) * 128], a[:, n, vc * 128:(vc + 1) * 128],
                                            ident[:], start=True, stop=True)
                return last

            ls.group(nc.tensor, pe)
            yield

            def ev(ecs=ecs):
                last = None
                for bi, ec in enumerate(ecs):
                    last = nc.scalar.copy(out=hT[:, ec, :], in_=ps[bi // 4][:, (bi % 4) * 128:(bi % 4 + 1) * 128])
                return last

            ls.group(nc.scalar, ev)
            yield
        ls.dmas(nc.sync, [dict(out=HN[hh * ECH * 128:(hh + 1) * ECH * 128, ts_].rearrange("(c p) t -> p c t", p=128), in_=hT[:])])
        yield

    lanes(nc, seq, mk6, [(tc, hh) for tc in range(NTC) for hh in range(2)], body6)

    with ExitStack() as es0:
        hw = es0.enter_context(nc.sbuf_tensor(uid("g5"), [128, EC], F32))
        sk = es0.enter_context(nc.sbuf_tensor(uid("g6"), [128, EC], F32))
        seq.dmas(nc.sync, [dict(out=hw[:], in_=I["a_hnorm_w"][j].rearrange("(c p) -> p c", p=128)),
                           dict(out=sk[:], in_=I["a_skip"][j].rearrange("(c p) -> p c", p=128))])

        def mk7(es, l):
            return dict(ht=es.enter_context(nc.sbuf_tensor(uid("g1"), [128, N], F32)),
                        xt=es.enter_context(nc.sbuf_tensor(uid("g2"), [128, N], F32)),
                        zt=es.enter_context(nc.sbuf_tensor(uid("g3"), [128, N], F32)),
                        go=es.enter_context(nc.sbuf_tensor(uid("g4"), [128, N], BF16)))

        def body7(ls, Bf, ec):
            ht, xt, zt, go = Bf["ht"], Bf["xt"], Bf["zt"], Bf["go"]
            rs_ = slice(ec * 128, (ec + 1) * 128)
            ls.dmas(nc.sync, [dict(out=ht[:], in_=HN[rs_, :]), dict(out=xt[:], in_=XC[rs_, :]), dict(out=zt[:], in_=Z[rs_, :])])
            yield

            def f1():
                nc.scalar.activation(out=ht[:], in_=ht[:], func=AF.Identity, scale=hw[:, ec:ec + 1])
                return nc.scalar.activation(out=zt[:], in_=zt[:], func=AF.Silu)

            ls.group(nc.scalar, f1)
            yield
            ls.group(nc.vector, lambda: nc.vector.scalar_tensor_tensor(out=xt[:], in0=xt[:], scalar=sk[:, ec:ec + 1], in1=ht[:],
                                                                       op0=ALU.mult, op1=ALU.add))
            yield
            ls.group(nc.vector, lambda: nc.vector.tensor_tensor(out=go[:], in0=xt[:], in1=zt[:], op=ALU.mult))
            yield
            ls.dmas(nc.sync, [dict(out=G[rs_, :], in_=go[:])])
            yield

        lanes(nc, seq, mk7, list(range(EC)), body7)


_NP2BIR = {np.dtype(np.float32): F32, np.dtype(ml_dtypes.bfloat16): BF16}


def run(cfg, inputs, debug=(), trace=False):
    consts = host_consts(cfg)
    B = inputs["x"].shape[0]
    weights = {k: np.ascontiguousarray(inputs[k], dtype=np.float32) for k in WEIGHT_NAMES}
    shapes = {k: (v.shape, F32) for k, v in weights.items()}
    shapes["xT"] = ((cfg.D, cfg.N), F32)
    shapes["ccT"] = ((cfg.D, 2), F32)
    const_shapes = {k: (v.shape, _NP2BIR[v.dtype]) for k, v in consts.items()}
    nc = build(cfg, shapes, const_shapes, debug=debug)
    in_maps = []
    for b in range(B):
        m = dict(weights)
        m.update(consts)
        m["xT"] = np.ascontiguousarray(np.concatenate([inputs["x"][b].T, inputs["ctx"][b].T], axis=1), dtype=np.float32)
        m["ccT"] = np.ascontiguousarray(np.stack([inputs["c"][b], inputs["c_ctx"]], axis=1), dtype=np.float32)
        in_maps.append(m)
    res = run_bass_kernel_spmd(nc, in_maps, core_ids=list(range(B)), trace=trace)
    out = np.stack([np.ascontiguousarray(res.results[b]["yT"].T) for b in range(B)], axis=0).astype(np.float32)
    return out, res


def kernel(**inputs):
    cfg = Cfg()
    out, _ = run(cfg, inputs)
    return out
```

```python
import math
from contextlib import ExitStack

import numpy as np
import ml_dtypes
import concourse.bass as bass
import concourse.mybir as mybir
from concourse.bass_utils import run_bass_kernel_spmd

F32 = mybir.dt.float32
BF16 = mybir.dt.bfloat16
AF = mybir.ActivationFunctionType
ALU = mybir.AluOpType
AX = mybir.AxisListType
NEG = -30000.0
EPS = 1e-6
POOL_WINDOWS = (2, 4, 8, 16)

_uid = [0]


def uid(p):
    _uid[0] += 1
    return f"{p}_{_uid[0]}"


class Seq:
    def __init__(self, nc, limit=20000, tag="seq"):
        self.nc = nc
        self.n = 0
        self.limit = limit
        self.tag = tag
        self._new_sem()
        self.prev = None

    def _new_sem(self):
        self.sem = self.nc.alloc_semaphore(f"{self.tag}{self.n}")
        self.n += 1
        self.val = 0

    def _pre(self, eng):
        if self.prev is not None:
            eng.wait_ge(self.prev[0], self.prev[1])
        if self.val > self.limit:
            self._new_sem()

    def dmas(self, eng, items):
        if eng is self.nc.gpsimd and len(items) > 1:
            for kw in items:
                self.dmas(eng, [kw])
            return
        self._pre(eng)
        for kw in items:
            eng.dma_start(**kw).then_inc(self.sem, 16)
            self.val += 16
        self.prev = (self.sem, self.val)

    def group(self, eng, fn):
        self._pre(eng)
        last = fn()
        last.then_inc(self.sem, 1)
        self.val += 1
        self.prev = (self.sem, self.val)


def load_eng(nc, src_dtype, dst_dtype):
    return nc.sync if src_dtype == dst_dtype else nc.gpsimd


def mm(nc, seq, out_hbm, pairs, *, scale=None, func=None, accumulate=False, nt=512, mb=None, fp32=False):
    M, N = out_hbm.shape
    func = func or AF.Identity
    cdt = F32 if fp32 else BF16
    esz = 4 if fp32 else 2
    Ks = [l.shape[0] for l, r in pairs]
    kcs = [max(1, K // 128) for K in Ks]
    kps = [min(K, 128) for K in Ks]
    tot_kc = sum(kcs)
    if mb is None:
        mb = 1024
        while tot_kc * mb * esz > 64 * 1024 and mb > 128:
            mb //= 2
    mb = min(mb, M)
    nt = min(nt, N)
    while tot_kc * nt * esz > 48 * 1024 and nt > 128:
        nt //= 2
    mcb = max(1, (mb + 127) // 128)
    assert mcb <= 8
    with ExitStack() as es:
        wsb = es.enter_context(nc.sbuf_tensor(uid("mm_w"), [128, tot_kc, mb], cdt))
        xsb = es.enter_context(nc.sbuf_tensor(uid("mm_x"), [128, tot_kc, nt], cdt))
        osb = es.enter_context(nc.sbuf_tensor(uid("mm_o"), [128, mcb, nt], out_hbm.dtype))
        ps = [es.enter_context(nc.psum_tensor(uid("mm_ps"), [128, 512], F32)) for _ in range(mcb)]
        for m0 in range(0, M, mb):
            mbs = min(mb, M - m0)
            items = []
            off = 0
            for (l, r), K, kc, kp in zip(pairs, Ks, kcs, kps):
                src = l[:, m0:m0 + mbs]
                if kc > 1:
                    src = src.rearrange("(kc p) m -> p kc m", p=128)
                    dst = wsb[:, off:off + kc, :mbs]
                else:
                    dst = wsb[:kp, off, :mbs]
                items.append(dict(out=dst, in_=src))
                off += kc
            seq.dmas(load_eng(nc, pairs[0][0].dtype, cdt), items)
            for n0 in range(0, N, nt):
                ns = min(nt, N - n0)
                items = []
                off = 0
                for (l, r), K, kc, kp in zip(pairs, Ks, kcs, kps):
                    src = r[:, n0:n0 + ns]
                    if kc > 1:
                        src = src.rearrange("(kc p) n -> p kc n", p=128)
                        dst = xsb[:, off:off + kc, :ns]
                    else:
                        dst = xsb[:kp, off, :ns]
                    items.append(dict(out=dst, in_=src))
                    off += kc
                seq.dmas(load_eng(nc, pairs[0][1].dtype, cdt), items)
                nmc = (mbs + 127) // 128
                steps = []
                off = 0
                for kc, kp in zip(kcs, kps):
                    for k in range(kc):
                        steps.append((off + k, kp))
                    off += kc

                def pe():
                    last = None
                    for mi in range(nmc):
                        ms = min(128, mbs - mi * 128)
                        for si, (kk, kp) in enumerate(steps):
                            last = nc.tensor.matmul(ps[mi][:ms, :ns], wsb[:kp, kk, mi * 128:mi * 128 + ms],
                                                    xsb[:kp, kk, :ns], start=(si == 0), stop=(si == len(steps) - 1))
                    return last

                seq.group(nc.tensor, pe)

                def ev():
                    last = None
                    for mi in range(nmc):
                        ms = min(128, mbs - mi * 128)
                        gm = (m0 // 128) + mi
                        sc = 1.0 if scale is None else scale(gm, n0)
                        if isinstance(sc, float):
                            last = nc.scalar.activation(out=osb[:ms, mi, :ns], in_=ps[mi][:ms, :ns], func=func, scale=sc)
                        else:
                            last = nc.scalar.activation(out=osb[:ms, mi, :ns], in_=ps[mi][:ms, :ns], func=func,
                                                        scale=sc[:ms])
                    return last

                seq.group(nc.scalar, ev)
                dst = out_hbm[m0:m0 + mbs, n0:n0 + ns]
                if nmc > 1:
                    dst = dst.rearrange("(mc p) n -> p mc n", p=128)
                    srcs = osb[:, :nmc, :ns]
                else:
                    srcs = osb[:mbs, 0, :ns]
                if accumulate:
                    seq.dmas(nc.gpsimd, [dict(out=dst, in_=srcs, accum_op=ALU.add)])
                else:
                    seq.dmas(nc.sync, [dict(out=dst, in_=srcs)])


class Pipe:
    def __init__(self, nc):
        self.sW = nc.alloc_semaphore("pW")
        self.vW = 0
        self.sW2 = nc.alloc_semaphore("pW2")
        self.vW2 = 0
        self.sL = [nc.alloc_semaphore(f"pL{i}") for i in range(2)]
        self.vL = [0, 0]
        self.sPE = nc.alloc_semaphore("pPE")
        self.vPE = 0
        self.sEV = nc.alloc_semaphore("pEV")
        self.vEV = 0
        self.sST = [nc.alloc_semaphore(f"pST{i}") for i in range(2)]
        self.vST = [0, 0]


def mm2(nc, seq, pipe, out_hbm, pairs, *, scale=None, func=None, accumulate=False, nt=512):
    M, N = out_hbm.shape
    func = func or AF.Identity
    cdt = BF16
    Ks = [l.shape[0] for l, r in pairs]
    kcs = [max(1, K // 128) for K in Ks]
    kps = [min(K, 128) for K in Ks]
    tot_kc = sum(kcs)
    mb = 1024
    while tot_kc * mb * 2 > 64 * 1024 and mb > 128:
        mb //= 2
    mb = min(mb, M)
    nt = min(nt, N)
    while tot_kc * nt * 2 > 32 * 1024 and nt > 128:
        nt //= 2
    mcb = max(1, (mb + 127) // 128)
    hb = 4 if mcb > 4 else mcb
    eW = load_eng(nc, pairs[0][0].dtype, cdt)
    eL = load_eng(nc, pairs[0][1].dtype, cdt)
    eS = nc.gpsimd if (accumulate or (eL is nc.sync and eW is nc.sync)) else nc.sync
    if accumulate:
        assert eS is nc.gpsimd
    start_tok = seq.prev
    steps_kk = []
    off = 0
    for kc, kp in zip(kcs, kps):
        for k in range(kc):
            steps_kk.append((off + k, kp))
        off += kc
    with ExitStack() as es:
        wsb = es.enter_context(nc.sbuf_tensor(uid("m2w"), [128, tot_kc, mb], cdt))
        xsb = [es.enter_context(nc.sbuf_tensor(uid("m2x"), [128, tot_kc, nt], cdt)) for _ in range(2)]
        osb = [es.enter_context(nc.sbuf_tensor(uid("m2o"), [128, mcb, nt], out_hbm.dtype)) for _ in range(2)]
        psets = [[es.enter_context(nc.psum_tensor(uid("m2p"), [128, 512], F32)) for _ in range(hb)] for _ in range(2)]
        blocks = [(m0, min(mb, M - m0)) for m0 in range(0, M, mb)]
        ntiles = [(n0, min(nt, N - n0)) for n0 in range(0, N, nt)]
        steps = [(bi, ni) for bi in range(len(blocks)) for ni in range(len(ntiles))]
        pe_after_step = {}
        st_after_step = {}
        set_free = [pipe.vEV, pipe.vEV]
        w_ready = None

        def chained_dmas(eng, items, sem, val):
            for kw in items:
                eng.dma_start(**kw).then_inc(sem, 16)
                val += 16
                if eng is nc.gpsimd and len(items) > 1:
                    eng.wait_ge(sem, val)
            return val

        def emit_L(si):
            bi, ni = steps[si]
            n0, ns = ntiles[ni]
            p = si % 2
            if si >= 2:
                eL.wait_ge(pipe.sPE, pe_after_step[si - 2])
            elif start_tok is not None:
                eL.wait_ge(start_tok[0], start_tok[1])
            items = []
            off = 0
            for (l, r), kc, kp in zip(pairs, kcs, kps):
                src = r[:, n0:n0 + ns]
                if kc > 1:
                    src = src.rearrange("(kc p) n -> p kc n", p=128)
                    dst = xsb[p][:, off:off + kc, :ns]
                else:
                    dst = xsb[p][:kp, off, :ns]
                items.append(dict(out=dst, in_=src))
                off += kc
            pipe.vL[p] = chained_dmas(eL, items, pipe.sL[p], pipe.vL[p])
            return pipe.vL[p]

        l_val = {}
        for si in range(min(2, len(steps))):
            if si == 0 or steps[si][0] == 0 or True:
                l_val[si] = None
        def emit_W(bi, half, wait_tok):
            m0, mbs = blocks[bi]
            if half is None:
                c_lo, c_hi = 0, mbs
            elif half == 0:
                c_lo, c_hi = 0, min(512, mbs)
            else:
                c_lo, c_hi = 512, mbs
            if wait_tok is not None:
                eW.wait_ge(wait_tok[0], wait_tok[1])
            items = []
            off = 0
            for (l, r), kc, kp in zip(pairs, kcs, kps):
                src = l[:, m0 + c_lo:m0 + c_hi]
                if kc > 1:
                    src = src.rearrange("(kc p) m -> p kc m", p=128)
                    dst = wsb[:, off:off + kc, c_lo:c_hi]
                else:
                    dst = wsb[:kp, off, c_lo:c_hi]
                items.append(dict(out=dst, in_=src))
                off += kc
            if half == 1:
                pipe.vW2 = chained_dmas(eW, items, pipe.sW2, pipe.vW2)
            else:
                pipe.vW = chained_dmas(eW, items, pipe.sW, pipe.vW)

        def split_block(bi):
            return blocks[bi][1] > 512

        w_ready = [None, None]
        nxt_ready = [None, None]
        last_step_of_block = {}
        for si_, (bi_, ni_) in enumerate(steps):
            last_step_of_block[bi_] = si_
        cur_block = -1
        for si, (bi, ni) in enumerate(steps):
            m0, mbs = blocks[bi]
            n0, ns = ntiles[ni]
            p = si % 2
            nmc = (mbs + 127) // 128
            if bi != cur_block:
                cur_block = bi
                if si == 0:
                    if split_block(bi):
                        emit_W(bi, 0, start_tok)
                        w_ready[0] = (pipe.sW, pipe.vW)
                        emit_W(bi, 1, None)
                        w_ready[1] = (pipe.sW2, pipe.vW2)
                    else:
                        emit_W(bi, None, start_tok)
                        w_ready[0] = w_ready[1] = (pipe.sW, pipe.vW)
                elif not _CTX.get("w_prefetched", False):
                    emit_W(bi, None, (pipe.sPE, pe_after_step[si - 1]))
                    w_ready[0] = w_ready[1] = (pipe.sW, pipe.vW)
                else:
                    w_ready[0], w_ready[1] = nxt_ready[0], nxt_ready[1]
                _CTX["w_prefetched"] = False
            if si == 0:
                l_val[0] = emit_L(0)
                if len(steps) > 1:
                    l_val[1] = emit_L(1)
            halves = [(0, 0, min(4, nmc)), (1, 4, nmc)] if nmc > 4 else [(si % 2, 0, nmc)]
            ev_last = None
            for (st_i, c0, c1) in halves:
                nc.tensor.wait_ge(pipe.sL[p], l_val[si])
                wr = w_ready[0 if c0 == 0 else 1]
                nc.tensor.wait_ge(wr[0], wr[1])
                if len(halves) == 1 and w_ready[1] is not w_ready[0]:
                    nc.tensor.wait_ge(w_ready[1][0], w_ready[1][1])
                nc.tensor.wait_ge(pipe.sEV, set_free[st_i])
                last = None
                for mi in range(c0, c1):
                    ms = min(128, mbs - mi * 128)
                    for k_i, (kk, kp) in enumerate(steps_kk):
                        last = nc.tensor.matmul(psets[st_i][mi - c0][:ms, :ns], wsb[:kp, kk, mi * 128:mi * 128 + ms],
                                                xsb[p][:kp, kk, :ns], start=(k_i == 0), stop=(k_i == len(steps_kk) - 1))
                last.then_inc(pipe.sPE, 1)
                pipe.vPE += 1
                pe_val = pipe.vPE
                if (si == last_step_of_block[bi] and bi + 1 < len(blocks) and len(halves) == 2 and split_block(bi + 1)):
                    hsel = 0 if c0 == 0 else 1
                    emit_W(bi + 1, hsel, (pipe.sPE, pe_val))
                    nxt_ready[hsel] = (pipe.sW, pipe.vW) if hsel == 0 else (pipe.sW2, pipe.vW2)
                    if hsel == 1:
                        _CTX["w_prefetched"] = True
                nc.scalar.wait_ge(pipe.sPE, pe_val)
                if si >= 2 and c0 == 0:
                    pp, vv = st_after_step[si - 2]
                    nc.scalar.wait_ge(pipe.sST[pp], vv)
                last = None
                for mi in range(c0, c1):
                    ms = min(128, mbs - mi * 128)
                    gm = (m0 // 128) + mi
                    sc = 1.0 if scale is None else scale(gm, n0)
                    if isinstance(sc, float):
                        last = nc.scalar.activation(out=osb[p][:ms, mi, :ns], in_=psets[st_i][mi - c0][:ms, :ns], func=func, scale=sc)
                    else:
                        last = nc.scalar.activation(out=osb[p][:ms, mi, :ns], in_=psets[st_i][mi - c0][:ms, :ns], func=func,
                                                    scale=sc[:ms])
                last.then_inc(pipe.sEV, 1)
                pipe.vEV += 1
                set_free[st_i] = pipe.vEV
                ev_last = pipe.vEV
            pe_after_step[si] = pipe.vPE
            if si + 2 < len(steps):
                l_val[si + 2] = emit_L(si + 2)
            eS.wait_ge(pipe.sEV, ev_last)
            dst = out_hbm[m0:m0 + mbs, n0:n0 + ns]
            if nmc > 1:
                dst = dst.rearrange("(mc p) n -> p mc n", p=128)
                srcs = osb[p][:, :nmc, :ns]
            else:
                srcs = osb[p][:mbs, 0, :ns]
            if accumulate:
                eS.dma_start(out=dst, in_=srcs, accum_op=ALU.add).then_inc(pipe.sST[p], 16)
            else:
                eS.dma_start(out=dst, in_=srcs).then_inc(pipe.sST[p], 16)
            pipe.vST[p] += 16
            st_after_step[si] = (p, pipe.vST[p])
        nc.vector.wait_ge(pipe.sST[0], pipe.vST[0])
        nc.vector.wait_ge(pipe.sST[1], pipe.vST[1])
        jt = es.enter_context(nc.sbuf_tensor(uid("m2j"), [128, 1], F32))
        seq.prev = None
        seq.group(nc.vector, lambda: nc.vector.memset(jt[:], 0.0))


_CTX = {}


def mmp(nc, seq, out_hbm, pairs, **kw):
    return mm2(nc, seq, _CTX["pipe"], out_hbm, pairs, **kw)


def lanes(nc, seq, make_bufs, items, body, nl=2):
    T0 = seq.prev
    lseqs = _CTX["lane_seqs"][:nl]
    with ExitStack() as es:
        bufs = [make_bufs(es, l) for l in range(nl)]
        T1 = seq.prev
        for ls in lseqs:
            ls.prev = T1
        pending = list(items)
        gens = [None] * nl
        while True:
            progressed = False
            for l in range(nl):
                if gens[l] is None and pending:
                    gens[l] = body(lseqs[l], bufs[l], pending.pop(0))
                if gens[l] is not None:
                    progressed = True
                    try:
                        next(gens[l])
                    except StopIteration:
                        gens[l] = None
            if not progressed:
                break
        for ls in lseqs:
            if ls.prev is not None:
                nc.vector.wait_ge(ls.prev[0], ls.prev[1])
        jt = es.enter_context(nc.sbuf_tensor(uid("lj"), [128, 1], F32))
        seq.prev = None
        seq.group(nc.vector, lambda: nc.vector.memset(jt[:], 0.0))


class Cfg:
    def __init__(self, D=4096, T=4096, TC=256, depth=4):
        self.D, self.T, self.TC, self.depth = D, T, TC, depth
        self.N = T + TC
        self.E = 2 * D
        self.NH = 8
        self.DH = self.E // 8
        self.GW = self.E // 4
        self.DC = D // 128
        self.EC = self.E // 128
        self.n_a = len(range(0, depth, 3))
        self.n_b = len(range(1, depth, 3))
        self.n_c = len(range(2, depth, 3))


def host_consts(cfg):
    T, TC, D, GW, N = cfg.T, cfg.TC, cfg.D, cfg.GW, cfg.N
    bf = ml_dtypes.bfloat16
    c = {}

    def dft(n):
        k = np.arange(n, dtype=np.float64)
        ang = 2.0 * np.pi * ((k[:, None] * k[None, :]) % n) / n
        return np.cos(ang) / math.sqrt(n), np.sin(ang) / math.sqrt(n)

    cc, sc = dft(GW)
    c["k_cc"], c["k_sc"] = cc.astype(bf), sc.astype(bf)
    ct, st = dft(T)
    c["k_ct"], c["k_nst"] = ct.astype(bf), (-st).astype(bf)
    ctc, stc = dft(TC)
    c["k_ctc"], c["k_nstc"] = ctc.astype(bf), (-stc).astype(bf)
    inv = np.zeros((4, N), np.float32)
    for g, w in enumerate(POOL_WINDOWS):
        lo = w // 2
        hi = w - 1 - lo
        for (o, n) in ((0, T), (T, TC)):
            pos = np.arange(n)
            a = np.clip(pos - lo, 0, n)
            b = np.clip(pos + hi + 1, 0, n)
            inv[g, o:o + n] = 1.0 / (b - a)
    c["k_invc"] = np.ascontiguousarray(np.broadcast_to(inv[:, None, :], (4, 128, N))).astype(np.float32)
    s = np.arange(128)[:, None]
    t = np.arange(256)[None, :]
    m = np.zeros((2, 2, 128, 256), np.float32)
    for half in range(2):
        m[0, half] = np.where(s + 128 * half <= t, 0.0, NEG)
        m[1, half] = np.where(s + 128 * half >= t, 0.0, NEG)
    c["k_mask"] = np.ascontiguousarray(m.reshape(4, 128, 256).transpose(1, 0, 2))
    c["k_ident"] = np.eye(128, dtype=np.float32)
    sel = np.zeros((8, 8, 128), np.float32)
    for n in range(8):
        sel[n, n, :] = 1.0
    c["k_sel"] = sel
    rows = T // 64
    r = np.repeat(np.arange(rows, dtype=np.float32), 64)
    col = np.tile(np.arange(64, dtype=np.float32), rows)
    quarter = D // 4
    omega = (1.0 / (np.float32(10000.0) ** (np.arange(quarter, dtype=np.float32) / np.float32(quarter)))).astype(np.float32)

    def axis_emb(p):
        a = (p[:, None] * omega[None, :]).astype(np.float32)
        return np.concatenate([np.sin(a), np.cos(a)], axis=-1)

    pe = np.concatenate([axis_emb(r), axis_emb(col)], axis=-1).astype(np.float32)
    c["k_posT"] = np.ascontiguousarray(pe.T)
    return c


WEIGHT_NAMES = ["ada_w", "ada_b", "norm_g", "final_g",
                "a_w_in", "a_conv_w", "a_conv_b", "a_wq", "a_wk", "a_wv", "a_w_ig", "a_b_ig", "a_w_fg", "a_b_fg",
                "a_hnorm_w", "a_skip", "a_w_out", "b_w_in", "b_w_grp", "b_scale", "b_w_out",
                "c_w_in", "c_w_grp", "c_w_out"]


def build(cfg, shapes, const_shapes, debug=()):
    D, T, TC, N, E, NH, DH, GW, DC, EC = cfg.D, cfg.T, cfg.TC, cfg.N, cfg.E, cfg.NH, cfg.DH, cfg.GW, cfg.DC, cfg.EC
    nc = bass.Bass("TRN2", target_bir_lowering=False)
    seq = Seq(nc)
    _CTX["pipe"] = Pipe(nc)
    _CTX["att"] = None
    _CTX["lane_seqs"] = [Seq(nc, limit=120000, tag=f"lane{l}_") for l in range(3)]
    I = {}
    for name, (shp, dt) in shapes.items():
        I[name] = nc.dram_tensor(name, list(shp), dt, kind="ExternalInput").ap()
    for name, (shp, dt) in const_shapes.items():
        I[name] = nc.dram_tensor(name, list(shp), dt, kind="ExternalInput").ap()
    yT = nc.dram_tensor("yT", [D, T], F32, kind="ExternalOutput").ap()

    def scratch(name, shape, dt):
        kind = "ExternalOutput" if name in debug else "Internal"
        return nc.dram_tensor(name, list(shape), dt, kind=kind)

    X_h = scratch("X", [D, N], F32)
    X = X_h.ap()
    H = scratch("H", [D, N], BF16).ap()
    U = scratch("U", [E, N], F32).ap()
    Z = scratch("Z", [E, N], F32).ap()
    G = scratch("G", [E, N], BF16).ap()
    P = scratch("P", [E, N], F32).ap()
    XC = scratch("XC", [E, N], F32).ap()
    DL = scratch("DL", [E, N], BF16).ap()
    QT = scratch("QT", [E, N], BF16).ap()
    KT = scratch("KT", [E, N], BF16).ap()
    VT = scratch("VT", [E, N], BF16).ap()
    VTOK = scratch("VTOK", [N, E], BF16).ap()
    FA = scratch("FA", [N, E], BF16).ap()
    FB = scratch("FB", [N, E], BF16).ap()
    HD = [scratch(f"HD{z}", [N, E], F32).ap() for z in range(2)]
    BD_h = scratch("BD", [3 * EC * 128, 128], F32)
    BD = BD_h.ap()
    WG = scratch("WG", [3 * E, 32], F32).ap()
    GP = scratch("GP", [32, N], F32).ap()
    ST = scratch("ST", [D, 2], F32).ap()
    BZ = scratch("BZ", [2, 8, N], F32).ap()
    LMB = scratch("LMB", [2, 8, N], F32).ap()
    MODT = scratch("MODT", [3 * D, 2], F32).ap()

    with ExitStack() as top:
        top.enter_context(nc.allow_non_contiguous_dma(reason="small per-channel parameter vectors"))

        def sb(name, shape, dt=F32):
            return top.enter_context(nc.sbuf_tensor(uid(name), shape, dt))

        ident = sb("ident", [128, 128])
        ones_f = sb("ones_f", [128, 128])
        ones_b = sb("ones_b", [128, 8], BF16)
        eps_t = sb("eps_t", [128, 1])
        one_t = sb("one_t", [128, 1])
        selT = sb("selT", [8, 8, 128])
        normA = [sb(f"normA{i}", [128, DC, 2]) for i in range(cfg.depth)]
        normB = [sb(f"normB{i}", [128, DC, 2]) for i in range(cfg.depth)]
        gateS = [sb(f"gateS{i}", [128, DC, 2]) for i in range(cfg.depth)]
        finA = sb("finA", [128, DC])
        zeroB = sb("zeroB", [128, DC])

        def g0():
            nc.vector.memset(ones_f[:], 1.0)
            nc.vector.memset(ones_b[:], 1.0)
            nc.vector.memset(eps_t[:], EPS)
            nc.vector.memset(zeroB[:], 0.0)
            return nc.vector.memset(one_t[:], 1.0)

        seq.group(nc.vector, g0)
        seq.dmas(nc.sync, [dict(out=ident[:], in_=I["k_ident"]), dict(out=selT[:], in_=I["k_sel"]),
                           dict(out=finA[:], in_=I["final_g"].rearrange("(c p) -> p c", p=128))])

        with ExitStack() as es:
            xt = es.enter_context(nc.sbuf_tensor(uid("xi"), [128, N], F32))
            pt = es.enter_context(nc.sbuf_tensor(uid("pi"), [128, T], F32))
            for dc in range(DC):
                rs = slice(dc * 128, (dc + 1) * 128)
                seq.dmas(nc.sync, [dict(out=xt[:], in_=I["xT"][rs, :]), dict(out=pt[:], in_=I["k_posT"][rs, :])])
                seq.group(nc.vector, lambda: nc.vector.tensor_tensor(out=xt[:, :T], in0=xt[:, :T], in1=pt[:], op=ALU.add))
                seq.dmas(nc.sync, [dict(out=X[rs, :], in_=xt[:])])

        with ExitStack() as es:
            cs = es.enter_context(nc.sbuf_tensor(uid("cs"), [128, DC, 2], F32))
            md = es.enter_context(nc.sbuf_tensor(uid("md"), [128, 3 * DC, 2], F32))
            ab = es.enter_context(nc.sbuf_tensor(uid("ab"), [128, 3 * DC], F32))
            gg = es.enter_context(nc.sbuf_tensor(uid("gg"), [128, DC], F32))
            t1 = es.enter_context(nc.sbuf_tensor(uid("t1"), [128, DC, 2], F32))
            seq.dmas(nc.sync, [dict(out=cs[:], in_=I["ccT"].rearrange("(c p) r -> p c r", p=128))])
            seq.group(nc.scalar, lambda: nc.scalar.activation(out=cs[:], in_=cs[:], func=AF.Silu))
            seq.dmas(nc.sync, [dict(out=ST.rearrange("(c p) r -> p c r", p=128), in_=cs[:])])
            for i in range(cfg.depth):
                mmp(nc, seq, MODT, [(I["ada_w"][i], ST)])
                seq.dmas(nc.sync, [dict(out=md[:], in_=MODT.rearrange("(c p) r -> p c r", p=128)),
                                   dict(out=ab[:], in_=I["ada_b"][i].rearrange("(c p) -> p c", p=128)),
                                   dict(out=gg[:], in_=I["norm_g"][i].rearrange("(c p) -> p c", p=128))])

                def f1():
                    last = None
                    for r in range(2):
                        last = nc.vector.tensor_tensor(out=md[:, :, r], in0=md[:, :, r], in1=ab[:], op=ALU.add)
                    return last

                seq.group(nc.vector, f1)

                def f2():
                    nc.vector.tensor_copy(out=normB[i][:], in_=md[:, 0:DC, :])
                    nc.vector.tensor_copy(out=gateS[i][:], in_=md[:, 2 * DC:3 * DC, :])
                    return nc.vector.tensor_scalar(out=t1[:], in0=md[:, DC:2 * DC, :], scalar1=1.0, scalar2=None, op0=ALU.add)

                seq.group(nc.vector, f2)

                def f3():
                    last = None
                    for r in range(2):
                        last = nc.vector.tensor_tensor(out=normA[i][:, :, r], in0=t1[:, :, r], in1=gg[:], op=ALU.mult)
                    return last

                seq.group(nc.vector, f3)

        def norm_stage(A_of, B_of, out_hbm, out_dt, ncols):
            NT = 128

            def mk(es, l):
                return dict(xt=es.enter_context(nc.sbuf_tensor(uid("nx"), [128, DC, NT], F32)),
                            sq=es.enter_context(nc.sbuf_tensor(uid("nsq"), [128, DC, NT], F32)),
                            ho=es.enter_context(nc.sbuf_tensor(uid("nh"), [128, DC, NT], out_dt)),
                            rs=es.enter_context(nc.sbuf_tensor(uid("nrs"), [128, NT], F32)),
                            pss=es.enter_context(nc.psum_tensor(uid("nps"), [128, 512], F32)))

            def body(ls, Bf, n0):
                xt, sq, ho, rs, pss = Bf["xt"], Bf["sq"], Bf["ho"], Bf["rs"], Bf["pss"]
                ns = min(NT, ncols - n0)
                r = 0 if n0 < T else 1
                ls.dmas(nc.sync, [dict(out=xt[:, :, :ns], in_=X[:, n0:n0 + ns].rearrange("(c p) n -> p c n", p=128))])
                yield
                ls.group(nc.scalar, lambda: nc.scalar.activation(out=sq[:, :, :ns], in_=xt[:, :, :ns], func=AF.Square))
                yield

                def pe():
                    last = None
                    for c in range(DC):
                        last = nc.tensor.matmul(pss[:, :ns], ones_f[:], sq[:, c, :ns], start=(c == 0), stop=(c == DC - 1))
                    return last

                ls.group(nc.tensor, pe)
                yield
                ls.group(nc.scalar, lambda: nc.scalar.activation(out=rs[:, :ns], in_=pss[:, :ns], func=AF.Sqrt,
                                                                 bias=eps_t[:], scale=1.0 / D))
                yield
                ls.group(nc.vector, lambda: nc.vector.reciprocal(out=rs[:, :ns], in_=rs[:, :ns]))
                yield

                def f1():
                    last = None
                    for c in range(DC):
                        last = nc.vector.tensor_tensor(out=sq[:, c, :ns], in0=xt[:, c, :ns], in1=rs[:, :ns], op=ALU.mult)
                    return last

                ls.group(nc.vector, f1)
                yield

                def f2():
                    last = None
                    for c in range(DC):
                        last = nc.scalar.activation(out=ho[:, c, :ns], in_=sq[:, c, :ns], func=AF.Identity,
                                                    scale=A_of(r)[:, c:c + 1], bias=B_of(r)[:, c:c + 1])
                    return last

                ls.group(nc.scalar, f2)
                yield
                ls.dmas(nc.sync, [dict(out=out_hbm[:, n0:n0 + ns].rearrange("(c p) n -> p c n", p=128), in_=ho[:, :, :ns])])
                yield

            lanes(nc, seq, mk, list(range(0, ncols, NT)), body, nl=3)

        def gate_stage(scale_hbm):
            with ExitStack() as es0:
                scs = es0.enter_context(nc.sbuf_tensor(uid("gs"), [128, EC], F32))
                if scale_hbm is not None:
                    seq.dmas(nc.sync, [dict(out=scs[:], in_=scale_hbm.rearrange("(c p) -> p c", p=128))])
                else:
                    seq.group(nc.vector, lambda: nc.vector.memset(scs[:], 1.0))

                def mk(es, l):
                    return dict(pt=es.enter_context(nc.sbuf_tensor(uid("gp"), [128, N], F32)),
                                zt=es.enter_context(nc.sbuf_tensor(uid("gz"), [128, N], F32)),
                                go=es.enter_context(nc.sbuf_tensor(uid("go"), [128, N], BF16)))

                def body(ls, Bf, ec):
                    pt_, zt, go = Bf["pt"], Bf["zt"], Bf["go"]
                    rs_ = slice(ec * 128, (ec + 1) * 128)
                    ls.dmas(nc.sync, [dict(out=pt_[:], in_=P[rs_, :]), dict(out=zt[:], in_=Z[rs_, :])])
                    yield
                    ls.group(nc.scalar, lambda: nc.scalar.activation(out=zt[:], in_=zt[:], func=AF.Silu))
                    yield
                    ls.group(nc.vector, lambda: nc.vector.scalar_tensor_tensor(out=go[:], in0=pt_[:], scalar=scs[:, ec:ec + 1],
                                                                               in1=zt[:], op0=ALU.mult, op1=ALU.mult))
                    yield
                    ls.dmas(nc.sync, [dict(out=G[rs_, :], in_=go[:])])
                    yield

                lanes(nc, seq, mk, list(range(EC)), body, nl=3)

        def wout_stage(i, w_out):
            def sc(gm, n0):
                return gateS[i][:, gm, (0 if n0 < T else 1):(1 if n0 < T else 2)]
            mmp(nc, seq, X, [(w_out, G)], scale=sc, accumulate=True)

        for i in range(cfg.depth):
            kind, j = i % 3, i // 3
            norm_stage(lambda r: normA[i][:, :, r], lambda r: normB[i][:, :, r], H, BF16, N)
            w_in = (I["a_w_in"], I["b_w_in"], I["c_w_in"])[kind][j]
            mmp(nc, seq, U, [(w_in[:, 0:E], H)])
            mmp(nc, seq, Z, [(w_in[:, E:2 * E], H)])
            if kind == 1:
                pool_layer(nc, seq, cfg, I, j, U, DL, P)
                gate_stage(I["b_scale"][j])
                wout_stage(i, I["b_w_out"][j])
            elif kind == 2:
                fourier_layer(nc, seq, cfg, I, j, U, FA, FB, DL, P)
                gate_stage(None)
                wout_stage(i, I["c_w_out"][j])
            else:
                mlstm_layer(nc, seq, cfg, I, j, dict(UZ=U, Z=Z, XC=XC, QT=QT, KT=KT, VT=VT, VTOK=VTOK, HD=HD, BD=BD, BD_h=BD_h,
                                                     WG=WG, GP=GP, HN=P, G=G, ident=ident, ones_b=ones_b, eps_t=eps_t,
                                                     one_t=one_t, selT=selT, BZ=BZ, LMB=LMB))
                wout_stage(i, I["a_w_out"][j])

        norm_stage(lambda r: finA[:], lambda r: zeroB[:], yT, F32, T)
        nc.sync.wait_ge(seq.prev[0], seq.prev[1])
    nc._n_seq_sems = seq.n
    return nc


def pool_layer(nc, seq, cfg, I, j, UZ, DL, P):
    T, TC, N, E, GW, EC = cfg.T, cfg.TC, cfg.N, cfg.E, cfg.GW, cfg.EC
    PAD = 16
    segs = ((0, T), (T, TC))
    with ExitStack() as es0:
        invc = es0.enter_context(nc.sbuf_tensor(uid("pinv"), [128, N], F32))
        for g in range(4):
            w = POOL_WINDOWS[g]
            lo = w // 2
            hi = w - 1 - lo
            seq.dmas(nc.sync, [dict(out=invc[:], in_=I["k_invc"][g])])

            def mk(es, l):
                bufs = []
                for si, (o, n) in enumerate(segs):
                    bufs.append(tuple(es.enter_context(nc.sbuf_tensor(uid("pb"), [128, n + 2 * PAD], F32)) for _ in range(3)))
                do = es.enter_context(nc.sbuf_tensor(uid("pdo"), [128, N], BF16))

                def z0():
                    last = None
                    for tri in bufs:
                        for t_ in tri:
                            last = nc.vector.memset(t_[:], 0.0)
                    return last

                seq.group(nc.vector, z0)
                return dict(bufs=bufs, do=do)

            def body(ls, Bf, ec, w=w, hi=hi):
                bufs, do = Bf["bufs"], Bf["do"]
                rs_ = slice(ec * 128, (ec + 1) * 128)
                ls.dmas(nc.sync, [dict(out=bufs[si][0][:, PAD:PAD + n], in_=UZ[rs_, o:o + n]) for si, (o, n) in enumerate(segs)])
                yield
                cov = 1
                cur = 0
                while cov < w:
                    nxt = 1 if cur != 1 else 2

                    def stp(cur=cur, nxt=nxt, cov=cov):
                        last = None
                        for si, (o, n) in enumerate(segs):
                            L = n + 2 * PAD
                            last = nc.vector.tensor_tensor(out=bufs[si][nxt][:, cov:L], in0=bufs[si][cur][:, cov:L],
                                                           in1=bufs[si][cur][:, 0:L - cov], op=ALU.add)
                        return last

                    ls.group(nc.vector, stp)
                    yield
                    cur = nxt
                    cov *= 2
                tmpi = 1 if cur != 1 else 2

                def m1(cur=cur, tmpi=tmpi):
                    last = None
                    for si, (o, n) in enumerate(segs):
                        last = nc.vector.tensor_tensor(out=bufs[si][tmpi][:, PAD:PAD + n], in0=bufs[si][cur][:, PAD + hi:PAD + hi + n],
                                                       in1=invc[:, o:o + n], op=ALU.mult)
                    return last

                ls.group(nc.vector, m1)
                yield

                def m2(tmpi=tmpi):
                    last = None
                    for si, (o, n) in enumerate(segs):
                        last = nc.vector.tensor_tensor(out=do[:, o:o + n], in0=bufs[si][tmpi][:, PAD:PAD + n],
                                                       in1=bufs[si][0][:, PAD:PAD + n], op=ALU.subtract)
                    return last

                ls.group(nc.vector, m2)
                yield
                ls.dmas(nc.sync, [dict(out=DL[rs_, :], in_=do[:])])
                yield

            cpg = GW // 128
            lanes(nc, seq, mk, list(range(g * cpg, (g + 1) * cpg)), body)
    for g in range(4):
        mmp(nc, seq, P[g * GW:(g + 1) * GW, :], [(I["b_w_grp"][j, g], DL[g * GW:(g + 1) * GW, :])])


def fourier_layer(nc, seq, cfg, I, j, UZ, FA, FB, DL, P):
    T, TC, N, E, GW = cfg.T, cfg.TC, cfg.N, cfg.E, cfg.GW
    for g in range(4):
        gs = slice(g * GW, (g + 1) * GW)
        mmp(nc, seq, FA[:, gs], [(UZ[gs, :], I["k_cc"])])
        mmp(nc, seq, FB[:, gs], [(UZ[gs, :], I["k_sc"])])
    mmp(nc, seq, DL[:, 0:T], [(FA[0:T, :], I["k_ct"]), (FB[0:T, :], I["k_nst"])])
    mmp(nc, seq, DL[:, T:N], [(FA[T:N, :], I["k_ctc"]), (FB[T:N, :], I["k_nstc"])])
    for g in range(4):
        gs = slice(g * GW, (g + 1) * GW)
        mmp(nc, seq, P[gs, :], [(I["c_w_grp"][j, g], DL[gs, :])])


def mlstm_layer(nc, seq, cfg, I, j, S):
    T, TC, N, E, NH, DH, EC = cfg.T, cfg.TC, cfg.N, cfg.E, cfg.NH, cfg.DH, cfg.EC
    UZ, XC, QT, KT, VT, VTOK, HD, BD, WG, GP, HN, G = (S[k] for k in ("UZ", "XC", "QT", "KT", "VT", "VTOK", "HD", "BD", "WG", "GP", "HN", "G"))
    Z = S["Z"]
    ident, ones_b, eps_t, one_t, selT = S["ident"], S["ones_b"], S["eps_t"], S["one_t"], S["selT"]
    segs = ((0, T), (T, TC))
    NTC = N // 128
    DCH = DH // 128

    with ExitStack() as es0:
        cw = es0.enter_context(nc.sbuf_tensor(uid("cw"), [128, 4, EC], F32))
        cb = es0.enter_context(nc.sbuf_tensor(uid("cb"), [128, EC], F32))
        seq.dmas(nc.sync, [dict(out=cw[:, t_, :], in_=I["a_conv_w"][j, t_].rearrange("(c p) -> p c", p=128)) for t_ in range(4)]
                 + [dict(out=cb[:], in_=I["a_conv_b"][j].rearrange("(c p) -> p c", p=128))])

        def mk(es, l):
            ub = [es.enter_context(nc.sbuf_tensor(uid("cu"), [128, n + 3], F32)) for (o, n) in segs]
            acc = [es.enter_context(nc.sbuf_tensor(uid("ca"), [128, N], F32)) for _ in range(2)]

            def z0():
                nc.vector.memset(ub[0][:], 0.0)
                return nc.vector.memset(ub[1][:], 0.0)

            seq.group(nc.vector, z0)
            return dict(ub=ub, acc=acc)

        def body(ls, Bf, ec):
            ub, acc = Bf["ub"], Bf["acc"]
            rs_ = slice(ec * 128, (ec + 1) * 128)
            ls.dmas(nc.sync, [dict(out=ub[si][:, 1:1 + n], in_=UZ[rs_, o:o + n]) for si, (o, n) in enumerate(segs)])
            yield
            for t_ in range(4):
                src, dst = acc[(t_ + 1) % 2], acc[t_ % 2]

                def stp(t_=t_, src=src, dst=dst):
                    last = None
                    for si, (o, n) in enumerate(segs):
                        if t_ == 0:
                            last = nc.vector.tensor_scalar(out=dst[:, o:o + n], in0=ub[si][:, 0:n], scalar1=cw[:, 0, ec:ec + 1],
                                                           scalar2=None, op0=ALU.mult)
                        else:
                            last = nc.vector.scalar_tensor_tensor(out=dst[:, o:o + n], in0=ub[si][:, t_:t_ + n],
                                                                  scalar=cw[:, t_, ec:ec + 1], in1=src[:, o:o + n],
                                                                  op0=ALU.mult, op1=ALU.add)
                    return last

                ls.group(nc.vector, stp)
                yield
            ls.group(nc.scalar, lambda: nc.scalar.activation(out=acc[0][:], in_=acc[1][:], func=AF.Silu, bias=cb[:, ec:ec + 1], scale=1.0))
            yield
            ls.dmas(nc.sync, [dict(out=XC[rs_, :], in_=acc[0][:])])
            yield

        lanes(nc, seq, mk, list(range(EC)), body, nl=3)

    with ExitStack() as es:
        zt = es.enter_context(nc.sbuf_tensor(uid("bz"), [128, EC, 128], F32))
        seq.group(nc.vector, lambda: nc.vector.memset(zt[:], 0.0))
        seq.dmas(nc.sync, [dict(out=BD[m * EC * 128:(m + 1) * EC * 128, :].rearrange("(c p) q -> p c q", p=128), in_=zt[:]) for m in range(3)])
        items = []
        for m, wname in enumerate(("a_wq", "a_wk", "a_wv")):
            w = I[wname]
            for c in range(EC):
                dst = bass.AP(S["BD_h"], (m * EC + c) * 128 * 128, [[516, 32], [128, 4], [1, 4]])
                items.append(dict(out=dst, in_=w[j, 32 * c:32 * (c + 1), :, :]))
        for k0 in range(0, len(items), 32):
            seq.dmas(nc.sync, items[k0:k0 + 32])
    if True:
        NT = 512
        tiles = [(n0, min(NT, N - n0)) for n0 in range(0, N, NT)]
        jobs = [(m, n0, ns) for m in range(3) for (n0, ns) in tiles]

        def mk(es, l):
            return dict(xcb=es.enter_context(nc.sbuf_tensor(uid("qx"), [128, N], BF16)),
                        ubf=es.enter_context(nc.sbuf_tensor(uid("qu"), [128, N], BF16)),
                        bdb=es.enter_context(nc.sbuf_tensor(uid("qb"), [128, 3, 128], BF16)),
                        oq=es.enter_context(nc.sbuf_tensor(uid("qo"), [128, 3, N], BF16)),
                        ov=es.enter_context(nc.sbuf_tensor(uid("qv"), [128, NTC, 128], BF16)),
                        ps=[es.enter_context(nc.psum_tensor(uid("qps"), [128, 512], F32)) for _ in range(4)])

        def body(ls, Bf, ec):
            xcb, ubf, bdb, oq, ov, ps = (Bf[k] for k in ("xcb", "ubf", "bdb", "oq", "ov", "ps"))
            rs_ = slice(ec * 128, (ec + 1) * 128)
            ls.dmas(nc.gpsimd, [dict(out=xcb[:], in_=XC[rs_, :]), dict(out=ubf[:], in_=UZ[rs_, :])]
                    + [dict(out=bdb[:, m, :], in_=BD[(m * EC + ec) * 128:(m * EC + ec + 1) * 128, :]) for m in range(3)])
            yield
            for b0 in range(0, len(jobs), 4):
                jb = jobs[b0:b0 + 4]

                def pe(jb=jb):
                    last = None
                    for bi, (m, n0, ns) in enumerate(jb):
                        src = ubf if m == 2 else xcb
                        last = nc.tensor.matmul(ps[bi][:, :ns], bdb[:, m, :], src[:, n0:n0 + ns], start=True, stop=True)
                    return last

                ls.group(nc.tensor, pe)
                yield

                def ev(jb=jb):
                    last = None
                    for bi, (m, n0, ns) in enumerate(jb):
                        last = nc.scalar.copy(out=oq[:, m, n0:n0 + ns], in_=ps[bi][:, :ns])
                    return last

                ls.group(nc.scalar, ev)
                yield
            ls.dmas(nc.sync, [dict(out=QT[rs_, :], in_=oq[:, 0, :]), dict(out=KT[rs_, :], in_=oq[:, 1, :]), dict(out=VT[rs_, :], in_=oq[:, 2, :])])
            yield
            for b0 in range(0, NTC, 16):
                tcs = list(range(b0, min(NTC, b0 + 16)))

                def pe2(tcs=tcs):
                    last = None
                    for bi, tc in enumerate(tcs):
                        last = nc.tensor.matmul(ps[bi // 4][:, (bi % 4) * 128:(bi % 4 + 1) * 128], ubf[:, tc * 128:(tc + 1) * 128],
                                                bdb[:, 2, :], start=True, stop=True)
                    return last

                ls.group(nc.tensor, pe2)
                yield

                def ev2(tcs=tcs):
                    last = None
                    for bi, tc in enumerate(tcs):
                        last = nc.scalar.copy(out=ov[:, tc, :], in_=ps[bi // 4][:, (bi % 4) * 128:(bi % 4 + 1) * 128])
                    return last

                ls.group(nc.scalar, ev2)
                yield
            ls.dmas(nc.sync, [dict(out=VTOK[:, rs_].rearrange("(tc p) e -> p tc e", p=128), in_=ov[:])])
            yield

        lanes(nc, seq, mk, list(range(EC)), body)

    items = []
    for gi, (wn, z) in enumerate((("a_w_ig", 0), ("a_w_ig", 1), ("a_w_fg", 0), ("a_w_fg", 1))):
        for r0 in range(0, 3 * E, 2048):
            r1 = min(3 * E, r0 + 2048)
            items.append(dict(out=WG[r0:r1, gi * 8:(gi + 1) * 8], in_=I[wn][j, z, r0:r1, :]))
    for k0 in range(0, len(items), 16):
        seq.dmas(nc.sync, items[k0:k0 + 16])
    mmp(nc, seq, GP, [(WG[0:E, :], QT), (WG[E:2 * E, :], KT), (WG[2 * E:3 * E, :], VT)])

    with ExitStack() as es:
        def t8(name):
            return es.enter_context(nc.sbuf_tensor(uid(name), [8, N], F32))
        Bz = [t8("Bz0"), t8("Bz1")]
        LmB = [t8("Lm0"), t8("Lm1")]
        li = [t8("li0"), t8("li1")]
        xf = [t8("xf0"), t8("xf1")]
        tmp = t8("tmp")
        onesr = t8("onesr")
        bi_ = es.enter_context(nc.sbuf_tensor(uid("bi"), [8, 2], F32))
        bf_ = es.enter_context(nc.sbuf_tensor(uid("bf"), [8, 2], F32))
        tot = es.enter_context(nc.sbuf_tensor(uid("tot"), [8, 4], F32))
        seq.dmas(nc.sync, [dict(out=li[z][:], in_=GP[8 * z:8 * z + 8, :]) for z in range(2)]
                 + [dict(out=xf[z][:], in_=GP[16 + 8 * z:24 + 8 * z, :]) for z in range(2)]
                 + [dict(out=bi_[:, z:z + 1], in_=I["a_b_ig"][j, z].rearrange("(n o) -> n o", o=1)) for z in range(2)]
                 + [dict(out=bf_[:, z:z + 1], in_=I["a_b_fg"][j, z].rearrange("(n o) -> n o", o=1)) for z in range(2)])

        def g1():
            nc.vector.memset(onesr[:], 1.0)
            return nc.vector.tensor_scalar(out=bf_[:], in0=bf_[:], scalar1=-1.0, scalar2=None, op0=ALU.mult)

        seq.group(nc.vector, g1)

        def g2():
            last = None
            for z in range(2):
                nc.scalar.activation(out=li[z][:], in_=li[z][:], func=AF.Identity, bias=bi_[:, z:z + 1], scale=1.0)
                last = nc.scalar.activation(out=xf[z][:], in_=xf[z][:], func=AF.Exp, bias=bf_[:, z:z + 1], scale=-1.0)
            return last

        seq.group(nc.scalar, g2)

        def g3():
            last = None
            for z in range(2):
                last = nc.scalar.activation(out=xf[z][:], in_=xf[z][:], func=AF.Ln, bias=S["one_t"][:8, :], scale=1.0)
            return last

        seq.group(nc.scalar, g3)
        def g4():
            last = None
            for z in range(2):
                for (o, n) in segs:
                    last = nc.vector.tensor_tensor_scan(out=Bz[z][:, o:o + n], data0=onesr[:, o:o + n], data1=xf[z][:, o:o + n],
                                                        initial=0.0, op0=ALU.mult, op1=ALU.add)
            return last

        seq.group(nc.vector, g4)
        def g5():
            nc.vector.tensor_copy(out=tot[:, 0:1], in_=Bz[0][:, N - 1:N])
            nc.vector.tensor_copy(out=tot[:, 1:2], in_=Bz[1][:, N - 1:N])
            nc.vector.tensor_copy(out=tot[:, 2:3], in_=Bz[1][:, T - 1:T])
            return nc.vector.tensor_tensor(out=tmp[:], in0=xf[1][:], in1=Bz[1][:], op=ALU.subtract)

        seq.group(nc.vector, g5)

        def g6():
            nc.vector.tensor_tensor(out=tot[:, 3:4], in0=tot[:, 2:3], in1=tot[:, 1:2], op=ALU.add)
            return nc.vector.tensor_scalar(out=Bz[0][:, 0:T], in0=Bz[0][:, 0:T], scalar1=tot[:, 0:1], scalar2=None, op0=ALU.add)

        seq.group(nc.vector, g6)

        def g7():
            nc.vector.tensor_scalar(out=Bz[1][:, 0:T], in0=tmp[:, 0:T], scalar1=tot[:, 3:4], scalar2=None, op0=ALU.add)
            nc.vector.tensor_scalar(out=Bz[1][:, T:N], in0=tmp[:, T:N], scalar1=tot[:, 1:2], scalar2=None, op0=ALU.add)
            return nc.vector.tensor_scalar(out=Bz[0][:], in0=Bz[0][:], scalar1=-1.0, scalar2=None, op0=ALU.mult)

        seq.group(nc.vector, g7)
        seq.group(nc.vector, lambda: nc.vector.tensor_scalar(out=Bz[1][:], in0=Bz[1][:], scalar1=-1.0, scalar2=None, op0=ALU.mult))

        def g8():
            last = None
            for z in range(2):
                last = nc.vector.scalar_tensor_tensor(out=LmB[z][:], in0=li[z][:], scalar=-0.5 * math.log(DH), in1=Bz[z][:],
                                                      op0=ALU.add, op1=ALU.subtract)
            return last

        seq.group(nc.vector, g8)
        seq.dmas(nc.sync, [dict(out=S["BZ"][z], in_=Bz[z][:]) for z in range(2)] + [dict(out=S["LMB"][z], in_=LmB[z][:]) for z in range(2)])

    if True:
        QW = 256
        TS = QW // 128
        VW = min(DH, 512)
        VH = DH // VW
        KTL = T // 128
        with ExitStack() as e5:
            kT = e5.enter_context(nc.sbuf_tensor(uid("akT"), [128, DCH, N], BF16))
            vk = e5.enter_context(nc.sbuf_tensor(uid("avk"), [128, NTC, DH], BF16))
            qT2 = [e5.enter_context(nc.sbuf_tensor(uid("aqT"), [128, DCH, QW], BF16)) for _ in range(2)]
            csb = e5.enter_context(nc.sbuf_tensor(uid("acs"), [128, NTC], F32))
            lmb = e5.enter_context(nc.sbuf_tensor(uid("almb"), [8, N], F32))
            bzt2 = [e5.enter_context(nc.sbuf_tensor(uid("abzt"), [8, QW], F32)) for _ in range(2)]
            brow = e5.enter_context(nc.sbuf_tensor(uid("abr"), [128, 3, QW], F32))
            mask = e5.enter_context(nc.sbuf_tensor(uid("amk"), [128, 4, 256], F32))
            esb = e5.enter_context(nc.sbuf_tensor(uid("aes"), [128, 4, QW], F32))
            wsb = e5.enter_context(nc.sbuf_tensor(uid("aws"), [128, 4, QW], BF16))
            rsb = e5.enter_context(nc.sbuf_tensor(uid("ars"), [128, TS], F32))
            hsb2 = [e5.enter_context(nc.sbuf_tensor(uid("ahs"), [128, TS, DH], F32)) for _ in range(2)]
            ps_num = [[e5.enter_context(nc.psum_tensor(uid("apn"), [128, 512], F32)) for _ in range(VH)] for _ in range(TS)]
            ps_den = e5.enter_context(nc.psum_tensor(uid("apd"), [128, 512], F32))
            ps_s = [e5.enter_context(nc.psum_tensor(uid("aps"), [128, 512], F32)) for _ in range(2)]
            ps_t = e5.enter_context(nc.psum_tensor(uid("apt"), [128, 512], F32))
            seq.dmas(nc.sync, [dict(out=mask[:], in_=I["k_mask"])])
            A = _CTX.setdefault("att", None) or dict(sS=nc.alloc_semaphore("aS"), sE=nc.alloc_semaphore("aE"), sV=nc.alloc_semaphore("aV"),
                                                     sN=nc.alloc_semaphore("aN"), vS=0, vE=0, vV=0, vN=0, gb=0, qn=0,
                                                     sLD=[nc.alloc_semaphore("aLD0"), nc.alloc_semaphore("aLD1")], vLD=[0, 0],
                                                     sST=[nc.alloc_semaphore("aST0"), nc.alloc_semaphore("aST1")], vST=[0, 0])
            _CTX["att"] = A
            for n in range(NH):
                hs_ = slice(n * DH, (n + 1) * DH)
                seq.dmas(nc.sync, [dict(out=kT[:], in_=KT[hs_, :].rearrange("(c p) t -> p c t", p=128)),
                                   dict(out=vk[:], in_=VTOK[:, hs_].rearrange("(tc p) v -> p tc v", p=128))])
                for z in range(2):
                    seq.dmas(nc.sync, [dict(out=lmb[:], in_=S["LMB"][z])])

                    def pc():
                        last = None
                        for kt in range(NTC):
                            last = nc.tensor.matmul(ps_t[:, kt:kt + 1], lmb[:, kt * 128:(kt + 1) * 128], ident[:8, n:n + 1],
                                                    start=True, stop=True)
                        return last

                    seq.group(nc.tensor, pc)
                    seq.group(nc.vector, lambda: nc.vector.tensor_copy(out=csb[:], in_=ps_t[:, :NTC]))
                    qtiles = [(q0, True) for q0 in range(0, T, QW)] + [(T, False)]
                    for qidx, (q0, is_lat) in enumerate(qtiles):
                        qpar = A["qn"] % 2
                        qT, bzt, hsb = qT2[qpar], bzt2[qpar], hsb2[qpar]
                        qi = q0 // QW if is_lat else 0
                        if is_lat:
                            full = [KTL + c for c in range(TC // 128)]
                            full += list(range(0, 2 * qi)) if z == 0 else list(range(2 * qi + 2, KTL))
                            keys = [(kt, 0) for kt in full] + [(2 * qi, 1), (2 * qi + 1, 2)]
                        else:
                            keys = [(KTL, 1), (KTL + 1, 2)]
                        def load_q(par_, q0_):
                            nc.sync.dma_start(out=qT2[par_][:], in_=QT[hs_, q0_:q0_ + QW].rearrange("(c p) t -> p c t", p=128)).then_inc(A["sLD"][par_], 16)
                            nc.sync.dma_start(out=bzt2[par_][:], in_=S["BZ"][z][:, q0_:q0_ + QW]).then_inc(A["sLD"][par_], 16)
                            A["vLD"][par_] += 32

                        if qidx == 0:
                            nc.sync.wait_ge(seq.prev[0], seq.prev[1])
                            load_q(qpar, q0)
                        ld_val = A["vLD"][qpar]

                        def brow_mm():
                            nc.tensor.wait_ge(A["sLD"][qpar], ld_val)
                            return nc.tensor.matmul(ps_t[:, :QW], selT[:, n, :], bzt[:], start=True, stop=True)

                        seq.group(nc.tensor, brow_mm)
                        if qidx + 1 < len(qtiles):
                            nc.sync.wait_ge(seq.prev[0], seq.prev[1])
                            load_q(1 - qpar, qtiles[qidx + 1][0])

                        def gb():
                            nc.vector.tensor_copy(out=brow[:, 0, :], in_=ps_t[:, :QW])
                            nc.vector.tensor_tensor(out=brow[:, 1, :], in0=ps_t[:, :QW], in1=mask[:, 2 * z, :], op=ALU.add)
                            return nc.vector.tensor_tensor(out=brow[:, 2, :], in0=ps_t[:, :QW], in1=mask[:, 2 * z + 1, :], op=ALU.add)

                        seq.group(nc.vector, gb)
                        nk = len(keys)
                        T0 = seq.prev
                        batches = [keys[b0:b0 + 2] for b0 in range(0, nk, 2)]
                        nb = len(batches)
                        sval, eval_, vval, nval = {}, {}, {}, {}

                        def emit_E(b):
                            par = (A["gb"] + b) % 2
                            if b == 0:
                                nc.scalar.wait_ge(T0[0], T0[1])
                            if b >= 2:
                                nc.scalar.wait_ge(A["sV"], vval[b - 2])
                            last = None
                            for bi, (kt, mt) in enumerate(batches[b]):
                                last = nc.scalar.activation(out=esb[:, par * 2 + bi, :], in_=brow[:, mt, :], func=AF.Exp,
                                                            bias=csb[:, kt:kt + 1], scale=1.0)
                            last.then_inc(A["sE"], 1)
                            A["vE"] += 1
                            eval_[b] = A["vE"]

                        def emit_S(b):
                            par = (A["gb"] + b) % 2
                            if b == 0:
                                nc.tensor.wait_ge(T0[0], T0[1])
                            if b >= 2:
                                nc.tensor.wait_ge(A["sV"], vval[b - 2])
                            last = None
                            for bi, (kt, mt) in enumerate(batches[b]):
                                o_ = ps_s[par][:, bi * QW:(bi + 1) * QW]
                                for dc in range(DCH):
                                    last = nc.tensor.matmul(o_, kT[:, dc, kt * 128:(kt + 1) * 128], qT[:, dc, :],
                                                            start=(dc == 0), stop=(dc == DCH - 1))
                            last.then_inc(A["sS"], 1)
                            A["vS"] += 1
                            sval[b] = A["vS"]

                        def emit_V(b):
                            par = (A["gb"] + b) % 2
                            nc.vector.wait_ge(A["sS"], sval[b])
                            nc.vector.wait_ge(A["sE"], eval_[b])
                            if b >= 2:
                                nc.vector.wait_ge(A["sN"], nval[b - 2])
                            last = None
                            for bi, (kt, mt) in enumerate(batches[b]):
                                o_ = ps_s[par][:, bi * QW:(bi + 1) * QW]
                                last = nc.vector.tensor_tensor(out=wsb[:, par * 2 + bi, :], in0=o_, in1=esb[:, par * 2 + bi, :], op=ALU.mult)
                            last.then_inc(A["sV"], 1)
                            A["vV"] += 1
                            vval[b] = A["vV"]

                        def emit_N(b):
                            par = (A["gb"] + b) % 2
                            nc.tensor.wait_ge(A["sV"], vval[b])
                            last = None
                            for bi, (kt, mt) in enumerate(batches[b]):
                                first = (b == 0 and bi == 0)
                                lastk = (b == nb - 1 and bi == len(batches[b]) - 1)
                                for ts in range(TS):
                                    lw = wsb[:, par * 2 + bi, ts * 128:(ts + 1) * 128]
                                    for vh in range(VH):
                                        nc.tensor.matmul(ps_num[ts][vh][:, :VW], lw, vk[:, kt, vh * VW:(vh + 1) * VW],
                                                         start=first, stop=lastk)
                                    last = nc.tensor.matmul(ps_den[:, ts:ts + 1], lw, ones_b[:, 0:1], start=(first and ts == 0),
                                                            stop=lastk, skip_group_check=True)
                            last.then_inc(A["sN"], 1)
                            A["vN"] += 1
                            nval[b] = A["vN"]

                        for b in range(nb):
                            emit_E(b)
                            emit_S(b)
                            emit_V(b)
                            if b >= 1:
                                emit_N(b - 1)
                        emit_N(nb - 1)
                        A["gb"] += nb
                        seq.prev = (A["sN"], A["vN"])
                        seq.group(nc.scalar, lambda: nc.scalar.activation(out=rsb[:], in_=ps_den[:, :TS], func=AF.Abs))
                        seq.group(nc.vector, lambda: nc.vector.tensor_scalar(out=rsb[:], in0=rsb[:], scalar1=1.0, scalar2=None,
                                                                             op0=ALU.max))
                        seq.group(nc.vector, lambda: nc.vector.reciprocal(out=rsb[:], in_=rsb[:]))

                        def a2():
                            last = None
                            for ts in range(TS):
                                for vh in range(VH):
                                    last = nc.scalar.activation(out=hsb[:, ts, vh * VW:(vh + 1) * VW], in_=ps_num[ts][vh][:, :VW],
                                                                func=AF.Identity, scale=rsb[:, ts:ts + 1])
                            return last

                        def a2w():
                            nc.scalar.wait_ge(A["sST"][qpar], A["vST"][qpar])
                            return a2()

                        seq.group(nc.scalar, a2w)
                        nc.sync.wait_ge(seq.prev[0], seq.prev[1])
                        nc.sync.dma_start(out=HD[z][q0:q0 + QW, hs_].rearrange("(ts p) v -> p ts v", p=128), in_=hsb[:]).then_inc(A["sST"][qpar], 16)
                        A["vST"][qpar] += 16
                        A["qn"] += 1
            nc.vector.wait_ge(A["sST"][0], A["vST"][0])
            nc.vector.wait_ge(A["sST"][1], A["vST"][1])
            seq.group(nc.vector, lambda: nc.vector.memset(rsb[:], 0.0))

    NHH = NH // 2
    ECH = EC // 2

    def mk6(es, l):
        return dict(a=es.enter_context(nc.sbuf_tensor(uid("fa"), [128, NHH, DH], F32)),
                    b=es.enter_context(nc.sbuf_tensor(uid("fb"), [128, NHH, DH], F32)),
                    c=es.enter_context(nc.sbuf_tensor(uid("fc"), [128, NHH, DH], F32)),
                    st=es.enter_context(nc.sbuf_tensor(uid("fs"), [128, 4, NHH], F32)),
                    hT=es.enter_context(nc.sbuf_tensor(uid("fT"), [128, ECH, 128], F32)),
                    ps=[es.enter_context(nc.psum_tensor(uid("fps"), [128, 512], F32)) for _ in range(4)])

    def body6(ls, Bf, item):
        tc, hh = item
        a, b, c, st, hT, ps = (Bf[k] for k in ("a", "b", "c", "st", "hT", "ps"))
        ts_ = slice(tc * 128, (tc + 1) * 128)
        es_ = slice(hh * NHH * DH, (hh + 1) * NHH * DH)
        ls.dmas(nc.sync, [dict(out=a[:], in_=HD[0][ts_, es_].rearrange("p (n v) -> p n v", n=NHH)),
                          dict(out=b[:], in_=HD[1][ts_, es_].rearrange("p (n v) -> p n v", n=NHH))])
        yield
        ls.group(nc.vector, lambda: nc.vector.tensor_tensor(out=a[:], in0=a[:], in1=b[:], op=ALU.add))
        yield
        ls.group(nc.vector, lambda: nc.vector.reduce_sum(out=st[:, 0, :], in_=a[:], axis=AX.X))
        yield
        ls.group(nc.vector, lambda: nc.vector.tensor_scalar(out=st[:, 1, :], in0=st[:, 0, :], scalar1=-1.0 / DH, scalar2=None, op0=ALU.mult))
        yield

        def f1():
            last = None
            for n in range(NHH):
                last = nc.scalar.activation(out=b[:, n, :], in_=a[:, n, :], func=AF.Identity, bias=st[:, 1, n:n + 1], scale=1.0)
            return last

        ls.group(nc.scalar, f1)
        yield
        ls.group(nc.vector, lambda: nc.vector.tensor_tensor(out=c[:], in0=b[:], in1=b[:], op=ALU.mult))
        yield
        ls.group(nc.vector, lambda: nc.vector.reduce_sum(out=st[:, 2, :], in_=c[:], axis=AX.X))
        yield
        ls.group(nc.scalar, lambda: nc.scalar.activation(out=st[:, 3, :], in_=st[:, 2, :], func=AF.Sqrt, bias=eps_t[:], scale=1.0 / DH))
        yield
        ls.group(nc.vector, lambda: nc.vector.reciprocal(out=st[:, 3, :], in_=st[:, 3, :]))
        yield

        def f3():
            last = None
            for n in range(NHH):
                last = nc.vector.tensor_scalar(out=a[:, n, :], in0=b[:, n, :], scalar1=st[:, 3, n:n + 1], scalar2=None, op0=ALU.mult)
            return last

        ls.group(nc.vector, f3)
        yield
        for b0 in range(0, ECH, 16):
            ecs = list(range(b0, min(ECH, b0 + 16)))

            def pe(ecs=ecs):
                last = None
                for bi, ec in enumerate(ecs):
                    n, vc = divmod(ec, DCH)
                    last = nc.tensor.matmul(ps[bi // 4][:, (bi % 4) * 128:(bi % 4 + 1) * 128], a[:, n, vc * 128:(vc + 1) * 128],
                                            ident[:], start=True, stop=True)
                return last

            ls.group(nc.tensor, pe)
            yield

            def ev(ecs=ecs):
                last = None
                for bi, ec in enumerate(ecs):
                    last = nc.scalar.copy(out=hT[:, ec, :], in_=ps[bi // 4][:, (bi % 4) * 128:(bi % 4 + 1) * 128])
                return last

            ls.group(nc.scalar, ev)
            yield
        ls.dmas(nc.sync, [dict(out=HN[hh * ECH * 128:(hh + 1) * ECH * 128, ts_].rearrange("(c p) t -> p c t", p=128), in_=hT[:])])
        yield

    lanes(nc, seq, mk6, [(tc, hh) for tc in range(NTC) for hh in range(2)], body6)

    with ExitStack() as es0:
        hw = es0.enter_context(nc.sbuf_tensor(uid("g5"), [128, EC], F32))
        sk = es0.enter_context(nc.sbuf_tensor(uid("g6"), [128, EC], F32))
        seq.dmas(nc.sync, [dict(out=hw[:], in_=I["a_hnorm_w"][j].rearrange("(c p) -> p c", p=128)),
                           dict(out=sk[:], in_=I["a_skip"][j].rearrange("(c p) -> p c", p=128))])

        def mk7(es, l):
            return dict(ht=es.enter_context(nc.sbuf_tensor(uid("g1"), [128, N], F32)),
                        xt=es.enter_context(nc.sbuf_tensor(uid("g2"), [128, N], F32)),
                        zt=es.enter_context(nc.sbuf_tensor(uid("g3"), [128, N], F32)),
                        go=es.enter_context(nc.sbuf_tensor(uid("g4"), [128, N], BF16)))

        def body7(ls, Bf, ec):
            ht, xt, zt, go = Bf["ht"], Bf["xt"], Bf["zt"], Bf["go"]
            rs_ = slice(ec * 128, (ec + 1) * 128)
            ls.dmas(nc.sync, [dict(out=ht[:], in_=HN[rs_, :]), dict(out=xt[:], in_=XC[rs_, :]), dict(out=zt[:], in_=Z[rs_, :])])
            yield

            def f1():
                nc.scalar.activation(out=ht[:], in_=ht[:], func=AF.Identity, scale=hw[:, ec:ec + 1])
                return nc.scalar.activation(out=zt[:], in_=zt[:], func=AF.Silu)

            ls.group(nc.scalar, f1)
            yield
            ls.group(nc.vector, lambda: nc.vector.scalar_tensor_tensor(out=xt[:], in0=xt[:], scalar=sk[:, ec:ec + 1], in1=ht[:],
                                                                       op0=ALU.mult, op1=ALU.add))
            yield
            ls.group(nc.vector, lambda: nc.vector.tensor_tensor(out=go[:], in0=xt[:], in1=zt[:], op=ALU.mult))
            yield
            ls.dmas(nc.sync, [dict(out=G[rs_, :], in_=go[:])])
            yield

        lanes(nc, seq, mk7, list(range(EC)), body7)


_NP2BIR = {np.dtype(np.float32): F32, np.dtype(ml_dtypes.bfloat16): BF16}


def run(cfg, inputs, debug=(), trace=False):
    consts = host_consts(cfg)
    B = inputs["x"].shape[0]
    weights = {k: np.ascontiguousarray(inputs[k], dtype=np.float32) for k in WEIGHT_NAMES}
    shapes = {k: (v.shape, F32) for k, v in weights.items()}
    shapes["xT"] = ((cfg.D, cfg.N), F32)
    shapes["ccT"] = ((cfg.D, 2), F32)
    const_shapes = {k: (v.shape, _NP2BIR[v.dtype]) for k, v in consts.items()}
    nc = build(cfg, shapes, const_shapes, debug=debug)
    in_maps = []
    for b in range(B):
        m = dict(weights)
        m.update(consts)
        m["xT"] = np.ascontiguousarray(np.concatenate([inputs["x"][b].T, inputs["ctx"][b].T], axis=1), dtype=np.float32)
        m["ccT"] = np.ascontiguousarray(np.stack([inputs["c"][b], inputs["c_ctx"]], axis=1), dtype=np.float32)
        in_maps.append(m)
    res = run_bass_kernel_spmd(nc, in_maps, core_ids=list(range(B)), trace=trace)
    out = np.stack([np.ascontiguousarray(res.results[b]["yT"].T) for b in range(B)], axis=0).astype(np.float32)
    return out, res


def kernel(**inputs):
    cfg = Cfg()
    out, _ = run(cfg, inputs)
    return out
```

```python
import math
from contextlib import ExitStack

import numpy as np
import ml_dtypes
import concourse.bass as bass
import concourse.mybir as mybir
from concourse.bass_utils import run_bass_kernel_spmd

F32 = mybir.dt.float32
BF16 = mybir.dt.bfloat16
AF = mybir.ActivationFunctionType
ALU = mybir.AluOpType
AX = mybir.AxisListType
NEG = -30000.0
EPS = 1e-6
POOL_WINDOWS = (2, 4, 8, 16)

_uid = [0]


def uid(p):
    _uid[0] += 1
    return f"{p}_{_uid[0]}"


class Seq:
    def __init__(self, nc, limit=20000, tag="seq"):
        self.nc = nc
        self.n = 0
        self.limit = limit
        self.tag = tag
        self._new_sem()
        self.prev = None

    def _new_sem(self):
        self.sem = self.nc.alloc_semaphore(f"{self.tag}{self.n}")
        self.n += 1
        self.val = 0

    def _pre(self, eng):
        if self.prev is not None:
            eng.wait_ge(self.prev[0], self.prev[1])
        if self.val > self.limit:
            self._new_sem()

    def dmas(self, eng, items):
        if eng is self.nc.gpsimd and len(items) > 1:
            for kw in items:
                self.dmas(eng, [kw])
            return
        self._pre(eng)
        for kw in items:
            eng.dma_start(**kw).then_inc(self.sem, 16)
            self.val += 16
        self.prev = (self.sem, self.val)

    def group(self, eng, fn):
        self._pre(eng)
        last = fn()
        last.then_inc(self.sem, 1)
        self.val += 1
        self.prev = (self.sem, self.val)


def load_eng(nc, src_dtype, dst_dtype):
    return nc.sync if src_dtype == dst_dtype else nc.gpsimd


def mm(nc, seq, out_hbm, pairs, *, scale=None, func=None, accumulate=False, nt=512, mb=None, fp32=False):
    M, N = out_hbm.shape
    func = func or AF.Identity
    cdt = F32 if fp32 else BF16
    esz = 4 if fp32 else 2
    Ks = [l.shape[0] for l, r in pairs]
    kcs = [max(1, K // 128) for K in Ks]
    kps = [min(K, 128) for K in Ks]
    tot_kc = sum(kcs)
    if mb is None:
        mb = 1024
        while tot_kc * mb * esz > 64 * 1024 and mb > 128:
            mb //= 2
    mb = min(mb, M)
    nt = min(nt, N)
    while tot_kc * nt * esz > 48 * 1024 and nt > 128:
        nt //= 2
    mcb = max(1, (mb + 127) // 128)
    assert mcb <= 8
    with ExitStack() as es:
        wsb = es.enter_context(nc.sbuf_tensor(uid("mm_w"), [128, tot_kc, mb], cdt))
        xsb = es.enter_context(nc.sbuf_tensor(uid("mm_x"), [128, tot_kc, nt], cdt))
        osb = es.enter_context(nc.sbuf_tensor(uid("mm_o"), [128, mcb, nt], out_hbm.dtype))
        ps = [es.enter_context(nc.psum_tensor(uid("mm_ps"), [128, 512], F32)) for _ in range(mcb)]
        for m0 in range(0, M, mb):
            mbs = min(mb, M - m0)
            items = []
            off = 0
            for (l, r), K, kc, kp in zip(pairs, Ks, kcs, kps):
                src = l[:, m0:m0 + mbs]
                if kc > 1:
                    src = src.rearrange("(kc p) m -> p kc m", p=128)
                    dst = wsb[:, off:off + kc, :mbs]
                else:
                    dst = wsb[:kp, off, :mbs]
                items.append(dict(out=dst, in_=src))
                off += kc
            seq.dmas(load_eng(nc, pairs[0][0].dtype, cdt), items)
            for n0 in range(0, N, nt):
                ns = min(nt, N - n0)
                items = []
                off = 0
                for (l, r), K, kc, kp in zip(pairs, Ks, kcs, kps):
                    src = r[:, n0:n0 + ns]
                    if kc > 1:
                        src = src.rearrange("(kc p) n -> p kc n", p=128)
                        dst = xsb[:, off:off + kc, :ns]
                    else:
                        dst = xsb[:kp, off, :ns]
                    items.append(dict(out=dst, in_=src))
                    off += kc
                seq.dmas(load_eng(nc, pairs[0][1].dtype, cdt), items)
                nmc = (mbs + 127) // 128
                steps = []
                off = 0
                for kc, kp in zip(kcs, kps):
                    for k in range(kc):
                        steps.append((off + k, kp))
                    off += kc

                def pe():
                    last = None
                    for mi in range(nmc):
                        ms = min(128, mbs - mi * 128)
                        for si, (kk, kp) in enumerate(steps):
                            last = nc.tensor.matmul(ps[mi][:ms, :ns], wsb[:kp, kk, mi * 128:mi * 128 + ms],
                                                    xsb[:kp, kk, :ns], start=(si == 0), stop=(si == len(steps) - 1))
                    return last

                seq.group(nc.tensor, pe)

                def ev():
                    last = None
                    for mi in range(nmc):
                        ms = min(128, mbs - mi * 128)
                        gm = (m0 // 128) + mi
                        sc = 1.0 if scale is None else scale(gm, n0)
                        if isinstance(sc, float):
                            last = nc.scalar.activation(out=osb[:ms, mi, :ns], in_=ps[mi][:ms, :ns], func=func, scale=sc)
                        else:
                            last = nc.scalar.activation(out=osb[:ms, mi, :ns], in_=ps[mi][:ms, :ns], func=func,
                                                        scale=sc[:ms])
                    return last

                seq.group(nc.scalar, ev)
                dst = out_hbm[m0:m0 + mbs, n0:n0 + ns]
                if nmc > 1:
                    dst = dst.rearrange("(mc p) n -> p mc n", p=128)
                    srcs = osb[:, :nmc, :ns]
                else:
                    srcs = osb[:mbs, 0, :ns]
                if accumulate:
                    seq.dmas(nc.gpsimd, [dict(out=dst, in_=srcs, accum_op=ALU.add)])
                else:
                    seq.dmas(nc.sync, [dict(out=dst, in_=srcs)])


class Pipe:
    def __init__(self, nc):
        self.sW = nc.alloc_semaphore("pW")
        self.vW = 0
        self.sW2 = nc.alloc_semaphore("pW2")
        self.vW2 = 0
        self.sL = [nc.alloc_semaphore(f"pL{i}") for i in range(2)]
        self.vL = [0, 0]
        self.sPE = nc.alloc_semaphore("pPE")
        self.vPE = 0
        self.sEV = nc.alloc_semaphore("pEV")
        self.vEV = 0
        self.sST = [nc.alloc_semaphore(f"pST{i}") for i in range(2)]
        self.vST = [0, 0]


def mm2(nc, seq, pipe, out_hbm, pairs, *, scale=None, func=None, accumulate=False, nt=512):
    M, N = out_hbm.shape
    func = func or AF.Identity
    cdt = BF16
    Ks = [l.shape[0] for l, r in pairs]
    kcs = [max(1, K // 128) for K in Ks]
    kps = [min(K, 128) for K in Ks]
    tot_kc = sum(kcs)
    mb = 1024
    while tot_kc * mb * 2 > 64 * 1024 and mb > 128:
        mb //= 2
    mb = min(mb, M)
    nt = min(nt, N)
    while tot_kc * nt * 2 > 32 * 1024 and nt > 128:
        nt //= 2
    mcb = max(1, (mb + 127) // 128)
    hb = 4 if mcb > 4 else mcb
    eW = load_eng(nc, pairs[0][0].dtype, cdt)
    eL = load_eng(nc, pairs[0][1].dtype, cdt)
    eS = nc.gpsimd if (accumulate or (eL is nc.sync and eW is nc.sync)) else nc.sync
    if accumulate:
        assert eS is nc.gpsimd
    start_tok = seq.prev
    steps_kk = []
    off = 0
    for kc, kp in zip(kcs, kps):
        for k in range(kc):
            steps_kk.append((off + k, kp))
        off += kc
    with ExitStack() as es:
        wsb = es.enter_context(nc.sbuf_tensor(uid("m2w"), [128, tot_kc, mb], cdt))
        xsb = [es.enter_context(nc.sbuf_tensor(uid("m2x"), [128, tot_kc, nt], cdt)) for _ in range(2)]
        osb = [es.enter_context(nc.sbuf_tensor(uid("m2o"), [128, mcb, nt], out_hbm.dtype)) for _ in range(2)]
        psets = [[es.enter_context(nc.psum_tensor(uid("m2p"), [128, 512], F32)) for _ in range(hb)] for _ in range(2)]
        blocks = [(m0, min(mb, M - m0)) for m0 in range(0, M, mb)]
        ntiles = [(n0, min(nt, N - n0)) for n0 in range(0, N, nt)]
        steps = [(bi, ni) for bi in range(len(blocks)) for ni in range(len(ntiles))]
        pe_after_step = {}
        st_after_step = {}
        set_free = [pipe.vEV, pipe.vEV]
        w_ready = None

        def chained_dmas(eng, items, sem, val):
            for kw in items:
                eng.dma_start(**kw).then_inc(sem, 16)
                val += 16
                if eng is nc.gpsimd and len(items) > 1:
                    eng.wait_ge(sem, val)
            return val

        def emit_L(si):
            bi, ni = steps[si]
            n0, ns = ntiles[ni]
            p = si % 2
            if si >= 2:
                eL.wait_ge(pipe.sPE, pe_after_step[si - 2])
            elif start_tok is not None:
                eL.wait_ge(start_tok[0], start_tok[1])
            items = []
            off = 0
            for (l, r), kc, kp in zip(pairs, kcs, kps):
                src = r[:, n0:n0 + ns]
                if kc > 1:
                    src = src.rearrange("(kc p) n -> p kc n", p=128)
                    dst = xsb[p][:, off:off + kc, :ns]
                else:
                    dst = xsb[p][:kp, off, :ns]
                items.append(dict(out=dst, in_=src))
                off += kc
            pipe.vL[p] = chained_dmas(eL, items, pipe.sL[p], pipe.vL[p])
            return pipe.vL[p]

        l_val = {}
        for si in range(min(2, len(steps))):
            if si == 0 or steps[si][0] == 0 or True:
                l_val[si] = None
        def emit_W(bi, half, wait_tok):
            m0, mbs = blocks[bi]
            if half is None:
                c_lo, c_hi = 0, mbs
            elif half == 0:
                c_lo, c_hi = 0, min(512, mbs)
            else:
                c_lo, c_hi = 512, mbs
            if wait_tok is not None:
                eW.wait_ge(wait_tok[0], wait_tok[1])
            items = []
            off = 0
            for (l, r), kc, kp in zip(pairs, kcs, kps):
                src = l[:, m0 + c_lo:m0 + c_hi]
                if kc > 1:
                    src = src.rearrange("(kc p) m -> p kc m", p=128)
                    dst = wsb[:, off:off + kc, c_lo:c_hi]
                else:
                    dst = wsb[:kp, off, c_lo:c_hi]
                items.append(dict(out=dst, in_=src))
                off += kc
            if half == 1:
                pipe.vW2 = chained_dmas(eW, items, pipe.sW2, pipe.vW2)
            else:
                pipe.vW = chained_dmas(eW, items, pipe.sW, pipe.vW)

        def split_block(bi):
            return blocks[bi][1] > 512

        w_ready = [None, None]
        nxt_ready = [None, None]
        last_step_of_block = {}
        for si_, (bi_, ni_) in enumerate(steps):
            last_step_of_block[bi_] = si_
        cur_block = -1
        for si, (bi, ni) in enumerate(steps):
            m0, mbs = blocks[bi]
            n0, ns = ntiles[ni]
            p = si % 2
            nmc = (mbs + 127) // 128
            if bi != cur_block:
                cur_block = bi
                if si == 0:
                    if split_block(bi):
                        emit_W(bi, 0, start_tok)
                        w_ready[0] = (pipe.sW, pipe.vW)
                        emit_W(bi, 1, None)
                        w_ready[1] = (pipe.sW2, pipe.vW2)
                    else:
                        emit_W(bi, None, start_tok)
                        w_ready[0] = w_ready[1] = (pipe.sW, pipe.vW)
                elif not _CTX.get("w_prefetched", False):
                    emit_W(bi, None, (pipe.sPE, pe_after_step[si - 1]))
                    w_ready[0] = w_ready[1] = (pipe.sW, pipe.vW)
                else:
                    w_ready[0], w_ready[1] = nxt_ready[0], nxt_ready[1]
                _CTX["w_prefetched"] = False
            if si == 0:
                l_val[0] = emit_L(0)
                if len(steps) > 1:
                    l_val[1] = emit_L(1)
            halves = [(0, 0, min(4, nmc)), (1, 4, nmc)] if nmc > 4 else [(si % 2, 0, nmc)]
            ev_last = None
            for (st_i, c0, c1) in halves:
                nc.tensor.wait_ge(pipe.sL[p], l_val[si])
                wr = w_ready[0 if c0 == 0 else 1]
                nc.tensor.wait_ge(wr[0], wr[1])
                if len(halves) == 1 and w_ready[1] is not w_ready[0]:
                    nc.tensor.wait_ge(w_ready[1][0], w_ready[1][1])
                nc.tensor.wait_ge(pipe.sEV, set_free[st_i])
                last = None
                for mi in range(c0, c1):
                    ms = min(128, mbs - mi * 128)
                    for k_i, (kk, kp) in enumerate(steps_kk):
                        last = nc.tensor.matmul(psets[st_i][mi - c0][:ms, :ns], wsb[:kp, kk, mi * 128:mi * 128 + ms],
                                                xsb[p][:kp, kk, :ns], start=(k_i == 0), stop=(k_i == len(steps_kk) - 1))
                last.then_inc(pipe.sPE, 1)
                pipe.vPE += 1
                pe_val = pipe.vPE
                if (si == last_step_of_block[bi] and bi + 1 < len(blocks) and len(halves) == 2 and split_block(bi + 1)):
                    hsel = 0 if c0 == 0 else 1
                    emit_W(bi + 1, hsel, (pipe.sPE, pe_val))
                    nxt_ready[hsel] = (pipe.sW, pipe.vW) if hsel == 0 else (pipe.sW2, pipe.vW2)
                    if hsel == 1:
                        _CTX["w_prefetched"] = True
                nc.scalar.wait_ge(pipe.sPE, pe_val)
                if si >= 2 and c0 == 0:
                    pp, vv = st_after_step[si - 2]
                    nc.scalar.wait_ge(pipe.sST[pp], vv)
                last = None
                for mi in range(c0, c1):
                    ms = min(128, mbs - mi * 128)
                    gm = (m0 // 128) + mi
                    sc = 1.0 if scale is None else scale(gm, n0)
                    if isinstance(sc, float):
                        last = nc.scalar.activation(out=osb[p][:ms, mi, :ns], in_=psets[st_i][mi - c0][:ms, :ns], func=func, scale=sc)
                    else:
                        last = nc.scalar.activation(out=osb[p][:ms, mi, :ns], in_=psets[st_i][mi - c0][:ms, :ns], func=func,
                                                    scale=sc[:ms])
                last.then_inc(pipe.sEV, 1)
                pipe.vEV += 1
                set_free[st_i] = pipe.vEV
                ev_last = pipe.vEV
            pe_after_step[si] = pipe.vPE
            if si + 2 < len(steps):
                l_val[si + 2] = emit_L(si + 2)
            eS.wait_ge(pipe.sEV, ev_last)
            dst = out_hbm[m0:m0 + mbs, n0:n0 + ns]
            if nmc > 1:
                dst = dst.rearrange("(mc p) n -> p mc n", p=128)
                srcs = osb[p][:, :nmc, :ns]
            else:
                srcs = osb[p][:mbs, 0, :ns]
            if accumulate:
                eS.dma_start(out=dst, in_=srcs, accum_op=ALU.add).then_inc(pipe.sST[p], 16)
            else:
                eS.dma_start(out=dst, in_=srcs).then_inc(pipe.sST[p], 16)
            pipe.vST[p] += 16
            st_after_step[si] = (p, pipe.vST[p])
        nc.vector.wait_ge(pipe.sST[0], pipe.vST[0])
        nc.vector.wait_ge(pipe.sST[1], pipe.vST[1])
        jt = es.enter_context(nc.sbuf_tensor(uid("m2j"), [128, 1], F32))
        seq.prev = None
        seq.group(nc.vector, lambda: nc.vector.memset(jt[:], 0.0))


_CTX = {}


def mmp(nc, seq, out_hbm, pairs, **kw):
    return mm2(nc, seq, _CTX["pipe"], out_hbm, pairs, **kw)


def lanes(nc, seq, make_bufs, items, body, nl=2):
    T0 = seq.prev
    lseqs = _CTX["lane_seqs"][:nl]
    with ExitStack() as es:
        bufs = [make_bufs(es, l) for l in range(nl)]
        T1 = seq.prev
        for ls in lseqs:
            ls.prev = T1
        pending = list(items)
        gens = [None] * nl
        while True:
            progressed = False
            for l in range(nl):
                if gens[l] is None and pending:
                    gens[l] = body(lseqs[l], bufs[l], pending.pop(0))
                if gens[l] is not None:
                    progressed = True
                    try:
                        next(gens[l])
                    except StopIteration:
                        gens[l] = None
            if not progressed:
                break
        for ls in lseqs:
            if ls.prev is not None:
                nc.vector.wait_ge(ls.prev[0], ls.prev[1])
        jt = es.enter_context(nc.sbuf_tensor(uid("lj"), [128, 1], F32))
        seq.prev = None
        seq.group(nc.vector, lambda: nc.vector.memset(jt[:], 0.0))


class Cfg:
    def __init__(self, D=4096, T=4096, TC=256, depth=4):
        self.D, self.T, self.TC, self.depth = D, T, TC, depth
        self.N = T + TC
        self.E = 2 * D
        self.NH = 8
        self.DH = self.E // 8
        self.GW = self.E // 4
        self.DC = D // 128
        self.EC = self.E // 128
        self.n_a = len(range(0, depth, 3))
        self.n_b = len(range(1, depth, 3))
        self.n_c = len(range(2, depth, 3))


def host_consts(cfg):
    T, TC, D, GW, N = cfg.T, cfg.TC, cfg.D, cfg.GW, cfg.N
    bf = ml_dtypes.bfloat16
    c = {}

    def dft(n):
        k = np.arange(n, dtype=np.float64)
        ang = 2.0 * np.pi * ((k[:, None] * k[None, :]) % n) / n
        return np.cos(ang) / math.sqrt(n), np.sin(ang) / math.sqrt(n)

    cc, sc = dft(GW)
    c["k_cc"], c["k_sc"] = cc.astype(bf), sc.astype(bf)
    ct, st = dft(T)
    c["k_ct"], c["k_nst"] = ct.astype(bf), (-st).astype(bf)
    ctc, stc = dft(TC)
    c["k_ctc"], c["k_nstc"] = ctc.astype(bf), (-stc).astype(bf)
    inv = np.zeros((4, N), np.float32)
    for g, w in enumerate(POOL_WINDOWS):
        lo = w // 2
        hi = w - 1 - lo
        for (o, n) in ((0, T), (T, TC)):
            pos = np.arange(n)
            a = np.clip(pos - lo, 0, n)
            b = np.clip(pos + hi + 1, 0, n)
            inv[g, o:o + n] = 1.0 / (b - a)
    c["k_invc"] = np.ascontiguousarray(np.broadcast_to(inv[:, None, :], (4, 128, N))).astype(np.float32)
    s = np.arange(128)[:, None]
    t = np.arange(256)[None, :]
    m = np.zeros((2, 2, 128, 256), np.float32)
    for half in range(2):
        m[0, half] = np.where(s + 128 * half <= t, 0.0, NEG)
        m[1, half] = np.where(s + 128 * half >= t, 0.0, NEG)
    c["k_mask"] = np.ascontiguousarray(m.reshape(4, 128, 256).transpose(1, 0, 2))
    c["k_ident"] = np.eye(128, dtype=np.float32)
    sel = np.zeros((8, 8, 128), np.float32)
    for n in range(8):
        sel[n, n, :] = 1.0
    c["k_sel"] = sel
    rows = T // 64
    r = np.repeat(np.arange(rows, dtype=np.float32), 64)
    col = np.tile(np.arange(64, dtype=np.float32), rows)
    quarter = D // 4
    omega = (1.0 / (np.float32(10000.0) ** (np.arange(quarter, dtype=np.float32) / np.float32(quarter)))).astype(np.float32)

    def axis_emb(p):
        a = (p[:, None] * omega[None, :]).astype(np.float32)
        return np.concatenate([np.sin(a), np.cos(a)], axis=-1)

    pe = np.concatenate([axis_emb(r), axis_emb(col)], axis=-1).astype(np.float32)
    c["k_posT"] = np.ascontiguousarray(pe.T)
    return c


WEIGHT_NAMES = ["ada_w", "ada_b", "norm_g", "final_g",
                "a_w_in", "a_conv_w", "a_conv_b", "a_wq", "a_wk", "a_wv", "a_w_ig", "a_b_ig", "a_w_fg", "a_b_fg",
                "a_hnorm_w", "a_skip", "a_w_out", "b_w_in", "b_w_grp", "b_scale", "b_w_out",
                "c_w_in", "c_w_grp", "c_w_out"]


def build(cfg, shapes, const_shapes, debug=()):
    D, T, TC, N, E, NH, DH, GW, DC, EC = cfg.D, cfg.T, cfg.TC, cfg.N, cfg.E, cfg.NH, cfg.DH, cfg.GW, cfg.DC, cfg.EC
    nc = bass.Bass("TRN2", target_bir_lowering=False)
    seq = Seq(nc)
    _CTX["pipe"] = Pipe(nc)
    _CTX["att"] = None
    _CTX["lane_seqs"] = [Seq(nc, limit=120000, tag=f"lane{l}_") for l in range(3)]
    I = {}
    for name, (shp, dt) in shapes.items():
        I[name] = nc.dram_tensor(name, list(shp), dt, kind="ExternalInput").ap()
    for name, (shp, dt) in const_shapes.items():
        I[name] = nc.dram_tensor(name, list(shp), dt, kind="ExternalInput").ap()
    yT = nc.dram_tensor("yT", [D, T], F32, kind="ExternalOutput").ap()

    def scratch(name, shape, dt):
        kind = "ExternalOutput" if name in debug else "Internal"
        return nc.dram_tensor(name, list(shape), dt, kind=kind)

    X_h = scratch("X", [D, N], F32)
    X = X_h.ap()
    H = scratch("H", [D, N], BF16).ap()
    U = scratch("U", [E, N], F32).ap()
    Z = scratch("Z", [E, N], F32).ap()
    G = scratch("G", [E, N], BF16).ap()
    P = scratch("P", [E, N], F32).ap()
    XC = scratch("XC", [E, N], F32).ap()
    DL = scratch("DL", [E, N], BF16).ap()
    QT = scratch("QT", [E, N], BF16).ap()
    KT = scratch("KT", [E, N], BF16).ap()
    VT = scratch("VT", [E, N], BF16).ap()
    VTOK = scratch("VTOK", [N, E], BF16).ap()
    FA = scratch("FA", [N, E], BF16).ap()
    FB = scratch("FB", [N, E], BF16).ap()
    HD = [scratch(f"HD{z}", [N, E], F32).ap() for z in range(2)]
    BD_h = scratch("BD", [3 * EC * 128, 128], F32)
    BD = BD_h.ap()
    WG = scratch("WG", [3 * E, 32], F32).ap()
    GP = scratch("GP", [32, N], F32).ap()
    ST = scratch("ST", [D, 2], F32).ap()
    BZ = scratch("BZ", [2, 8, N], F32).ap()
    LMB = scratch("LMB", [2, 8, N], F32).ap()
    MODT = scratch("MODT", [3 * D, 2], F32).ap()

    with ExitStack() as top:
        top.enter_context(nc.allow_non_contiguous_dma(reason="small per-channel parameter vectors"))

        def sb(name, shape, dt=F32):
            return top.enter_context(nc.sbuf_tensor(uid(name), shape, dt))

        ident = sb("ident", [128, 128])
        ones_f = sb("ones_f", [128, 128])
        ones_b = sb("ones_b", [128, 8], BF16)
        eps_t = sb("eps_t", [128, 1])
        one_t = sb("one_t", [128, 1])
        selT = sb("selT", [8, 8, 128])
        normA = [sb(f"normA{i}", [128, DC, 2]) for i in range(cfg.depth)]
        normB = [sb(f"normB{i}", [128, DC, 2]) for i in range(cfg.depth)]
        gateS = [sb(f"gateS{i}", [128, DC, 2]) for i in range(cfg.depth)]
        finA = sb("finA", [128, DC])
        zeroB = sb("zeroB", [128, DC])

        def g0():
            nc.vector.memset(ones_f[:], 1.0)
            nc.vector.memset(ones_b[:], 1.0)
            nc.vector.memset(eps_t[:], EPS)
            nc.vector.memset(zeroB[:], 0.0)
            return nc.vector.memset(one_t[:], 1.0)

        seq.group(nc.vector, g0)
        seq.dmas(nc.sync, [dict(out=ident[:], in_=I["k_ident"]), dict(out=selT[:], in_=I["k_sel"]),
                           dict(out=finA[:], in_=I["final_g"].rearrange("(c p) -> p c", p=128))])

        with ExitStack() as es:
            xt = es.enter_context(nc.sbuf_tensor(uid("xi"), [128, N], F32))
            pt = es.enter_context(nc.sbuf_tensor(uid("pi"), [128, T], F32))
            for dc in range(DC):
                rs = slice(dc * 128, (dc + 1) * 128)
                seq.dmas(nc.sync, [dict(out=xt[:], in_=I["xT"][rs, :]), dict(out=pt[:], in_=I["k_posT"][rs, :])])
                seq.group(nc.vector, lambda: nc.vector.tensor_tensor(out=xt[:, :T], in0=xt[:, :T], in1=pt[:], op=ALU.add))
                seq.dmas(nc.sync, [dict(out=X[rs, :], in_=xt[:])])

        with ExitStack() as es:
            cs = es.enter_context(nc.sbuf_tensor(uid("cs"), [128, DC, 2], F32))
            md = es.enter_context(nc.sbuf_tensor(uid("md"), [128, 3 * DC, 2], F32))
            ab = es.enter_context(nc.sbuf_tensor(uid("ab"), [128, 3 * DC], F32))
            gg = es.enter_context(nc.sbuf_tensor(uid("gg"), [128, DC], F32))
            t1 = es.enter_context(nc.sbuf_tensor(uid("t1"), [128, DC, 2], F32))
            seq.dmas(nc.sync, [dict(out=cs[:], in_=I["ccT"].rearrange("(c p) r -> p c r", p=128))])
            seq.group(nc.scalar, lambda: nc.scalar.activation(out=cs[:], in_=cs[:], func=AF.Silu))
            seq.dmas(nc.sync, [dict(out=ST.rearrange("(c p) r -> p c r", p=128), in_=cs[:])])
            for i in range(cfg.depth):
                mmp(nc, seq, MODT, [(I["ada_w"][i], ST)])
                seq.dmas(nc.sync, [dict(out=md[:], in_=MODT.rearrange("(c p) r -> p c r", p=128)),
                                   dict(out=ab[:], in_=I["ada_b"][i].rearrange("(c p) -> p c", p=128)),
                                   dict(out=gg[:], in_=I["norm_g"][i].rearrange("(c p) -> p c", p=128))])

                def f1():
                    last = None
                    for r in range(2):
                        last = nc.vector.tensor_tensor(out=md[:, :, r], in0=md[:, :, r], in1=ab[:], op=ALU.add)
                    return last

                seq.group(nc.vector, f1)

                def f2():
                    nc.vector.tensor_copy(out=normB[i][:], in_=md[:, 0:DC, :])
                    nc.vector.tensor_copy(out=gateS[i][:], in_=md[:, 2 * DC:3 * DC, :])
                    return nc.vector.tensor_scalar(out=t1[:], in0=md[:, DC:2 * DC, :], scalar1=1.0, scalar2=None, op0=ALU.add)

                seq.group(nc.vector, f2)

                def f3():
                    last = None
                    for r in range(2):
                        last = nc.vector.tensor_tensor(out=normA[i][:, :, r], in0=t1[:, :, r], in1=gg[:], op=ALU.mult)
                    return last

                seq.group(nc.vector, f3)

        def norm_stage(A_of, B_of, out_hbm, out_dt, ncols):
            NT = 128

            def mk(es, l):
                return dict(xt=es.enter_context(nc.sbuf_tensor(uid("nx"), [128, DC, NT], F32)),
                            sq=es.enter_context(nc.sbuf_tensor(uid("nsq"), [128, DC, NT], F32)),
                            ho=es.enter_context(nc.sbuf_tensor(uid("nh"), [128, DC, NT], out_dt)),
                            rs=es.enter_context(nc.sbuf_tensor(uid("nrs"), [128, NT], F32)),
                            pss=es.enter_context(nc.psum_tensor(uid("nps"), [128, 512], F32)))

            def body(ls, Bf, n0):
                xt, sq, ho, rs, pss = Bf["xt"], Bf["sq"], Bf["ho"], Bf["rs"], Bf["pss"]
                ns = min(NT, ncols - n0)
                r = 0 if n0 < T else 1
                ls.dmas(nc.sync, [dict(out=xt[:, :, :ns], in_=X[:, n0:n0 + ns].rearrange("(c p) n -> p c n", p=128))])
                yield
                ls.group(nc.scalar, lambda: nc.scalar.activation(out=sq[:, :, :ns], in_=xt[:, :, :ns], func=AF.Square))
                yield

                def pe():
                    last = None
                    for c in range(DC):
                        last = nc.tensor.matmul(pss[:, :ns], ones_f[:], sq[:, c, :ns], start=(c == 0), stop=(c == DC - 1))
                    return last

                ls.group(nc.tensor, pe)
                yield
                ls.group(nc.scalar, lambda: nc.scalar.activation(out=rs[:, :ns], in_=pss[:, :ns], func=AF.Sqrt,
                                                                 bias=eps_t[:], scale=1.0 / D))
                yield
                ls.group(nc.vector, lambda: nc.vector.reciprocal(out=rs[:, :ns], in_=rs[:, :ns]))
                yield

                def f1():
                    last = None
                    for c in range(DC):
                        last = nc.vector.tensor_tensor(out=sq[:, c, :ns], in0=xt[:, c, :ns], in1=rs[:, :ns], op=ALU.mult)
                    return last

                ls.group(nc.vector, f1)
                yield

                def f2():
                    last = None
                    for c in range(DC):
                        last = nc.scalar.activation(out=ho[:, c, :ns], in_=sq[:, c, :ns], func=AF.Identity,
                                                    scale=A_of(r)[:, c:c + 1], bias=B_of(r)[:, c:c + 1])
                    return last

                ls.group(nc.scalar, f2)
                yield
                ls.dmas(nc.sync, [dict(out=out_hbm[:, n0:n0 + ns].rearrange("(c p) n -> p c n", p=128), in_=ho[:, :, :ns])])
                yield

            lanes(nc, seq, mk, list(range(0, ncols, NT)), body, nl=3)

        def gate_stage(scale_hbm):
            with ExitStack() as es0:
                scs = es0.enter_context(nc.sbuf_tensor(uid("gs"), [128, EC], F32))
                if scale_hbm is not None:
                    seq.dmas(nc.sync, [dict(out=scs[:], in_=scale_hbm.rearrange("(c p) -> p c", p=128))])
                else:
                    seq.group(nc.vector, lambda: nc.vector.memset(scs[:], 1.0))

                def mk(es, l):
                    return dict(pt=es.enter_context(nc.sbuf_tensor(uid("gp"), [128, N], F32)),
                                zt=es.enter_context(nc.sbuf_tensor(uid("gz"), [128, N], F32)),
                                go=es.enter_context(nc.sbuf_tensor(uid("go"), [128, N], BF16)))

                def body(ls, Bf, ec):
                    pt_, zt, go = Bf["pt"], Bf["zt"], Bf["go"]
                    rs_ = slice(ec * 128, (ec + 1) * 128)
                    ls.dmas(nc.sync, [dict(out=pt_[:], in_=P[rs_, :]), dict(out=zt[:], in_=Z[rs_, :])])
                    yield
                    ls.group(nc.scalar, lambda: nc.scalar.activation(out=zt[:], in_=zt[:], func=AF.Silu))
                    yield
                    ls.group(nc.vector, lambda: nc.vector.scalar_tensor_tensor(out=go[:], in0=pt_[:], scalar=scs[:, ec:ec + 1],
                                                                               in1=zt[:], op0=ALU.mult, op1=ALU.mult))
                    yield
                    ls.dmas(nc.sync, [dict(out=G[rs_, :], in_=go[:])])
                    yield

                lanes(nc, seq, mk, list(range(EC)), body, nl=3)

        def wout_stage(i, w_out):
            def sc(gm, n0):
                return gateS[i][:, gm, (0 if n0 < T else 1):(1 if n0 < T else 2)]
            mmp(nc, seq, X, [(w_out, G)], scale=sc, accumulate=True)

        for i in range(cfg.depth):
            kind, j = i % 3, i // 3
            norm_stage(lambda r: normA[i][:, :, r], lambda r: normB[i][:, :, r], H, BF16, N)
            w_in = (I["a_w_in"], I["b_w_in"], I["c_w_in"])[kind][j]
            mmp(nc, seq, U, [(w_in[:, 0:E], H)])
            mmp(nc, seq, Z, [(w_in[:, E:2 * E], H)])
            if kind == 1:
                pool_layer(nc, seq, cfg, I, j, U, DL, P)
                gate_stage(I["b_scale"][j])
                wout_stage(i, I["b_w_out"][j])
            elif kind == 2:
                fourier_layer(nc, seq, cfg, I, j, U, FA, FB, DL, P)
                gate_stage(None)
                wout_stage(i, I["c_w_out"][j])
            else:
                mlstm_layer(nc, seq, cfg, I, j, dict(UZ=U, Z=Z, XC=XC, QT=QT, KT=KT, VT=VT, VTOK=VTOK, HD=HD, BD=BD, BD_h=BD_h,
                                                     WG=WG, GP=GP, HN=P, G=G, ident=ident, ones_b=ones_b, eps_t=eps_t,
                                                     one_t=one_t, selT=selT, BZ=BZ, LMB=LMB))
                wout_stage(i, I["a_w_out"][j])

        norm_stage(lambda r: finA[:], lambda r: zeroB[:], yT, F32, T)
        nc.sync.wait_ge(seq.prev[0], seq.prev[1])
    nc._n_seq_sems = seq.n
    return nc


def pool_layer(nc, seq, cfg, I, j, UZ, DL, P):
    T, TC, N, E, GW, EC = cfg.T, cfg.TC, cfg.N, cfg.E, cfg.GW, cfg.EC
    PAD = 16
    segs = ((0, T), (T, TC))
    with ExitStack() as es0:
        invc = es0.enter_context(nc.sbuf_tensor(uid("pinv"), [128, N], F32))
        for g in range(4):
            w = POOL_WINDOWS[g]
            lo = w // 2
            hi = w - 1 - lo
            seq.dmas(nc.sync, [dict(out=invc[:], in_=I["k_invc"][g])])

            def mk(es, l):
                bufs = []
                for si, (o, n) in enumerate(segs):
                    bufs.append(tuple(es.enter_context(nc.sbuf_tensor(uid("pb"), [128, n + 2 * PAD], F32)) for _ in range(3)))
                do = es.enter_context(nc.sbuf_tensor(uid("pdo"), [128, N], BF16))

                def z0():
                    last = None
                    for tri in bufs:
                        for t_ in tri:
                            last = nc.vector.memset(t_[:], 0.0)
                    return last

                seq.group(nc.vector, z0)
                return dict(bufs=bufs, do=do)

            def body(ls, Bf, ec, w=w, hi=hi):
                bufs, do = Bf["bufs"], Bf["do"]
                rs_ = slice(ec * 128, (ec + 1) * 128)
                ls.dmas(nc.sync, [dict(out=bufs[si][0][:, PAD:PAD + n], in_=UZ[rs_, o:o + n]) for si, (o, n) in enumerate(segs)])
                yield
                cov = 1
                cur = 0
                while cov < w:
                    nxt = 1 if cur != 1 else 2

                    def stp(cur=cur, nxt=nxt, cov=cov):
                        last = None
                        for si, (o, n) in enumerate(segs):
                            L = n + 2 * PAD
                            last = nc.vector.tensor_tensor(out=bufs[si][nxt][:, cov:L], in0=bufs[si][cur][:, cov:L],
                                                           in1=bufs[si][cur][:, 0:L - cov], op=ALU.add)
                        return last

                    ls.group(nc.vector, stp)
                    yield
                    cur = nxt
                    cov *= 2
                tmpi = 1 if cur != 1 else 2

                def m1(cur=cur, tmpi=tmpi):
                    last = None
                    for si, (o, n) in enumerate(segs):
                        last = nc.vector.tensor_tensor(out=bufs[si][tmpi][:, PAD:PAD + n], in0=bufs[si][cur][:, PAD + hi:PAD + hi + n],
                                                       in1=invc[:, o:o + n], op=ALU.mult)
                    return last

                ls.group(nc.vector, m1)
                yield

                def m2(tmpi=tmpi):
                    last = None
                    for si, (o, n) in enumerate(segs):
                        last = nc.vector.tensor_tensor(out=do[:, o:o + n], in0=bufs[si][tmpi][:, PAD:PAD + n],
                                                       in1=bufs[si][0][:, PAD:PAD + n], op=ALU.subtract)
                    return last

                ls.group(nc.vector, m2)
                yield
                ls.dmas(nc.sync, [dict(out=DL[rs_, :], in_=do[:])])
                yield

            cpg = GW // 128
            lanes(nc, seq, mk, list(range(g * cpg, (g + 1) * cpg)), body)
    for g in range(4):
        mmp(nc, seq, P[g * GW:(g + 1) * GW, :], [(I["b_w_grp"][j, g], DL[g * GW:(g + 1) * GW, :])])


def fourier_layer(nc, seq, cfg, I, j, UZ, FA, FB, DL, P):
    T, TC, N, E, GW = cfg.T, cfg.TC, cfg.N, cfg.E, cfg.GW
    for g in range(4):
        gs = slice(g * GW, (g + 1) * GW)
        mmp(nc, seq, FA[:, gs], [(UZ[gs, :], I["k_cc"])])
        mmp(nc, seq, FB[:, gs], [(UZ[gs, :], I["k_sc"])])
    mmp(nc, seq, DL[:, 0:T], [(FA[0:T, :], I["k_ct"]), (FB[0:T, :], I["k_nst"])])
    mmp(nc, seq, DL[:, T:N], [(FA[T:N, :], I["k_ctc"]), (FB[T:N, :], I["k_nstc"])])
    for g in range(4):
        gs = slice(g * GW, (g + 1) * GW)
        mmp(nc, seq, P[gs, :], [(I["c_w_grp"][j, g], DL[gs, :])])


def mlstm_layer(nc, seq, cfg, I, j, S):
    T, TC, N, E, NH, DH, EC = cfg.T, cfg.TC, cfg.N, cfg.E, cfg.NH, cfg.DH, cfg.EC
    UZ, XC, QT, KT, VT, VTOK, HD, BD, WG, GP, HN, G = (S[k] for k in ("UZ", "XC", "QT", "KT", "VT", "VTOK", "HD", "BD", "WG", "GP", "HN", "G"))
    Z = S["Z"]
    ident, ones_b, eps_t, one_t, selT = S["ident"], S["ones_b"], S["eps_t"], S["one_t"], S["selT"]
    segs = ((0, T), (T, TC))
    NTC = N // 128
    DCH = DH // 128

    with ExitStack() as es0:
        cw = es0.enter_context(nc.sbuf_tensor(uid("cw"), [128, 4, EC], F32))
        cb = es0.enter_context(nc.sbuf_tensor(uid("cb"), [128, EC], F32))
        seq.dmas(nc.sync, [dict(out=cw[:, t_, :], in_=I["a_conv_w"][j, t_].rearrange("(c p) -> p c", p=128)) for t_ in range(4)]
                 + [dict(out=cb[:], in_=I["a_conv_b"][j].rearrange("(c p) -> p c", p=128))])

        def mk(es, l):
            ub = [es.enter_context(nc.sbuf_tensor(uid("cu"), [128, n + 3], F32)) for (o, n) in segs]
            acc = [es.enter_context(nc.sbuf_tensor(uid("ca"), [128, N], F32)) for _ in range(2)]

            def z0():
                nc.vector.memset(ub[0][:], 0.0)
                return nc.vector.memset(ub[1][:], 0.0)

            seq.group(nc.vector, z0)
            return dict(ub=ub, acc=acc)

        def body(ls, Bf, ec):
            ub, acc = Bf["ub"], Bf["acc"]
            rs_ = slice(ec * 128, (ec + 1) * 128)
            ls.dmas(nc.sync, [dict(out=ub[si][:, 1:1 + n], in_=UZ[rs_, o:o + n]) for si, (o, n) in enumerate(segs)])
            yield
            for t_ in range(4):
                src, dst = acc[(t_ + 1) % 2], acc[t_ % 2]

                def stp(t_=t_, src=src, dst=dst):
                    last = None
                    for si, (o, n) in enumerate(segs):
                        if t_ == 0:
                            last = nc.vector.tensor_scalar(out=dst[:, o:o + n], in0=ub[si][:, 0:n], scalar1=cw[:, 0, ec:ec + 1],
                                                           scalar2=None, op0=ALU.mult)
                        else:
                            last = nc.vector.scalar_tensor_tensor(out=dst[:, o:o + n], in0=ub[si][:, t_:t_ + n],
                                                                  scalar=cw[:, t_, ec:ec + 1], in1=src[:, o:o + n],
                                                                  op0=ALU.mult, op1=ALU.add)
                    return last

                ls.group(nc.vector, stp)
                yield
            ls.group(nc.scalar, lambda: nc.scalar.activation(out=acc[0][:], in_=acc[1][:], func=AF.Silu, bias=cb[:, ec:ec + 1], scale=1.0))
            yield
            ls.dmas(nc.sync, [dict(out=XC[rs_, :], in_=acc[0][:])])
            yield

        lanes(nc, seq, mk, list(range(EC)), body, nl=3)

    with ExitStack() as es:
        zt = es.enter_context(nc.sbuf_tensor(uid("bz"), [128, EC, 128], F32))
        seq.group(nc.vector, lambda: nc.vector.memset(zt[:], 0.0))
        seq.dmas(nc.sync, [dict(out=BD[m * EC * 128:(m + 1) * EC * 128, :].rearrange("(c p) q -> p c q", p=128), in_=zt[:]) for m in range(3)])
        items = []
        for m, wname in enumerate(("a_wq", "a_wk", "a_wv")):
            w = I[wname]
            for c in range(EC):
                dst = bass.AP(S["BD_h"], (m * EC + c) * 128 * 128, [[516, 32], [128, 4], [1, 4]])
                items.append(dict(out=dst, in_=w[j, 32 * c:32 * (c + 1), :, :]))
        for k0 in range(0, len(items), 32):
            seq.dmas(nc.sync, items[k0:k0 + 32])
    if True:
        NT = 512
        tiles = [(n0, min(NT, N - n0)) for n0 in range(0, N, NT)]
        jobs = [(m, n0, ns) for m in range(3) for (n0, ns) in tiles]

        def mk(es, l):
            return dict(xcb=es.enter_context(nc.sbuf_tensor(uid("qx"), [128, N], BF16)),
                        ubf=es.enter_context(nc.sbuf_tensor(uid("qu"), [128, N], BF16)),
                        bdb=es.enter_context(nc.sbuf_tensor(uid("qb"), [128, 3, 128], BF16)),
                        oq=es.enter_context(nc.sbuf_tensor(uid("qo"), [128, 3, N], BF16)),
                        ov=es.enter_context(nc.sbuf_tensor(uid("qv"), [128, NTC, 128], BF16)),
                        ps=[es.enter_context(nc.psum_tensor(uid("qps"), [128, 512], F32)) for _ in range(4)])

        def body(ls, Bf, ec):
            xcb, ubf, bdb, oq, ov, ps = (Bf[k] for k in ("xcb", "ubf", "bdb", "oq", "ov", "ps"))
            rs_ = slice(ec * 128, (ec + 1) * 128)
            ls.dmas(nc.gpsimd, [dict(out=xcb[:], in_=XC[rs_, :]), dict(out=ubf[:], in_=UZ[rs_, :])]
                    + [dict(out=bdb[:, m, :], in_=BD[(m * EC + ec) * 128:(m * EC + ec + 1) * 128, :]) for m in range(3)])
            yield
            for b0 in range(0, len(jobs), 4):
                jb = jobs[b0:b0 + 4]

                def pe(jb=jb):
                    last = None
                    for bi, (m, n0, ns) in enumerate(jb):
                        src = ubf if m == 2 else xcb
                        last = nc.tensor.matmul(ps[bi][:, :ns], bdb[:, m, :], src[:, n0:n0 + ns], start=True, stop=True)
                    return last

                ls.group(nc.tensor, pe)
                yield

                def ev(jb=jb):
                    last = None
                    for bi, (m, n0, ns) in enumerate(jb):
                        last = nc.scalar.copy(out=oq[:, m, n0:n0 + ns], in_=ps[bi][:, :ns])
                    return last

                ls.group(nc.scalar, ev)
                yield
            ls.dmas(nc.sync, [dict(out=QT[rs_, :], in_=oq[:, 0, :]), dict(out=KT[rs_, :], in_=oq[:, 1, :]), dict(out=VT[rs_, :], in_=oq[:, 2, :])])
            yield
            for b0 in range(0, NTC, 16):
                tcs = list(range(b0, min(NTC, b0 + 16)))

                def pe2(tcs=tcs):
                    last = None
                    for bi, tc in enumerate(tcs):
                        last = nc.tensor.matmul(ps[bi // 4][:, (bi % 4) * 128:(bi % 4 + 1) * 128], ubf[:, tc * 128:(tc + 1) * 128],
                                                bdb[:, 2, :], start=True, stop=True)
                    return last

                ls.group(nc.tensor, pe2)
                yield

                def ev2(tcs=tcs):
                    last = None
                    for bi, tc in enumerate(tcs):
                        last = nc.scalar.copy(out=ov[:, tc, :], in_=ps[bi // 4][:, (bi % 4) * 128:(bi % 4 + 1) * 128])
                    return last

                ls.group(nc.scalar, ev2)
                yield
            ls.dmas(nc.sync, [dict(out=VTOK[:, rs_].rearrange("(tc p) e -> p tc e", p=128), in_=ov[:])])
            yield

        lanes(nc, seq, mk, list(range(EC)), body)

    items = []
    for gi, (wn, z) in enumerate((("a_w_ig", 0), ("a_w_ig", 1), ("a_w_fg", 0), ("a_w_fg", 1))):
        for r0 in range(0, 3 * E, 2048):
            r1 = min(3 * E, r0 + 2048)
            items.append(dict(out=WG[r0:r1, gi * 8:(gi + 1) * 8], in_=I[wn][j, z, r0:r1, :]))
    for k0 in range(0, len(items), 16):
        seq.dmas(nc.sync, items[k0:k0 + 16])
    mmp(nc, seq, GP, [(WG[0:E, :], QT), (WG[E:2 * E, :], KT), (WG[2 * E:3 * E, :], VT)])

    with ExitStack() as es:
        def t8(name):
            return es.enter_context(nc.sbuf_tensor(uid(name), [8, N], F32))
        Bz = [t8("Bz0"), t8("Bz1")]
        LmB = [t8("Lm0"), t8("Lm1")]
        li = [t8("li0"), t8("li1")]
        xf = [t8("xf0"), t8("xf1")]
        tmp = t8("tmp")
        onesr = t8("onesr")
        bi_ = es.enter_context(nc.sbuf_tensor(uid("bi"), [8, 2], F32))
        bf_ = es.enter_context(nc.sbuf_tensor(uid("bf"), [8, 2], F32))
        tot = es.enter_context(nc.sbuf_tensor(uid("tot"), [8, 4], F32))
        seq.dmas(nc.sync, [dict(out=li[z][:], in_=GP[8 * z:8 * z + 8, :]) for z in range(2)]
                 + [dict(out=xf[z][:], in_=GP[16 + 8 * z:24 + 8 * z, :]) for z in range(2)]
                 + [dict(out=bi_[:, z:z + 1], in_=I["a_b_ig"][j, z].rearrange("(n o) -> n o", o=1)) for z in range(2)]
                 + [dict(out=bf_[:, z:z + 1], in_=I["a_b_fg"][j, z].rearrange("(n o) -> n o", o=1)) for z in range(2)])

        def g1():
            nc.vector.memset(onesr[:], 1.0)
            return nc.vector.tensor_scalar(out=bf_[:], in0=bf_[:], scalar1=-1.0, scalar2=None, op0=ALU.mult)

        seq.group(nc.vector, g1)

        def g2():
            last = None
            for z in range(2):
                nc.scalar.activation(out=li[z][:], in_=li[z][:], func=AF.Identity, bias=bi_[:, z:z + 1], scale=1.0)
                last = nc.scalar.activation(out=xf[z][:], in_=xf[z][:], func=AF.Exp, bias=bf_[:, z:z + 1], scale=-1.0)
            return last

        seq.group(nc.scalar, g2)

        def g3():
            last = None
            for z in range(2):
                last = nc.scalar.activation(out=xf[z][:], in_=xf[z][:], func=AF.Ln, bias=S["one_t"][:8, :], scale=1.0)
            return last

        seq.group(nc.scalar, g3)
        def g4():
            last = None
            for z in range(2):
                for (o, n) in segs:
                    last = nc.vector.tensor_tensor_scan(out=Bz[z][:, o:o + n], data0=onesr[:, o:o + n], data1=xf[z][:, o:o + n],
                                                        initial=0.0, op0=ALU.mult, op1=ALU.add)
            return last

        seq.group(nc.vector, g4)
        def g5():
            nc.vector.tensor_copy(out=tot[:, 0:1], in_=Bz[0][:, N - 1:N])
            nc.vector.tensor_copy(out=tot[:, 1:2], in_=Bz[1][:, N - 1:N])
            nc.vector.tensor_copy(out=tot[:, 2:3], in_=Bz[1][:, T - 1:T])
            return nc.vector.tensor_tensor(out=tmp[:], in0=xf[1][:], in1=Bz[1][:], op=ALU.subtract)

        seq.group(nc.vector, g5)

        def g6():
            nc.vector.tensor_tensor(out=tot[:, 3:4], in0=tot[:, 2:3], in1=tot[:, 1:2], op=ALU.add)
            return nc.vector.tensor_scalar(out=Bz[0][:, 0:T], in0=Bz[0][:, 0:T], scalar1=tot[:, 0:1], scalar2=None, op0=ALU.add)

        seq.group(nc.vector, g6)

        def g7():
            nc.vector.tensor_scalar(out=Bz[1][:, 0:T], in0=tmp[:, 0:T], scalar1=tot[:, 3:4], scalar2=None, op0=ALU.add)
            nc.vector.tensor_scalar(out=Bz[1][:, T:N], in0=tmp[:, T:N], scalar1=tot[:, 1:2], scalar2=None, op0=ALU.add)
            return nc.vector.tensor_scalar(out=Bz[0][:], in0=Bz[0][:], scalar1=-1.0, scalar2=None, op0=ALU.mult)

        seq.group(nc.vector, g7)
        seq.group(nc.vector, lambda: nc.vector.tensor_scalar(out=Bz[1][:], in0=Bz[1][:], scalar1=-1.0, scalar2=None, op0=ALU.mult))

        def g8():
            last = None
            for z in range(2):
                last = nc.vector.scalar_tensor_tensor(out=LmB[z][:], in0=li[z][:], scalar=-0.5 * math.log(DH), in1=Bz[z][:],
                                                      op0=ALU.add, op1=ALU.subtract)
            return last

        seq.group(nc.vector, g8)
        seq.dmas(nc.sync, [dict(out=S["BZ"][z], in_=Bz[z][:]) for z in range(2)] + [dict(out=S["LMB"][z], in_=LmB[z][:]) for z in range(2)])

    if True:
        QW = 256
        TS = QW // 128
        VW = min(DH, 512)
        VH = DH // VW
        KTL = T // 128
        with ExitStack() as e5:
            kT = e5.enter_context(nc.sbuf_tensor(uid("akT"), [128, DCH, N], BF16))
            vk = e5.enter_context(nc.sbuf_tensor(uid("avk"), [128, NTC, DH], BF16))
            qT2 = [e5.enter_context(nc.sbuf_tensor(uid("aqT"), [128, DCH, QW], BF16)) for _ in range(2)]
            csb = e5.enter_context(nc.sbuf_tensor(uid("acs"), [128, NTC], F32))
            lmb = e5.enter_context(nc.sbuf_tensor(uid("almb"), [8, N], F32))
            bzt2 = [e5.enter_context(nc.sbuf_tensor(uid("abzt"), [8, QW], F32)) for _ in range(2)]
            brow = e5.enter_context(nc.sbuf_tensor(uid("abr"), [128, 3, QW], F32))
            mask = e5.enter_context(nc.sbuf_tensor(uid("amk"), [128, 4, 256], F32))
            esb = e5.enter_context(nc.sbuf_tensor(uid("aes"), [128, 4, QW], F32))
            wsb = e5.enter_context(nc.sbuf_tensor(uid("aws"), [128, 4, QW], BF16))
            rsb = e5.enter_context(nc.sbuf_tensor(uid("ars"), [128, TS], F32))
            hsb2 = [e5.enter_context(nc.sbuf_tensor(uid("ahs"), [128, TS, DH], F32)) for _ in range(2)]
            ps_num = [[e5.enter_context(nc.psum_tensor(uid("apn"), [128, 512], F32)) for _ in range(VH)] for _ in range(TS)]
            ps_den = e5.enter_context(nc.psum_tensor(uid("apd"), [128, 512], F32))
            ps_s = [e5.enter_context(nc.psum_tensor(uid("aps"), [128, 512], F32)) for _ in range(2)]
            ps_t = e5.enter_context(nc.psum_tensor(uid("apt"), [128, 512], F32))
            seq.dmas(nc.sync, [dict(out=mask[:], in_=I["k_mask"])])
            A = _CTX.setdefault("att", None) or dict(sS=nc.alloc_semaphore("aS"), sE=nc.alloc_semaphore("aE"), sV=nc.alloc_semaphore("aV"),
                                                     sN=nc.alloc_semaphore("aN"), vS=0, vE=0, vV=0, vN=0, gb=0, qn=0,
                                                     sLD=[nc.alloc_semaphore("aLD0"), nc.alloc_semaphore("aLD1")], vLD=[0, 0],
                                                     sST=[nc.alloc_semaphore("aST0"), nc.alloc_semaphore("aST1")], vST=[0, 0])
            _CTX["att"] = A
            for n in range(NH):
                hs_ = slice(n * DH, (n + 1) * DH)
                seq.dmas(nc.sync, [dict(out=kT[:], in_=KT[hs_, :].rearrange("(c p) t -> p c t", p=128)),
                                   dict(out=vk[:], in_=VTOK[:, hs_].rearrange("(tc p) v -> p tc v", p=128))])
                for z in range(2):
                    seq.dmas(nc.sync, [dict(out=lmb[:], in_=S["LMB"][z])])

                    def pc():
                        last = None
                        for kt in range(NTC):
                            last = nc.tensor.matmul(ps_t[:, kt:kt + 1], lmb[:, kt * 128:(kt + 1) * 128], ident[:8, n:n + 1],
                                                    start=True, stop=True)
                        return last

                    seq.group(nc.tensor, pc)
                    seq.group(nc.vector, lambda: nc.vector.tensor_copy(out=csb[:], in_=ps_t[:, :NTC]))
                    qtiles = [(q0, True) for q0 in range(0, T, QW)] + [(T, False)]
                    for qidx, (q0, is_lat) in enumerate(qtiles):
                        qpar = A["qn"] % 2
                        qT, bzt, hsb = qT2[qpar], bzt2[qpar], hsb2[qpar]
                        qi = q0 // QW if is_lat else 0
                        if is_lat:
                            full = [KTL + c for c in range(TC // 128)]
                            full += list(range(0, 2 * qi)) if z == 0 else list(range(2 * qi + 2, KTL))
                            keys = [(kt, 0) for kt in full] + [(2 * qi, 1), (2 * qi + 1, 2)]
                        else:
                            keys = [(KTL, 1), (KTL + 1, 2)]
                        def load_q(par_, q0_):
                            nc.sync.dma_start(out=qT2[par_][:], in_=QT[hs_, q0_:q0_ + QW].rearrange("(c p) t -> p c t", p=128)).then_inc(A["sLD"][par_], 16)
                            nc.sync.dma_start(out=bzt2[par_][:], in_=S["BZ"][z][:, q0_:q0_ + QW]).then_inc(A["sLD"][par_], 16)
                            A["vLD"][par_] += 32

                        if qidx == 0:
                            nc.sync.wait_ge(seq.prev[0], seq.prev[1])
                            load_q(qpar, q0)
                        ld_val = A["vLD"][qpar]

                        def brow_mm():
                            nc.tensor.wait_ge(A["sLD"][qpar], ld_val)
                            return nc.tensor.matmul(ps_t[:, :QW], selT[:, n, :], bzt[:], start=True, stop=True)

                        seq.group(nc.tensor, brow_mm)
                        if qidx + 1 < len(qtiles):
                            nc.sync.wait_ge(seq.prev[0], seq.prev[1])
                            load_q(1 - qpar, qtiles[qidx + 1][0])

                        def gb():
                            nc.vector.tensor_copy(out=brow[:, 0, :], in_=ps_t[:, :QW])
                            nc.vector.tensor_tensor(out=brow[:, 1, :], in0=ps_t[:, :QW], in1=mask[:, 2 * z, :], op=ALU.add)
                            return nc.vector.tensor_tensor(out=brow[:, 2, :], in0=ps_t[:, :QW], in1=mask[:, 2 * z + 1, :], op=ALU.add)

                        seq.group(nc.vector, gb)
                        nk = len(keys)
                        T0 = seq.prev
                        batches = [keys[b0:b0 + 2] for b0 in range(0, nk, 2)]
                        nb = len(batches)
                        sval, eval_, vval, nval = {}, {}, {}, {}

                        def emit_E(b):
                            par = (A["gb"] + b) % 2
                            if b == 0:
                                nc.scalar.wait_ge(T0[0], T0[1])
                            if b >= 2:
                                nc.scalar.wait_ge(A["sV"], vval[b - 2])
                            last = None
                            for bi, (kt, mt) in enumerate(batches[b]):
                                last = nc.scalar.activation(out=esb[:, par * 2 + bi, :], in_=brow[:, mt, :], func=AF.Exp,
                                                            bias=csb[:, kt:kt + 1], scale=1.0)
                            last.then_inc(A["sE"], 1)
                            A["vE"] += 1
                            eval_[b] = A["vE"]

                        def emit_S(b):
                            par = (A["gb"] + b) % 2
                            if b == 0:
                                nc.tensor.wait_ge(T0[0], T0[1])
                            if b >= 2:
                                nc.tensor.wait_ge(A["sV"], vval[b - 2])
                            last = None
                            for bi, (kt, mt) in enumerate(batches[b]):
                                o_ = ps_s[par][:, bi * QW:(bi + 1) * QW]
                                for dc in range(DCH):
                                    last = nc.tensor.matmul(o_, kT[:, dc, kt * 128:(kt + 1) * 128], qT[:, dc, :],
                                                            start=(dc == 0), stop=(dc == DCH - 1))
                            last.then_inc(A["sS"], 1)
                            A["vS"] += 1
                            sval[b] = A["vS"]

                        def emit_V(b):
                            par = (A["gb"] + b) % 2
                            nc.vector.wait_ge(A["sS"], sval[b])
                            nc.vector.wait_ge(A["sE"], eval_[b])
                            if b >= 2:
                                nc.vector.wait_ge(A["sN"], nval[b - 2])
                            last = None
                            for bi, (kt, mt) in enumerate(batches[b]):
                                o_ = ps_s[par][:, bi * QW:(bi + 1) * QW]
                                last = nc.vector.tensor_tensor(out=wsb[:, par * 2 + bi, :], in0=o_, in1=esb[:, par * 2 + bi, :], op=ALU.mult)
                            last.then_inc(A["sV"], 1)
                            A["vV"] += 1
                            vval[b] = A["vV"]

                        def emit_N(b):
                            par = (A["gb"] + b) % 2
                            nc.tensor.wait_ge(A["sV"], vval[b])
                            last = None
                            for bi, (kt, mt) in enumerate(batches[b]):
                                first = (b == 0 and bi == 0)
                                lastk = (b == nb - 1 and bi == len(batches[b]) - 1)
                                for ts in range(TS):
                                    lw = wsb[:, par * 2 + bi, ts * 128:(ts + 1) * 128]
                                    for vh in range(VH):
                                        nc.tensor.matmul(ps_num[ts][vh][:, :VW], lw, vk[:, kt, vh * VW:(vh + 1) * VW],
                                                         start=first, stop=lastk)
                                    last = nc.tensor.matmul(ps_den[:, ts:ts + 1], lw, ones_b[:, 0:1], start=(first and ts == 0),
                                                            stop=lastk, skip_group_check=True)
                            last.then_inc(A["sN"], 1)
                            A["vN"] += 1
                            nval[b] = A["vN"]

                        for b in range(nb):
                            emit_E(b)
                            emit_S(b)
                            emit_V(b)
                            if b >= 1:
                                emit_N(b - 1)
                        emit_N(nb - 1)
                        A["gb"] += nb
                        seq.prev = (A["sN"], A["vN"])
                        seq.group(nc.scalar, lambda: nc.scalar.activation(out=rsb[:], in_=ps_den[:, :TS], func=AF.Abs))
                        seq.group(nc.vector, lambda: nc.vector.tensor_scalar(out=rsb[:], in0=rsb[:], scalar1=1.0, scalar2=None,
                                                                             op0=ALU.max))
                        seq.group(nc.vector, lambda: nc.vector.reciprocal(out=rsb[:], in_=rsb[:]))

                        def a2():
                            last = None
                            for ts in range(TS):
                                for vh in range(VH):
                                    last = nc.scalar.activation(out=hsb[:, ts, vh * VW:(vh + 1) * VW], in_=ps_num[ts][vh][:, :VW],
                                                                func=AF.Identity, scale=rsb[:, ts:ts + 1])
                            return last

                        def a2w():
                            nc.scalar.wait_ge(A["sST"][qpar], A["vST"][qpar])
                            return a2()

                        seq.group(nc.scalar, a2w)
                        nc.sync.wait_ge(seq.prev[0], seq.prev[1])
                        nc.sync.dma_start(out=HD[z][q0:q0 + QW, hs_].rearrange("(ts p) v -> p ts v", p=128), in_=hsb[:]).then_inc(A["sST"][qpar], 16)
                        A["vST"][qpar] += 16
                        A["qn"] += 1
            nc.vector.wait_ge(A["sST"][0], A["vST"][0])
            nc.vector.wait_ge(A["sST"][1], A["vST"][1])
            seq.group(nc.vector, lambda: nc.vector.memset(rsb[:], 0.0))

    NHH = NH // 2
    ECH = EC // 2

    def mk6(es, l):
        return dict(a=es.enter_context(nc.sbuf_tensor(uid("fa"), [128, NHH, DH], F32)),
                    b=es.enter_context(nc.sbuf_tensor(uid("fb"), [128, NHH, DH], F32)),
                    c=es.enter_context(nc.sbuf_tensor(uid("fc"), [128, NHH, DH], F32)),
                    st=es.enter_context(nc.sbuf_tensor(uid("fs"), [128, 4, NHH], F32)),
                    hT=es.enter_context(nc.sbuf_tensor(uid("fT"), [128, ECH, 128], F32)),
                    ps=[es.enter_context(nc.psum_tensor(uid("fps"), [128, 512], F32)) for _ in range(4)])

    def body6(ls, Bf, item):
        tc, hh = item
        a, b, c, st, hT, ps = (Bf[k] for k in ("a", "b", "c", "st", "hT", "ps"))
        ts_ = slice(tc * 128, (tc + 1) * 128)
        es_ = slice(hh * NHH * DH, (hh + 1) * NHH * DH)
        ls.dmas(nc.sync, [dict(out=a[:], in_=HD[0][ts_, es_].rearrange("p (n v) -> p n v", n=NHH)),
                          dict(out=b[:], in_=HD[1][ts_, es_].rearrange("p (n v) -> p n v", n=NHH))])
        yield
        ls.group(nc.vector, lambda: nc.vector.tensor_tensor(out=a[:], in0=a[:], in1=b[:], op=ALU.add))
        yield
        ls.group(nc.vector, lambda: nc.vector.reduce_sum(out=st[:, 0, :], in_=a[:], axis=AX.X))
        yield
        ls.group(nc.vector, lambda: nc.vector.tensor_scalar(out=st[:, 1, :], in0=st[:, 0, :], scalar1=-1.0 / DH, scalar2=None, op0=ALU.mult))
        yield

        def f1():
            last = None
            for n in range(NHH):
                last = nc.scalar.activation(out=b[:, n, :], in_=a[:, n, :], func=AF.Identity, bias=st[:, 1, n:n + 1], scale=1.0)
            return last

        ls.group(nc.scalar, f1)
        yield
        ls.group(nc.vector, lambda: nc.vector.tensor_tensor(out=c[:], in0=b[:], in1=b[:], op=ALU.mult))
        yield
        ls.group(nc.vector, lambda: nc.vector.reduce_sum(out=st[:, 2, :], in_=c[:], axis=AX.X))
        yield
        ls.group(nc.scalar, lambda: nc.scalar.activation(out=st[:, 3, :], in_=st[:, 2, :], func=AF.Sqrt, bias=eps_t[:], scale=1.0 / DH))
        yield
        ls.group(nc.vector, lambda: nc.vector.reciprocal(out=st[:, 3, :], in_=st[:, 3, :]))
        yield

        def f3():
            last = None
            for n in range(NHH):
                last = nc.vector.tensor_scalar(out=a[:, n, :], in0=b[:, n, :], scalar1=st[:, 3, n:n + 1], scalar2=None, op0=ALU.mult)
            return last

        ls.group(nc.vector, f3)
        yield
        for b0 in range(0, ECH, 16):
            ecs = list(range(b0, min(ECH, b0 + 16)))

            def pe(ecs=ecs):
                last = None
                for bi, ec in enumerate(ecs):
                    n, vc = divmod(ec, DCH)
                    last = nc.tensor.matmul(ps[bi // 4][:, (bi % 4) * 128:(bi % 4 + 1) * 128], a[:, n, vc * 128:(vc + 1) * 128],
                                            ident[:], start=True, stop=True)
                return last

            ls.group(nc.tensor, pe)
            yield

            def ev(ecs=ecs):
                last = None
                for bi, ec in enumerate(ecs):
                    last = nc.scalar.copy(out=hT[:, ec, :], in_=ps[bi // 4][:, (bi % 4) * 128:(bi % 4 + 1) * 128])
                return last

            ls.group(nc.scalar, ev)
            yield
        ls.dmas(nc.sync, [dict(out=HN[hh * ECH * 128:(hh + 1) * ECH * 128, ts_].rearrange("(c p) t -> p c t", p=128), in_=hT[:])])
        yield

    lanes(nc, seq, mk6, [(tc, hh) for tc in range(NTC) for hh in range(2)], body6)

    with ExitStack() as es0:
        hw = es0.enter_context(nc.sbuf_tensor(uid("g5"), [128, EC], F32))
        sk = es0.enter_context(nc.sbuf_tensor(uid("g6"), [128, EC], F32))
        seq.dmas(nc.sync, [dict(out=hw[:], in_=I["a_hnorm_w"][j].rearrange("(c p) -> p c", p=128)),
                           dict(out=sk[:], in_=I["a_skip"][j].rearrange("(c p) -> p c", p=128))])

        def mk7(es, l):
            return dict(ht=es.enter_context(nc.sbuf_tensor(uid("g1"), [128, N], F32)),
                        xt=es.enter_context(nc.sbuf_tensor(uid("g2"), [128, N], F32)),
                        zt=es.enter_context(nc.sbuf_tensor(uid("g3"), [128, N], F32)),
                        go=es.enter_context(nc.sbuf_tensor(uid("g4"), [128, N], BF16)))

        def body7(ls, Bf, ec):
            ht, xt, zt, go = Bf["ht"], Bf["xt"], Bf["zt"], Bf["go"]
            rs_ = slice(ec * 128, (ec + 1) * 128)
            ls.dmas(nc.sync, [dict(out=ht[:], in_=HN[rs_, :]), dict(out=xt[:], in_=XC[rs_, :]), dict(out=zt[:], in_=Z[rs_, :])])
            yield

            def f1():
                nc.scalar.activation(out=ht[:], in_=ht[:], func=AF.Identity, scale=hw[:, ec:ec + 1])
                return nc.scalar.activation(out=zt[:], in_=zt[:], func=AF.Silu)

            ls.group(nc.scalar, f1)
            yield
            ls.group(nc.vector, lambda: nc.vector.scalar_tensor_tensor(out=xt[:], in0=xt[:], scalar=sk[:, ec:ec + 1], in1=ht[:],
                                                                       op0=ALU.mult, op1=ALU.add))
            yield
            ls.group(nc.vector, lambda: nc.vector.tensor_tensor(out=go[:], in0=xt[:], in1=zt[:], op=ALU.mult))
            yield
            ls.dmas(nc.sync, [dict(out=G[rs_, :], in_=go[:])])
            yield

        lanes(nc, seq, mk7, list(range(EC)), body7, nl=3)


_NP2BIR = {np.dtype(np.float32): F32, np.dtype(ml_dtypes.bfloat16): BF16}


def run(cfg, inputs, debug=(), trace=False):
    consts = host_consts(cfg)
    B = inputs["x"].shape[0]
    weights = {k: np.ascontiguousarray(inputs[k], dtype=np.float32) for k in WEIGHT_NAMES}
    shapes = {k: (v.shape, F32) for k, v in weights.items()}
    shapes["xT"] = ((cfg.D, cfg.N), F32)
    shapes["ccT"] = ((cfg.D, 2), F32)
    const_shapes = {k: (v.shape, _NP2BIR[v.dtype]) for k, v in consts.items()}
    nc = build(cfg, shapes, const_shapes, debug=debug)
    in_maps = []
    for b in range(B):
        m = dict(weights)
        m.update(consts)
        m["xT"] = np.ascontiguousarray(np.concatenate([inputs["x"][b].T, inputs["ctx"][b].T], axis=1), dtype=np.float32)
        m["ccT"] = np.ascontiguousarray(np.stack([inputs["c"][b], inputs["c_ctx"]], axis=1), dtype=np.float32)
        in_maps.append(m)
    res = run_bass_kernel_spmd(nc, in_maps, core_ids=list(range(B)), trace=trace)
    out = np.stack([np.ascontiguousarray(res.results[b]["yT"].T) for b in range(B)], axis=0).astype(np.float32)
    return out, res


def kernel(**inputs):
    cfg = Cfg()
    out, _ = run(cfg, inputs)
    return out
```
